# Optimizing a Trainium2 kernel written in Bass

```python
import math
import jax, jax.numpy as jnp
from jax import lax
import numpy as np

D_MODEL = 1024
BATCH = 16
SEQ = 2048
DEPTH = 4

CHUNK = 64
N_EVEN = (DEPTH + 1) // 2
N_ODD = DEPTH // 2
DEEPNORM_ALPHA = (2.0 * DEPTH) ** 0.25
DEEPNORM_BETA = (8.0 * DEPTH) ** -0.25
D_FF = 2816
N_SUB = 3
LN_EPS = 1e-5
RMS_EPS = 1e-6
NEG_INF = -1e30

A_HEADS = 8
A_HEAD_DIM = 64
A_WIDTH = A_HEADS * A_HEAD_DIM
IDX_HEADS = 8
IDX_DIM = 64
TOPK_MAX = 256
SPARSE_Q_BLOCK = 32

B_HEADS = 4
B_HEAD_DIM = 128
B_WIDTH = B_HEADS * B_HEAD_DIM
CONV_K = 4

EVEN_SIZES = (A_WIDTH, A_WIDTH, A_WIDTH, IDX_HEADS * IDX_DIM, IDX_DIM, IDX_HEADS,
              3 * B_WIDTH, B_HEADS, B_HEADS, B_WIDTH)
EVEN_IN = sum(EVEN_SIZES)
EVEN_SPLITS = tuple(int(s) for s in np.cumsum(EVEN_SIZES)[:-1])
EVEN_MIX = A_WIDTH + B_WIDTH

C_HEADS = 16
C_Q_RANK = 384
C_KV_RANK = 256
C_NOPE = 64
C_ROPE = 32
C_V = 64
C_IN = C_Q_RANK + C_KV_RANK + C_ROPE
C_MIX = C_HEADS * C_V
ROPE_THETA = 10000.0
DENSE_Q_BLOCK = 128

kernel_name = "hybrid_dsa_gdn_mla_deepnorm_adaln"


def layer_norm(x, g, b):
    xf = x.astype(jnp.float32)
    mu = jnp.mean(xf, -1, keepdims=True)
    var = jnp.mean(jnp.square(xf - mu), -1, keepdims=True)
    return ((xf - mu) * lax.rsqrt(var + LN_EPS)).astype(x.dtype) * g + b


def rms_norm(x, g):
    xf = x.astype(jnp.float32)
    return (xf * lax.rsqrt(jnp.mean(xf * xf, -1, keepdims=True) + RMS_EPS)).astype(x.dtype) * g


def l2_normalize(x):
    return x * lax.rsqrt(jnp.sum(x * x, -1, keepdims=True) + RMS_EPS)


def swiglu(u, w_gate, w_up, w_down):
    return (jax.nn.silu(u @ w_gate) * (u @ w_up)) @ w_down


def alibi_slopes(n):
    return jnp.asarray(2.0 ** (-8.0 * np.arange(1, n + 1) / n), dtype=jnp.float32)


def rope(x, pos):
    half = x.shape[-1] // 2
    inv_freq = ROPE_THETA ** (-jnp.arange(half, dtype=jnp.float32) / half)
    ang = pos.astype(jnp.float32)[:, :, None] * inv_freq
    cos, sin = jnp.cos(ang)[:, :, None, :], jnp.sin(ang)[:, :, None, :]
    xf = x.astype(jnp.float32)
    x1, x2 = xf[..., :half], xf[..., half:]
    return jnp.concatenate([x1 * cos - x2 * sin, x1 * sin + x2 * cos], -1).astype(x.dtype)


def to_query_blocks(t, size):
    return t.reshape(t.shape[0], t.shape[1] // size, size, *t.shape[2:]).swapaxes(0, 1)


def from_query_blocks(t):
    t = t.swapaxes(0, 1)
    return t.reshape(t.shape[0], t.shape[1] * t.shape[2], -1)


def dsa_attention(q, k, v, iq, ik, iw, pos):
    top_k = min(TOPK_MAX, pos.shape[1] // 4)
    key_chunk = pos // CHUNK
    slopes = alibi_slopes(A_HEADS)
    ik32 = ik.astype(jnp.float32)
    iw32 = iw.astype(jnp.float32) * IDX_HEADS ** -0.5
    gather = jax.vmap(lambda table, idx: table[idx])

    def block(args):
        qb, iqb, iwb, pb = args
        qchunk = pb // CHUNK
        dots = jnp.einsum('bqhd,bsd->bqhs', iqb.astype(jnp.float32), ik32) * IDX_DIM ** -0.5
        score = jnp.einsum('bqh,bqhs->bqs', iwb, jax.nn.relu(dots))
        visible = key_chunk[:, None, :] <= qchunk[:, :, None]
        score = jnp.where(visible, score, NEG_INF)
        _, sel = lax.top_k(score, top_k)
        k_sel = gather(k, sel)
        v_sel = gather(v, sel)
        pos_sel = gather(pos, sel)
        valid = (pos_sel // CHUNK) <= qchunk[:, :, None]
        dist = jnp.abs(pb[:, :, None] - pos_sel).astype(jnp.float32)
        logits = jnp.einsum('bqhd,bqkhd->bhqk', qb, k_sel,
                            preferred_element_type=jnp.float32) * A_HEAD_DIM ** -0.5
        logits = logits - slopes[None, :, None, None] * dist[:, None]
        p = jax.nn.softmax(jnp.where(valid[:, None], logits, NEG_INF), axis=-1)
        return jnp.einsum('bhqk,bqkhd->bqhd', p.astype(v.dtype), v_sel)

    out = lax.map(block, (to_query_blocks(q, SPARSE_Q_BLOCK), to_query_blocks(iq, SPARSE_Q_BLOCK),
                          to_query_blocks(iw32, SPARSE_Q_BLOCK), to_query_blocks(pos, SPARSE_Q_BLOCK)))
    return from_query_blocks(out)


def causal_depthwise_conv(x, w):
    return lax.conv_general_dilated(x, w[:, None, :].astype(x.dtype), window_strides=(1,),
                                    padding=[(CONV_K - 1, 0)],
                                    dimension_numbers=('NWC', 'WIO', 'NWC'),
                                    feature_group_count=x.shape[-1])


def gated_delta_rule(q, k, v, g, beta):
    bsz, seq, nh, dk = q.shape
    n = seq // CHUNK

    def to_chunks(t):
        t = t.reshape(bsz, n, CHUNK, nh, *t.shape[3:])
        return jnp.moveaxis(t, (1, 3), (0, 2))

    qc = to_chunks(q * dk ** -0.5)
    kc, vc, bc = to_chunks(k), to_chunks(v), to_chunks(beta)
    gc = jnp.cumsum(to_chunks(g), axis=-1)
    incl = jnp.tril(jnp.ones((CHUNK, CHUNK), dtype=bool))
    strict = jnp.tril(jnp.ones((CHUNK, CHUNK), dtype=bool), -1)
    decay = jnp.exp(jnp.where(incl, gc[..., :, None] - gc[..., None, :], -jnp.inf))
    kb = kc * bc[..., None]
    vb = vc * bc[..., None]
    m = jnp.where(strict, jnp.einsum('nbhid,nbhjd->nbhij', kb, kc) * decay, 0.0)
    eye = jnp.eye(CHUNK, dtype=jnp.float32)
    t_inv = lax.linalg.triangular_solve(eye + m, jnp.broadcast_to(eye, m.shape), left_side=True,
                                        lower=True, unit_diagonal=True)
    u = t_inv @ vb
    w = t_inv @ (kb * jnp.exp(gc)[..., None])
    attn_intra = jnp.where(incl, jnp.einsum('nbhid,nbhjd->nbhij', qc, kc) * decay, 0.0)

    def step(state, xs):
        q_i, k_i, u_i, w_i, g_i, a_i = xs
        v_new = u_i - w_i @ state
        o = (q_i * jnp.exp(g_i)[..., None]) @ state + a_i @ v_new
        g_last = g_i[..., -1]
        k_dec = k_i * jnp.exp(g_last[..., None] - g_i)[..., None]
        state = state * jnp.exp(g_last)[..., None, None] + jnp.einsum('bhcd,bhce->bhde', k_dec, v_new)
        return state, o

    s0 = jnp.zeros((bsz, nh, dk, v.shape[-1]), jnp.float32)
    _, o = lax.scan(step, s0, (qc, kc, u, w, gc, attn_intra))
    return jnp.moveaxis(o, (0, 2), (1, 3)).reshape(bsz, seq, nh, v.shape[-1])


def gdn_mixer(qkv, a, b, z, conv_w, a_log, dt_bias, norm_g):
    bsz, seq, _ = qkv.shape
    qkv = jax.nn.silu(causal_depthwise_conv(qkv, conv_w))
    q, k, v = jnp.split(qkv.astype(jnp.float32), 3, axis=-1)
    q = l2_normalize(q.reshape(bsz, seq, B_HEADS, B_HEAD_DIM))
    k = l2_normalize(k.reshape(bsz, seq, B_HEADS, B_HEAD_DIM))
    v = v.reshape(bsz, seq, B_HEADS, B_HEAD_DIM)
    g = -jnp.exp(a_log.astype(jnp.float32)) * jax.nn.softplus(a.astype(jnp.float32) + dt_bias.astype(jnp.float32))
    beta = jax.nn.sigmoid(b.astype(jnp.float32))
    o = gated_delta_rule(q, k, v, g, beta).astype(z.dtype)
    o = rms_norm(o, norm_g) * jax.nn.silu(z.reshape(bsz, seq, B_HEADS, B_HEAD_DIM))
    return o.reshape(bsz, seq, B_WIDTH)


def hybrid_mixer(u, pos, w_in, w_out, conv_w, a_log, dt_bias, norm_g):
    bsz, seq, _ = u.shape
    a_q, a_k, a_v, i_q, i_k, i_w, b_qkv, b_a, b_b, b_z = jnp.split(u @ w_in, EVEN_SPLITS, axis=-1)
    heads = lambda t, nh: t.reshape(bsz, seq, nh, -1)
    out_a = dsa_attention(heads(a_q, A_HEADS), heads(a_k, A_HEADS), heads(a_v, A_HEADS),
                          heads(i_q, IDX_HEADS), i_k, i_w, pos)
    out_b = gdn_mixer(b_qkv, b_a, b_b, b_z, conv_w, a_log, dt_bias, norm_g)
    return jnp.concatenate([out_a.astype(u.dtype), out_b], axis=-1) @ w_out


def chunk_causal_attention(q, k, v, pos, scale):
    key_chunk = pos // CHUNK

    def block(args):
        qb, pb = args
        logits = jnp.einsum('bqhd,bshd->bhqs', qb, k, preferred_element_type=jnp.float32) * scale
        visible = key_chunk[:, None, None, :] <= (pb // CHUNK)[:, None, :, None]
        p = jax.nn.softmax(jnp.where(visible, logits, NEG_INF), axis=-1)
        return jnp.einsum('bhqs,bshd->bqhd', p.astype(v.dtype), v)

    out = lax.map(block, (to_query_blocks(q, DENSE_Q_BLOCK), to_query_blocks(pos, DENSE_Q_BLOCK)))
    return from_query_blocks(out)


def mla_mixer(u, pos, w_in, q_norm_g, w_q_up, kv_norm_g, w_kv_up, w_out):
    bsz, seq, _ = u.shape
    c_q, c_kv, k_rope = jnp.split(u @ w_in, [C_Q_RANK, C_Q_RANK + C_KV_RANK], axis=-1)
    q = (rms_norm(c_q, q_norm_g) @ w_q_up).reshape(bsz, seq, C_HEADS, C_NOPE + C_ROPE)
    kv = (rms_norm(c_kv, kv_norm_g) @ w_kv_up).reshape(bsz, seq, C_HEADS, C_NOPE + C_V)
    q = jnp.concatenate([q[..., :C_NOPE], rope(q[..., C_NOPE:], pos)], axis=-1)
    k_rope = jnp.broadcast_to(rope(k_rope[:, :, None, :], pos), (bsz, seq, C_HEADS, C_ROPE))
    k = jnp.concatenate([kv[..., :C_NOPE], k_rope], axis=-1)
    v = kv[..., C_NOPE:]
    out = chunk_causal_attention(q, k, v, pos, (C_NOPE + C_ROPE) ** -0.5)
    return out @ w_out


def adaln_sublayer(x, m, f, ln_g, ln_b):
    shift, scale, gate = m[:, None, 0], m[:, None, 1], m[:, None, 2]
    y = f(x * (1 + scale) + shift)
    return layer_norm(DEEPNORM_ALPHA * x + (1 + gate) * y, ln_g, ln_b)


def setup_inputs(seed: int = 0) -> dict:
    key = jax.random.key(seed)
    ks = iter(jax.random.split(key, 32))

    def nrm(shape, fan_in, s=1.0):
        return jax.random.normal(next(ks), shape, jnp.float32) * (s * fan_in ** -0.5)

    def gain(shape):
        return 1.0 + 0.02 * jax.random.normal(next(ks), shape, jnp.float32)

    def small(shape, s=0.02):
        return s * jax.random.normal(next(ks), shape, jnp.float32)

    x = jax.random.normal(next(ks), (BATCH, SEQ, D_MODEL), jnp.float32)
    c = jax.random.normal(next(ks), (BATCH, D_MODEL), jnp.float32)
    offsets = jax.random.randint(next(ks), (BATCH, 1), 0, 1024) * CHUNK
    positions = (offsets + jnp.arange(SEQ)[None, :]).astype(jnp.int32)
    dt = jnp.exp(jax.random.uniform(next(ks), (N_EVEN, B_HEADS), jnp.float32,
                                    math.log(1e-3), math.log(1e-1)))
    return {
        "x": x,
        "c": c,
        "positions": positions,
        "mod_w": nrm((DEPTH, D_MODEL, N_SUB * 3 * D_MODEL), D_MODEL, 0.1),
        "mod_b": small((DEPTH, N_SUB * 3 * D_MODEL), 0.01),
        "ln_g": gain((DEPTH, N_SUB, D_MODEL)),
        "ln_b": small((DEPTH, N_SUB, D_MODEL)),
        "ffn_w_gate": nrm((DEPTH, 2, D_MODEL, D_FF), D_MODEL),
        "ffn_w_up": nrm((DEPTH, 2, D_MODEL, D_FF), D_MODEL),
        "ffn_w_down": nrm((DEPTH, 2, D_FF, D_MODEL), D_FF, DEEPNORM_BETA),
        "hyb_w_in": nrm((N_EVEN, D_MODEL, EVEN_IN), D_MODEL),
        "hyb_w_out": nrm((N_EVEN, EVEN_MIX, D_MODEL), EVEN_MIX, DEEPNORM_BETA),
        "gdn_conv_w": nrm((N_EVEN, CONV_K, 3 * B_WIDTH), CONV_K),
        "gdn_a_log": jnp.log(jax.random.uniform(next(ks), (N_EVEN, B_HEADS), jnp.float32, 1.0, 16.0)),
        "gdn_dt_bias": dt + jnp.log(-jnp.expm1(-dt)),
        "gdn_norm_g": gain((N_EVEN, B_HEAD_DIM)),
        "mla_w_in": nrm((N_ODD, D_MODEL, C_IN), D_MODEL),
        "mla_q_norm_g": gain((N_ODD, C_Q_RANK)),
        "mla_w_q_up": nrm((N_ODD, C_Q_RANK, C_HEADS * (C_NOPE + C_ROPE)), C_Q_RANK),
        "mla_kv_norm_g": gain((N_ODD, C_KV_RANK)),
        "mla_w_kv_up": nrm((N_ODD, C_KV_RANK, C_HEADS * (C_NOPE + C_V)), C_KV_RANK),
        "mla_w_out": nrm((N_ODD, C_MIX, D_MODEL), C_MIX, DEEPNORM_BETA),
    }


def reference(x, c, positions, mod_w, mod_b, ln_g, ln_b, ffn_w_gate, ffn_w_up, ffn_w_down,
              hyb_w_in, hyb_w_out, gdn_conv_w, gdn_a_log, gdn_dt_bias, gdn_norm_g,
              mla_w_in, mla_q_norm_g, mla_w_q_up, mla_kv_norm_g, mla_w_kv_up, mla_w_out):
    bsz = x.shape[0]
    cs = jax.nn.silu(c)
    for layer in range(DEPTH):
        mod = (cs @ mod_w[layer] + mod_b[layer]).reshape(bsz, N_SUB, 3, D_MODEL)
        x = adaln_sublayer(x, mod[:, 0], lambda u: 0.5 * swiglu(u, ffn_w_gate[layer, 0], ffn_w_up[layer, 0],
                                                               ffn_w_down[layer, 0]),
                           ln_g[layer, 0], ln_b[layer, 0])
        if layer % 2 == 0:
            e = layer // 2
            x = adaln_sublayer(x, mod[:, 1], lambda u: hybrid_mixer(u, positions, hyb_w_in[e], hyb_w_out[e],
                                                                    gdn_conv_w[e], gdn_a_log[e],
                                                                    gdn_dt_bias[e], gdn_norm_g[e]),
                               ln_g[layer, 1], ln_b[layer, 1])
        else:
            o = layer // 2
            x = adaln_sublayer(x, mod[:, 1], lambda u: mla_mixer(u, positions, mla_w_in[o], mla_q_norm_g[o],
                                                                 mla_w_q_up[o], mla_kv_norm_g[o],
                                                                 mla_w_kv_up[o], mla_w_out[o]),
                               ln_g[layer, 1], ln_b[layer, 1])
        x = adaln_sublayer(x, mod[:, 2], lambda u: 0.5 * swiglu(u, ffn_w_gate[layer, 1], ffn_w_up[layer, 1],
                                                               ffn_w_down[layer, 1]),
                           ln_g[layer, 2], ln_b[layer, 2])
    return x
```

```python
import numpy as np
from contextlib import ExitStack
import concourse.bass as bass
import concourse.mybir as mybir
from concourse.bass_utils import run_bass_kernel_spmd

F32 = mybir.dt.float32
BF16 = mybir.dt.bfloat16
I32 = mybir.dt.int32
AF = mybir.ActivationFunctionType
ALU = mybir.AluOpType
AX = mybir.AxisListType

D = 1024
S = 2048
DFF = 2816
NT = S // 128
DEPTH = 4
ALPHA = float((2.0 * DEPTH) ** 0.25)
LN_EPS = 1e-5
RMS_EPS = 1e-6
NB = 2

FULL_PLAN = [(l, s) for l in range(DEPTH) for s in range(3)]


class Sched:
    NDMA = 32

    def __init__(self, nc, es):
        self.nc = nc
        self.es = es
        self.eng = {'pe': nc.tensor, 'act': nc.scalar, 'dve': nc.vector, 'pool': nc.gpsimd, 'sp': nc.sync}
        self.sem = {}
        self.val = {}
        self.waited = {e: {} for e in self.eng}
        for e in self.eng:
            self._mksem(e)
        self.dsem = [f'd{i}' for i in range(self.NDMA)]
        for d in self.dsem:
            self._mksem(d)
        self.qpool = {'sp': self.dsem[:12], 'pool': self.dsem[12:]}
        self.rr = {'sp': 0, 'pool': 0}
        self.lastw = {}
        self.readers = {}
        self.n_ins = 0

    def _mksem(self, name):
        self.sem[name] = self.es.enter_context(self.nc.semaphore('s_' + name))
        self.val[name] = 0

    def _deps(self, reads, writes):
        deps = {}

        def add(evs):
            for s, v in evs.items():
                if deps.get(s, 0) < v:
                    deps[s] = v
        for k in reads:
            add(self.lastw.get(k, {}))
        for k in writes:
            add(self.lastw.get(k, {}))
            add(self.readers.get(k, {}))
        return deps

    def _wait(self, e, deps):
        for s, v in deps.items():
            if s == e and e == 'pe':
                continue
            if self.waited[e].get(s, 0) >= v:
                continue
            self.eng[e].wait_ge(self.sem[s], v)
            self.waited[e][s] = v
            self.n_ins += 1

    def op(self, e, fn, reads=(), writes=()):
        self._wait(e, self._deps(reads, writes))
        ins = fn(self.eng[e])
        self.val[e] += 1
        v = self.val[e]
        ins.then_inc(self.sem[e], 1)
        self.n_ins += 1
        for k in reads:
            r = self.readers.setdefault(k, {})
            if r.get(e, 0) < v:
                r[e] = v
        for k in writes:
            self.lastw[k] = {e: v}
            self.readers[k] = {}

    def dma(self, q, out, in_, reads=(), writes=(), chan=None, **kw):
        pool_ = self.qpool[q]
        s = pool_[self.rr[q] % len(pool_)]
        self.rr[q] += 1
        deps = self._deps(reads, writes)
        if self.val[s] > 0:
            deps[s] = self.val[s]
        self._wait(q, deps)
        ins = self.eng[q].dma_start(out=out, in_=in_, **kw)
        self.val[s] += 16
        v = self.val[s]
        ins.then_inc(self.sem[s], 16)
        self.n_ins += 1
        for k in reads:
            self.readers.setdefault(k, {})[s] = v
        for k in writes:
            prev = self.lastw.get(k, {})
            keep = {a: b for a, b in prev.items() if a in self.val and a.startswith('d') and a[1:].isdigit()}
            keep[s] = v
            self.lastw[k] = keep
            self.readers[k] = {}

    def barrier(self):
        for e in self.eng:
            for s, v in self.val.items():
                if (s == e and e == 'pe') or v == 0:
                    continue
                if self.waited[e].get(s, 0) >= v:
                    continue
                self.eng[e].wait_ge(self.sem[s], v)
                self.waited[e][s] = v
        self.lastw.clear()
        self.readers.clear()

    def wait_all_dma(self, e):
        for s in self.dsem:
            v = self.val[s]
            if v > 0 and self.waited[e].get(s, 0) < v:
                self.eng[e].wait_ge(self.sem[s], v)
                self.waited[e][s] = v


class Builder:
    def __init__(self, plan):
        self.plan = plan
        self.nc = bass.Bass("TRN2", target_bir_lowering=False)
        nc = self.nc
        di = lambda n, sh, dt=F32: nc.dram_tensor(n, sh, dt, kind="ExternalInput").ap()
        self.x = di("x", [NB, S, D])
        self.c = di("c", [NB, D])
        self.pos = di("positions", [NB, S], I32)
        self.win = {}
        self.wsrc = {}
        self.out = nc.dram_tensor("out", [NB, S, D], F32, kind="ExternalOutput").ap()
        self.dbg = nc.dram_tensor("dbg", [128, 4096], F32, kind="ExternalOutput").ap() if DEBUG else None
        self.dumped = set()

    def W(self, key, idx, shape, cols=None):
        name = key + "_" + "_".join(str(i) for i in idx) + ("" if cols is None else f"_c{cols[0]}")
        if name not in self.win:
            self.win[name] = self.nc.dram_tensor(name, list(shape), F32, kind="ExternalInput").ap()
            self.wsrc[name] = (key, tuple(idx), cols)
        return self.win[name]

    def sb(self, es, name, shape, dt):
        self._uid = getattr(self, '_uid', 0) + 1
        return es.enter_context(self.nc.sbuf_tensor(f"{name}_{self._uid}", shape, dt))

    def dump(self, tag, ap, key, col0, shape=None):
        if not DEBUG or tag in self.dumped:
            return
        self.dumped.add(tag)
        n = int(np.prod(ap.shape[1:]))
        dst = self.dbg[:, col0:col0 + n]
        if len(ap.shape) == 3:
            dst = dst.rearrange("p (a b) -> p a b", a=ap.shape[1])
        self.sc.dma('pool', dst, ap, reads=[key] if isinstance(key, str) else key, chan='dbg')

    def build(self):
        nc = self.nc
        with ExitStack() as es:
            self.sc = Sched(nc, es)
            sc = self.sc
            self.psbig = es.enter_context(nc.psum_tensor("psbig", [128, 2048], F32))
            self.ps = [self.psbig[:, i * 512:(i + 1) * 512] for i in range(4)] + \
                      [es.enter_context(nc.psum_tensor(f"ps{i}", [128, 512], F32)) for i in range(4, 8)]
            self.ident_f = self.sb(es, "ident_f", [128, 128], F32)
            self.ident_b = self.sb(es, "ident_b", [128, 128], BF16)
            self.iot = self.sb(es, "iot", [128, 128], I32)
            self.eps_ln = self.sb(es, "eps_ln", [128, 1], F32)
            sc.op('pool', lambda e: e.iota(self.iot[:], pattern=[[1, 128]], base=0, channel_multiplier=-1),
                  writes=['iot'])
            sc.op('dve', lambda e: e.tensor_scalar(out=self.ident_f[:], in0=self.iot[:], scalar1=0.0, scalar2=None,
                                                   op0=ALU.is_equal), reads=['iot'], writes=['ident_f'])
            sc.op('dve', lambda e: e.tensor_copy(out=self.ident_b[:], in_=self.ident_f[:]),
                  reads=['ident_f'], writes=['ident_b'])
            sc.op('dve', lambda e: e.memset(self.eps_ln[:], LN_EPS), writes=['eps_ln'])
            self.X = [self.sb(es, f"X{t}", [128, D], F32) for t in range(NT)]
            self.csrep = self.sb(es, "csrep", [128, 8, 128], BF16)
            self.cs_f = self.sb(es, "cs_f", [128, 8], F32)
            self.cs_b = self.sb(es, "cs_b", [128, 8], BF16)
            for b in range(NB):
                self.run_sequence(b)
            sc.wait_all_dma('sp')
        return nc

    def run_sequence(self, b):
        sc = self.sc
        nc = self.nc
        for t in range(NT):
            sc.dma('sp', self.X[t][:], self.x[b, t * 128:(t + 1) * 128, :], writes=[f'X{t}'], chan='xin')
        sc.dma('sp', self.cs_f[:], self.c[b].rearrange("(k p) -> p k", p=128), writes=['cs_f'], chan='misc',
               allow_slow_non_contiguous=True)
        sc.op('act', lambda e: e.activation(out=self.cs_b[:], in_=self.cs_f[:], func=AF.Silu),
              reads=['cs_f'], writes=['cs_b'])
        sc.op('dve', lambda e: e.tensor_copy(out=self.csrep[:], in_=self.cs_b[:].unsqueeze(2).to_broadcast([128, 8, 128])),
              reads=['cs_b'], writes=['csrep'])
        for (l, s) in self.plan:
            if s in (0, 2):
                self.ffn_sublayer(b, l, s)
            elif l % 2 == 1:
                self.mla_sublayer(b, l)
            else:
                self.hyb_sublayer(b, l)
        for t in range(NT):
            sc.dma('sp', self.out[b, t * 128:(t + 1) * 128, :], self.X[t][:], reads=[f'X{t}'], chan='out')

    def modulation(self, es, b, l, s, gate_mul, which=('SH', 'SC1', 'G1', 'LG', 'LB')):
        sc = self.sc
        mk = lambda n: self.sb(es, n, [128, D], F32) if n in which else None
        SH, SC1, G1, LG, LB = mk('SH'), mk('SC1'), mk('G1'), mk('LG'), mk('LB')
        with ExitStack() as es2:
            self._modulation(es2, b, l, s, gate_mul, which, SH, SC1, G1, LG, LB)
            sc.barrier()
        return SH, SC1, G1, LG, LB

    def _modulation(self, es, b, l, s, gate_mul, which, SH, SC1, G1, LG, LB):
        sc = self.sc
        mw = [self.sb(es, f"mw{i}", [128, 8, 512], BF16) for i in range(2)]
        mb = [self.sb(es, f"mb{i}", [128, 512], F32) for i in range(2)]
        if LG is not None:
            sc.dma('sp', LG[:], self.W('ln_g', (l, s), [1, D]).partition_broadcast(128), writes=['LG'], chan='misc')
        if LB is not None:
            sc.dma('sp', LB[:], self.W('ln_b', (l, s), [1, D]).partition_broadcast(128), writes=['LB'], chan='misc')
        dst = [SH, SC1, G1]
        names = ['SH', 'SC1', 'G1']
        i = 0
        for j in range(3):
            if dst[j] is None:
                continue
            for hf in range(2):
                c0 = j * D + hf * 512
                mwd = self.W('mod_w', (l,), [D, 3 * D], cols=(s * 3 * D, (s + 1) * 3 * D))
                mbd = self.W('mod_b', (l,), [1, 3 * D], cols=(s * 3 * D, (s + 1) * 3 * D))
                sl = i % 2
                sc.dma('pool', mw[sl][:], mwd[:, c0:c0 + 512].rearrange("(k p) n -> p k n", p=128),
                       writes=[f'mw{sl}'], chan=f'mw{sl}')
                sc.dma('sp', mb[sl][:], mbd[:, c0:c0 + 512].partition_broadcast(128),
                       writes=[f'mb{sl}'], chan=f'mb{sl}')
                pb = 6 + sl
                for k in range(8):
                    sc.op('pe', lambda e, k=k: e.matmul(self.ps[pb][:], lhsT=self.csrep[:, k, :], rhs=mw[sl][:, k, :],
                                                        start=(k == 0), stop=(k == 7)),
                          reads=['csrep', f'mw{sl}'], writes=[f'ps{pb}'])
                addc = 0.0 if j == 0 else 1.0
                sc.op('dve', lambda e: e.scalar_tensor_tensor(out=dst[j][:, hf * 512:(hf + 1) * 512], in0=self.ps[pb][:],
                                                              scalar=addc, in1=mb[sl][:], op0=ALU.add, op1=ALU.add),
                      reads=[f'ps{pb}', f'mb{sl}'], writes=[names[j]])
                i += 1
        if gate_mul != 1.0 and G1 is not None:
            sc.op('dve', lambda e: e.tensor_scalar(out=G1[:], in0=G1[:], scalar1=gate_mul, scalar2=None, op0=ALU.mult),
                  reads=['G1'], writes=['G1'])

    def layer_norm_tile(self, t, LG, LB, lnbuf):
        sc = self.sc
        X = self.X[t]
        st, mv, sd, rstd, nb = lnbuf
        k = f'X{t}'
        for hf in range(2):
            sc.op('dve', lambda e, hf=hf: e.bn_stats(out=st[:, hf, :], in_=X[:, hf * 512:(hf + 1) * 512]),
                  reads=[k], writes=['ln_st'])
        sc.op('dve', lambda e: e.bn_aggr(out=mv[:], in_=st[:].rearrange("p a b -> p (a b)")), reads=['ln_st'], writes=['ln_mv'])
        sc.op('act', lambda e: e.activation(out=sd[:], in_=mv[:, 1:2], func=AF.Sqrt, bias=self.eps_ln[:], scale=1.0),
              reads=['ln_mv', 'eps_ln'], writes=['ln_sd'])
        sc.op('dve', lambda e: e.reciprocal(out=rstd[:], in_=sd[:]), reads=['ln_sd'], writes=['ln_rstd'])
        sc.op('dve', lambda e: e.tensor_scalar(out=nb[:], in0=mv[:, 0:1], scalar1=rstd[:], scalar2=-1.0,
                                               op0=ALU.mult, op1=ALU.mult), reads=['ln_mv', 'ln_rstd'], writes=['ln_nb'])
        sc.op('act', lambda e: e.activation(out=X[:], in_=X[:], func=AF.Identity, bias=nb[:], scale=rstd[:]),
              reads=[k, 'ln_nb', 'ln_rstd'], writes=[k])
        sc.op('dve', lambda e: e.tensor_tensor(out=X[:], in0=X[:], in1=LG[:], op=ALU.mult), reads=[k, 'LG'], writes=[k])
        sc.op(POOL_ELEM, lambda e: e.tensor_tensor(out=X[:], in0=X[:], in1=LB[:], op=ALU.add), reads=[k, 'LB'], writes=[k])

    def ln_bufs(self, es):
        return (self.sb(es, "ln_st", [128, 2, 6], F32), self.sb(es, "ln_mv", [128, 2], F32),
                self.sb(es, "ln_sd", [128, 1], F32), self.sb(es, "ln_rstd", [128, 1], F32),
                self.sb(es, "ln_nb", [128, 1], F32))

    def mod_transpose_tile(self, t, SC1, SH, utmp, ub, dstT, dst_key, col0, pbank):
        sc = self.sc
        sl = t % 2
        sc.op('dve', lambda e: e.tensor_tensor(out=utmp[sl][:], in0=self.X[t][:], in1=SC1[:], op=ALU.mult),
              reads=[f'X{t}', 'SC1'], writes=[f'utmp{sl}'])
        sc.op(POOL_ELEM, lambda e: e.tensor_tensor(out=ub[sl][:], in0=utmp[sl][:], in1=SH[:], op=ALU.add),
              reads=[f'utmp{sl}', 'SH'], writes=[f'ub{sl}'])
        pv = self.ps[pbank][:].bitcast(BF16)
        for k in range(8):
            sc.op('pe', lambda e, k=k: e.transpose(out=pv[:, k * 128:(k + 1) * 128], in_=ub[sl][:, k * 128:(k + 1) * 128],
                                                   identity=self.ident_b[:]),
                  reads=[f'ub{sl}', 'ident_b'], writes=[f'ps{pbank}'])
        sc.op('act', lambda e: e.activation(out=dstT[:, :, col0:col0 + 128], in_=pv.rearrange("p (k n) -> p k n", k=8),
                                            func=AF.Copy),
              reads=[f'ps{pbank}'], writes=[dst_key])

    def ffn_sublayer(self, b, l, s):
        sc = self.sc
        fi = 0 if s == 0 else 1
        with ExitStack() as es:
            SH, SC1, G1, LG, LB = self.modulation(es, b, l, s, 0.5)
            lnbuf = self.ln_bufs(es)
            utmp = [self.sb(es, f"utmp{i}", [128, D], F32) for i in range(2)]
            ub = [self.sb(es, f"ub{i}", [128, D], BF16) for i in range(2)]
            uT = self.sb(es, "uT", [128, 8, 1024], BF16)
            hT = self.sb(es, "hT", [128, 12, 1024], BF16)
            wgb = [self.sb(es, f"wgb{i}", [128, 8, 256], BF16) for i in range(2)]
            wub = [self.sb(es, f"wub{i}", [128, 8, 256], BF16) for i in range(2)]
            wdb = [self.sb(es, f"wdb{i}", [128, 2, D], BF16) for i in range(6)]
            sg = [self.sb(es, f"sg{i}", [128, 512], F32) for i in range(2)]
            t1 = [self.sb(es, f"t1_{i}", [128, 512], F32) for i in range(2)]
            cnt = 0
            self.dump('SC1', SC1[:], 'SC1', 0)
            self.dump('G1', G1[:], 'G1', 1024)
            for grp in range(2):
                for tl in range(8):
                    t = grp * 8 + tl
                    self.mod_transpose_tile(t, SC1, SH, utmp, ub, uT, f'uT{tl}', tl * 128, tl % 2)
                self.dump('uT', uT[:, :, 0:128], 'uT0', 2048)
                for ph in range(2):
                    blocks = list(range(0, 6)) if ph == 0 else list(range(6, 11))
                    for bi, blk in enumerate(blocks):
                        sl = cnt % 2
                        cnt += 1
                        c0 = blk * 256
                        sc.dma('pool', wgb[sl][:], self.W('ffn_w_gate', (l, fi), [D, DFF])[:, c0:c0 + 256].rearrange("(k p) n -> p k n", p=128),
                               writes=[f'wgb{sl}'], chan=f'wgb{sl}')
                        sc.dma('pool', wub[sl][:], self.W('ffn_w_up', (l, fi), [D, DFF])[:, c0:c0 + 256].rearrange("(k p) n -> p k n", p=128),
                               writes=[f'wub{sl}'], chan=f'wub{sl}')
                        sc.dma('pool', wdb[bi][:], self.W('ffn_w_down', (l, fi), [DFF, D])[c0:c0 + 256, :].rearrange("(k p) n -> p k n", p=128),
                               writes=[f'wdb{bi}'], chan=f'wdb{bi}')
                        for cc in range(2):
                            fl = bi * 2 + cc
                            for hf in range(2):
                                pg = 2 + hf
                                pu = 4 + hf
                                for k in range(8):
                                    sc.op('pe', lambda e, k=k: e.matmul(self.ps[pg][:], lhsT=wgb[sl][:, k, cc * 128:(cc + 1) * 128],
                                                                        rhs=uT[:, k, hf * 512:(hf + 1) * 512],
                                                                        start=(k == 0), stop=(k == 7)),
                                          reads=[f'wgb{sl}'] + [f'uT{i}' for i in range(hf * 4, hf * 4 + 4)], writes=[f'ps{pg}'])
                                for k in range(8):
                                    sc.op('pe', lambda e, k=k: e.matmul(self.ps[pu][:], lhsT=wub[sl][:, k, cc * 128:(cc + 1) * 128],
                                                                        rhs=uT[:, k, hf * 512:(hf + 1) * 512],
                                                                        start=(k == 0), stop=(k == 7)),
                                          reads=[f'wub{sl}'] + [f'uT{i}' for i in range(hf * 4, hf * 4 + 4)], writes=[f'ps{pu}'])
                                sc.op('act', lambda e: e.activation(out=sg[hf][:], in_=self.ps[pg][:], func=AF.Silu),
                                      reads=[f'ps{pg}'], writes=[f'sg{hf}'])
                                sc.op('dve', lambda e: e.tensor_tensor(out=hT[:, fl, hf * 512:(hf + 1) * 512], in0=sg[hf][:],
                                                                       in1=self.ps[pu][:], op=ALU.mult),
                                      reads=[f'sg{hf}', f'ps{pu}'], writes=[f'hT{fl}_{hf}'])
                    nfl = len(blocks) * 2
                    self.dump('hT', hT[:, 0, 0:512], 'hT0_0', 3072)
                    for tl in range(8):
                        t = grp * 8 + tl
                        for hf in range(2):
                            py = 6 + hf
                            for fl in range(nfl):
                                sc.op('pe', lambda e, fl=fl: e.matmul(self.ps[py][:], lhsT=hT[:, fl, tl * 128:(tl + 1) * 128],
                                                                      rhs=wdb[fl // 2][:, fl % 2, hf * 512:(hf + 1) * 512],
                                                                      start=(fl == 0), stop=(fl == nfl - 1)),
                                      reads=[f'hT{fl}_{tl // 4}', f'wdb{fl // 2}'], writes=[f'ps{py}'])
                            Xs = self.X[t][:, hf * 512:(hf + 1) * 512]
                            sc.op('dve', lambda e: e.tensor_tensor(out=t1[hf][:], in0=self.ps[py][:], in1=G1[:, hf * 512:(hf + 1) * 512],
                                                                   op=ALU.mult),
                                  reads=[f'ps{py}', 'G1'], writes=[f't1_{hf}'])
                            if hf == 0:
                                self.dump('t1', t1[0][:], 't1_0', 3584)
                            if ph == 0:
                                sc.op('dve', lambda e: e.scalar_tensor_tensor(out=Xs, in0=Xs, scalar=ALPHA, in1=t1[hf][:],
                                                                              op0=ALU.mult, op1=ALU.add),
                                      reads=[f'X{t}', f't1_{hf}'], writes=[f'X{t}'])
                            else:
                                sc.op(POOL_ELEM, lambda e: e.tensor_tensor(out=Xs, in0=Xs, in1=t1[hf][:], op=ALU.add),
                                      reads=[f'X{t}', f't1_{hf}'], writes=[f'X{t}'])
                        if ph == 1:
                            self.layer_norm_tile(t, LG, LB, lnbuf)
            sc.barrier()


    def mixer_tail(self, b, l, O, okey, wout_d):
        sc = self.sc
        with ExitStack() as es:
            _, _, G1, LG, LB = self.modulation(es, b, l, 1, 1.0, which=('G1', 'LG', 'LB'))
            lnbuf = self.ln_bufs(es)
            wo = self.sb(es, "wo", [128, 8, D], BF16)
            OT = [self.sb(es, f"OT{i}", [128, 8, 128], BF16) for i in range(2)]
            t1 = [self.sb(es, f"t1_{i}", [128, 512], F32) for i in range(2)]
            for hf in range(2):
                sc.dma('pool', wo[:, :, hf * 512:(hf + 1) * 512],
                       wout_d[:, hf * 512:(hf + 1) * 512].rearrange("(k p) n -> p k n", p=128), writes=[f'wo{hf}'], chan=f'wo{hf}')
            for t in range(NT):
                sl = t % 2
                pb = 4 + sl
                pv = self.ps[pb][:].bitcast(BF16)
                for k in range(8):
                    sc.op('pe', lambda e, k=k: e.transpose(out=pv[:, k * 128:(k + 1) * 128], in_=O[:, t, k * 128:(k + 1) * 128],
                                                           identity=self.ident_b[:]),
                          reads=[okey(t), 'ident_b'], writes=[f'ps{pb}'])
                sc.op('act', lambda e: e.activation(out=OT[sl][:], in_=pv.rearrange("p (k n) -> p k n", k=8), func=AF.Copy),
                      reads=[f'ps{pb}'], writes=[f'OT{sl}'])
                for hf in range(2):
                    py = 6 + hf
                    for k in range(8):
                        sc.op('pe', lambda e, k=k: e.matmul(self.ps[py][:], lhsT=OT[sl][:, k, :], rhs=wo[:, k, hf * 512:(hf + 1) * 512],
                                                            start=(k == 0), stop=(k == 7)),
                              reads=[f'OT{sl}', f'wo{hf}'], writes=[f'ps{py}'])
                    Xs = self.X[t][:, hf * 512:(hf + 1) * 512]
                    sc.op('dve', lambda e: e.tensor_tensor(out=t1[hf][:], in0=self.ps[py][:], in1=G1[:, hf * 512:(hf + 1) * 512], op=ALU.mult),
                          reads=[f'ps{py}', 'G1'], writes=[f't1_{hf}'])
                    sc.op('dve', lambda e: e.scalar_tensor_tensor(out=Xs, in0=Xs, scalar=ALPHA, in1=t1[hf][:], op0=ALU.mult, op1=ALU.add),
                          reads=[f'X{t}', f't1_{hf}'], writes=[f'X{t}'])
                self.layer_norm_tile(t, LG, LB, lnbuf)
            sc.barrier()

    def attn_tile(self, qT_ap, kT, kT_key, V, V_key, KV, scale, out_ap, out_key, bufs, maskblk, q_keys, bias=None, bias_key=None,
                  v_of=None, dv=None):
        sc = self.sc
        P, PT, st = bufs
        if v_of is None:
            dv = V.shape[2]
            v_of = lambda blk: (V[:, blk, :], V_key)
        Sp = self.psbig
        PSK = ['psS', 'ps0', 'ps1', 'ps2', 'ps3']
        nkc = (KV + 511) // 512
        for kc in range(nkc):
            c0 = kc * 512
            c1 = min(KV, c0 + 512)
            last = (kc == nkc - 1)
            sc.op('pe', lambda e: e.matmul(Sp[:, c0:c1], lhsT=qT_ap, rhs=kT[:, c0:c1], start=True,
                                           stop=(not last) or (maskblk is None)),
                  reads=list(q_keys) + [kT_key], writes=PSK)
        if maskblk is not None:
            sc.op('pe', lambda e: e.matmul(Sp[:, KV - 128:KV], lhsT=self.ident_b[:], rhs=maskblk[:], start=False, stop=True),
                  reads=['ident_b', 'maskblk'], writes=PSK)
        src = Sp[:, 0:KV]
        skeys = PSK
        if bias is not None:
            sc.op('dve', lambda e: e.tensor_tensor(out=bias[:, 0:KV], in0=Sp[:, 0:KV], in1=bias[:, 0:KV], op=ALU.add),
                  reads=PSK + [bias_key], writes=[bias_key])
            src = bias[:, 0:KV]
            skeys = [bias_key]
        sc.op('dve', lambda e: e.tensor_reduce(out=st[:, 0:1], in_=src, axis=AX.X, op=ALU.max), reads=skeys, writes=['at_m'])
        sc.op('dve', lambda e: e.tensor_scalar(out=st[:, 1:2], in0=st[:, 0:1], scalar1=-scale, scalar2=None, op0=ALU.mult),
              reads=['at_m'], writes=['at_nm'])
        sc.op('act', lambda e: e.activation(out=P[:, 0:KV], in_=src, func=AF.Exp, bias=st[:, 1:2], scale=scale, accum_out=st[:, 2:3]),
              reads=skeys + ['at_nm'], writes=['at_P', 'at_sum'])
        sc.op('dve', lambda e: e.reciprocal(out=st[:, 3:4], in_=st[:, 2:3]), reads=['at_sum'], writes=['at_rinv'])
        nblk = KV // 128
        for g in range((nblk + 7) // 8):
            pb = 4 + (g % 2)
            pv = self.ps[pb][:].bitcast(BF16)
            nb_ = min(8, nblk - g * 8)
            for j in range(nb_):
                blk = g * 8 + j
                sc.op('pe', lambda e, j=j, blk=blk: e.transpose(out=pv[:, j * 128:(j + 1) * 128], in_=P[:, blk * 128:(blk + 1) * 128],
                                                                identity=self.ident_b[:]),
                      reads=['at_P', 'ident_b'], writes=[f'ps{pb}'])
            eng = 'act' if g % 2 == 0 else 'dve'
            if eng == 'act':
                sc.op('act', lambda e: e.activation(out=PT[:, g * 8:g * 8 + nb_, :], in_=pv[:, 0:nb_ * 128].rearrange("p (k n) -> p k n", n=128),
                                                    func=AF.Copy), reads=[f'ps{pb}'], writes=[f'at_PT{g}'])
            else:
                sc.op('dve', lambda e: e.tensor_copy(out=PT[:, g * 8:g * 8 + nb_, :], in_=pv[:, 0:nb_ * 128].rearrange("p (k n) -> p k n", n=128)),
                      reads=[f'ps{pb}'], writes=[f'at_PT{g}'])
        po = self.ps[6]
        for blk in range(nblk):
            vap, vkey = v_of(blk)
            sc.op('pe', lambda e, blk=blk: e.matmul(po[:, 0:dv], lhsT=PT[:, blk, :], rhs=vap, start=(blk == 0), stop=(blk == nblk - 1)),
                  reads=[f'at_PT{blk // 8}', vkey], writes=['ps6'])
        sc.op('act', lambda e: e.activation(out=out_ap, in_=po[:, 0:dv], func=AF.Copy, scale=st[:, 3:4]),
              reads=['ps6', 'at_rinv'], writes=[out_key])

    def make_maskblk(self, es):
        sc = self.sc
        mi = self.sb(es, "mask_i", [128, 128], I32)
        mf = self.sb(es, "mask_f", [128, 128], F32)
        mb = self.sb(es, "maskblk", [128, 128], BF16)
        sc.op('pool', lambda e: e.iota(mi[:], pattern=[[1, 128]], base=0, channel_multiplier=0), writes=['mask_i'])
        sc.op('dve', lambda e: e.tensor_scalar(out=mf[:], in0=mi[:], scalar1=64.0, scalar2=None, op0=ALU.is_ge),
              reads=['mask_i'], writes=['mask_f'])
        sc.op('dve', lambda e: e.memset(mf[64:128, :], 0.0), reads=['mask_f'], writes=['mask_f'])
        sc.op('dve', lambda e: e.tensor_scalar(out=mb[:], in0=mf[:], scalar1=-1e30, scalar2=None, op0=ALU.mult),
              reads=['mask_f'], writes=['maskblk'])
        return mb

    def rope_tables(self, es, b):
        sc = self.sc
        COS = self.sb(es, "COS", [32, S], F32)
        SINS = self.sb(es, "SINS", [32, S], F32)
        with ExitStack() as es2:
            pi_ = self.sb(es2, "rp_pi", [32, S], I32)
            ang = self.sb(es2, "rp_ang", [32, S], F32)
            kf = self.sb(es2, "rp_kf", [32, S], F32)
            ki = self.sb(es2, "rp_ki", [32, S], I32)
            r = self.sb(es2, "rp_r", [32, S], F32)
            m = self.sb(es2, "rp_m", [32, S], F32)
            fi = self.sb(es2, "rp_fi", [32, 1], I32)
            ff = self.sb(es2, "rp_ff", [32, 4], F32)
            sc.dma('sp', pi_[:], self.pos[b:b + 1, :].partition_broadcast(32), writes=['rp_pi'], chan='misc')
            sc.op('pool', lambda e: e.iota(fi[:], pattern=[[0, 1]], base=0, channel_multiplier=1), writes=['rp_fi'])
            sc.op('dve', lambda e: e.tensor_copy(out=ff[:, 0:1], in_=fi[:]), reads=['rp_fi'], writes=['rp_ff'])
            sc.op('dve', lambda e: e.tensor_scalar(out=ff[:, 1:2], in0=ff[:, 0:1], scalar1=16.0, scalar2=-16.0, op0=ALU.is_ge, op1=ALU.mult),
                  reads=['rp_ff'], writes=['rp_ff'])
            sc.op('dve', lambda e: e.tensor_tensor(out=ff[:, 2:3], in0=ff[:, 0:1], in1=ff[:, 1:2], op=ALU.add), reads=['rp_ff'], writes=['rp_ff'])
            sc.op('act', lambda e: e.activation(out=ff[:, 3:4], in_=ff[:, 2:3], func=AF.Exp, scale=-float(np.log(10000.0)) / 16.0),
                  reads=['rp_ff'], writes=['rp_ff'])
            sc.op('dve', lambda e: e.tensor_copy(out=ang[:], in_=pi_[:]), reads=['rp_pi'], writes=['rp_ang'])
            sc.op('dve', lambda e: e.tensor_scalar(out=ang[:], in0=ang[:], scalar1=ff[:, 3:4], scalar2=None, op0=ALU.mult),
                  reads=['rp_ang', 'rp_ff'], writes=['rp_ang'])
            TWO_PI = float(2 * np.pi)
            C1 = 6.28125
            C2 = float(2 * np.pi - 6.28125)
            sc.op('dve', lambda e: e.tensor_scalar(out=kf[:], in0=ang[:], scalar1=1.0 / TWO_PI, scalar2=None, op0=ALU.mult),
                  reads=['rp_ang'], writes=['rp_kf'])
            sc.op('dve', lambda e: e.tensor_copy(out=ki[:], in_=kf[:]), reads=['rp_kf'], writes=['rp_ki'])
            sc.op('dve', lambda e: e.tensor_copy(out=kf[:], in_=ki[:]), reads=['rp_ki'], writes=['rp_kf'])
            sc.op('dve', lambda e: e.scalar_tensor_tensor(out=r[:], in0=kf[:], scalar=-C1, in1=ang[:], op0=ALU.mult, op1=ALU.add),
                  reads=['rp_kf', 'rp_ang'], writes=['rp_r'])
            sc.op('dve', lambda e: e.scalar_tensor_tensor(out=r[:], in0=kf[:], scalar=-C2, in1=r[:], op0=ALU.mult, op1=ALU.add),
                  reads=['rp_kf', 'rp_r'], writes=['rp_r'])

            def wrap(buf, key):
                sc.op('dve', lambda e: e.tensor_scalar(out=m[:], in0=buf[:], scalar1=float(np.pi), scalar2=-TWO_PI, op0=ALU.is_gt, op1=ALU.mult),
                      reads=[key], writes=['rp_m'])
                sc.op('dve', lambda e: e.tensor_tensor(out=buf[:], in0=buf[:], in1=m[:], op=ALU.add), reads=[key, 'rp_m'], writes=[key])
                sc.op('dve', lambda e: e.tensor_scalar(out=m[:], in0=buf[:], scalar1=-float(np.pi), scalar2=TWO_PI, op0=ALU.is_lt, op1=ALU.mult),
                      reads=[key], writes=['rp_m'])
                sc.op('dve', lambda e: e.tensor_tensor(out=buf[:], in0=buf[:], in1=m[:], op=ALU.add), reads=[key, 'rp_m'], writes=[key])
            wrap(r, 'rp_r')
            sc.op('act', lambda e: e.activation(out=SINS[:], in_=r[:], func=AF.Sin), reads=['rp_r'], writes=['SINS'])
            sc.op('dve', lambda e: e.tensor_scalar(out=ff[:, 1:2], in0=ff[:, 0:1], scalar1=16.0, scalar2=2.0, op0=ALU.is_ge, op1=ALU.mult),
                  reads=['rp_ff'], writes=['rp_ff'])
            sc.op('dve', lambda e: e.tensor_scalar(out=ff[:, 1:2], in0=ff[:, 1:2], scalar1=-1.0, scalar2=None, op0=ALU.add),
                  reads=['rp_ff'], writes=['rp_ff'])
            sc.op('dve', lambda e: e.tensor_scalar(out=SINS[:], in0=SINS[:], scalar1=ff[:, 1:2], scalar2=None, op0=ALU.mult),
                  reads=['SINS', 'rp_ff'], writes=['SINS'])
            sc.op('dve', lambda e: e.tensor_scalar(out=r[:], in0=r[:], scalar1=float(np.pi / 2), scalar2=None, op0=ALU.add),
                  reads=['rp_r'], writes=['rp_r'])
            wrap(r, 'rp_r')
            sc.op('act', lambda e: e.activation(out=COS[:], in_=r[:], func=AF.Sin), reads=['rp_r'], writes=['COS'])
            sc.barrier()
        return COS, SINS

    def mla_sublayer(self, b, l):
        sc = self.sc
        o = l // 2
        HD = 96
        scale = float(HD ** -0.5)
        with ExitStack() as esO:
            O = self.sb(esO, "O", [128, NT, D], BF16)
            with ExitStack() as es:
                cnT = self.sb(es, "cnT", [128, 5, S], BF16)
                krT = self.sb(es, "krT", [32, S], BF16)
                COS, SINS = self.rope_tables(es, b)
                with ExitStack() as esA:
                    SH, SC1, _, _, _ = self.modulation(esA, b, l, 1, 1.0, which=('SH', 'SC1'))
                    utmp = [self.sb(esA, f"utmp{i}", [128, D], F32) for i in range(2)]
                    ub = [self.sb(esA, f"ub{i}", [128, D], BF16) for i in range(2)]
                    uT4 = self.sb(esA, "uT4", [128, 8, 512], BF16)
                    win = self.sb(esA, "win", [128, 8, 672], BF16)
                    winsw = self.sb(esA, "winsw", [128, 8, 32], BF16)
                    GQ = self.sb(esA, "GQ", [128, 640], F32)
                    cn = [self.sb(esA, f"cn{i}", [128, 640], BF16) for i in range(2)]
                    junk = self.sb(esA, "junk", [128, 384], F32)
                    rs = self.sb(esA, "rs", [128, 8], F32)
                    eps_r = self.sb(esA, "eps_r", [128, 1], F32)
                    rt = [self.sb(esA, f"rt{i}", [32, 512], F32) for i in range(2)]
                    wd_ = self.W('mla_w_in', (o,), [D, 672])
                    sc.dma('pool', win[:], wd_.rearrange("(k p) n -> p k n", p=128), writes=['win'], chan='win')
                    sc.dma('pool', winsw[:, :, 0:16], wd_[:, 656:672].rearrange("(k p) n -> p k n", p=128), writes=['winsw'], chan='win')
                    sc.dma('pool', winsw[:, :, 16:32], wd_[:, 640:656].rearrange("(k p) n -> p k n", p=128), writes=['winsw'], chan='win')
                    sc.dma('sp', GQ[:, 0:384], self.W('mla_q_norm_g', (o,), [1, 384]).partition_broadcast(128), writes=['GQ'], chan='misc')
                    sc.dma('sp', GQ[:, 384:640], self.W('mla_kv_norm_g', (o,), [1, 256]).partition_broadcast(128), writes=['GQ'], chan='misc')
                    sc.op('dve', lambda e: e.memset(eps_r[:], RMS_EPS), writes=['eps_r'])
                    for g4 in range(4):
                        for tl in range(4):
                            t = g4 * 4 + tl
                            self.mod_transpose_tile(t, SC1, SH, utmp, ub, uT4, f'uT{tl}', tl * 128, 4 + t % 2)
                            for (c0, c1, pb) in ((0, 512, 0), (512, 640, 1)):
                                for k in range(8):
                                    sc.op('pe', lambda e, k=k: e.matmul(self.ps[pb][:, 0:c1 - c0], lhsT=uT4[:, k, tl * 128:(tl + 1) * 128],
                                                                        rhs=win[:, k, c0:c1], start=(k == 0), stop=(k == 7)),
                                          reads=[f'uT{tl}', 'win'], writes=[f'ps{pb}'])
                            cps = self.psbig[:, 0:640]
                            sc.op('act', lambda e: e.activation(out=junk[:, 0:384], in_=cps[:, 0:384], func=AF.Square, accum_out=rs[:, 0:1]),
                                  reads=['ps0'], writes=['junk', 'rs0'])
                            sc.op('act', lambda e: e.activation(out=junk[:, 0:256], in_=cps[:, 384:640], func=AF.Square, accum_out=rs[:, 1:2]),
                                  reads=['ps0', 'ps1'], writes=['junk', 'rs1'])
                            sc.op('act', lambda e: e.activation(out=rs[:, 2:3], in_=rs[:, 0:1], func=AF.Sqrt, bias=eps_r[:], scale=1.0 / 384),
                                  reads=['rs0', 'eps_r'], writes=['rs2'])
                            sc.op('act', lambda e: e.activation(out=rs[:, 3:4], in_=rs[:, 1:2], func=AF.Sqrt, bias=eps_r[:], scale=1.0 / 256),
                                  reads=['rs1', 'eps_r'], writes=['rs3'])
                            sc.op('dve', lambda e: e.reciprocal(out=rs[:, 4:6], in_=rs[:, 2:4]), reads=['rs2', 'rs3'], writes=['rs4'])
                            sl = t % 2
                            sc.op('dve', lambda e: e.scalar_tensor_tensor(out=cn[sl][:, 0:384], in0=cps[:, 0:384], scalar=rs[:, 4:5], in1=GQ[:, 0:384],
                                                                          op0=ALU.mult, op1=ALU.mult),
                                  reads=['ps0', 'rs4', 'GQ'], writes=[f'cn{sl}'])
                            sc.op('dve', lambda e: e.scalar_tensor_tensor(out=cn[sl][:, 384:640], in0=cps[:, 384:640], scalar=rs[:, 5:6], in1=GQ[:, 384:640],
                                                                          op0=ALU.mult, op1=ALU.mult),
                                  reads=['ps0', 'ps1', 'rs4', 'GQ'], writes=[f'cn{sl}'])
                            pb = 2 + sl
                            pv = self.ps[pb][:].bitcast(BF16)
                            for k in range(5):
                                sc.op('pe', lambda e, k=k: e.transpose(out=pv[:, k * 128:(k + 1) * 128], in_=cn[sl][:, k * 128:(k + 1) * 128],
                                                                       identity=self.ident_b[:]),
                                      reads=[f'cn{sl}', 'ident_b'], writes=[f'ps{pb}'])
                            sc.op('act', lambda e: e.activation(out=cnT[:, :, t * 128:(t + 1) * 128],
                                                                in_=pv[:, 0:640].rearrange("p (k n) -> p k n", k=5), func=AF.Copy),
                                  reads=[f'ps{pb}'], writes=[f'cnT{t}'])
                        cols = slice(g4 * 512, (g4 + 1) * 512)
                        for k in range(8):
                            sc.op('pe', lambda e, k=k: e.matmul(self.ps[6][0:32, :], lhsT=win[:, k, 640:672], rhs=uT4[:, k, :],
                                                                start=(k == 0), stop=(k == 7)),
                                  reads=['win'] + [f'uT{i}' for i in range(4)], writes=['ps6'])
                        for k in range(8):
                            sc.op('pe', lambda e, k=k: e.matmul(self.ps[7][0:32, :], lhsT=winsw[:, k, :], rhs=uT4[:, k, :],
                                                                start=(k == 0), stop=(k == 7)),
                                  reads=['winsw'] + [f'uT{i}' for i in range(4)], writes=['ps7'])
                        sc.op('dve', lambda e: e.tensor_tensor(out=rt[0][:], in0=self.ps[6][0:32, :], in1=COS[:, cols], op=ALU.mult),
                              reads=['ps6', 'COS'], writes=['rt0'])
                        sc.op('dve', lambda e: e.tensor_tensor(out=rt[1][:], in0=self.ps[7][0:32, :], in1=SINS[:, cols], op=ALU.mult),
                              reads=['ps7', 'SINS'], writes=['rt1'])
                        sc.op('dve', lambda e: e.tensor_tensor(out=krT[:, cols], in0=rt[0][:], in1=rt[1][:], op=ALU.add),
                              reads=['rt0', 'rt1'], writes=['krT'])
                    sc.barrier()
                with ExitStack() as esB:
                    wq = self.sb(esB, "wq", [128, 3, 16, 128], BF16)
                    wkv = self.sb(esB, "wkv", [128, 2, 16, 160], BF16)
                    qT = [self.sb(esB, f"qT{i}", [96, S], BF16) for i in range(2)]
                    kT = [self.sb(esB, f"kT{i}", [96, S], BF16) for i in range(2)]
                    Vh = [self.sb(esB, f"Vh{i}", [128, NT, 64], BF16) for i in range(2)]
                    P = self.sb(esB, "P", [128, S], BF16)
                    PT = self.sb(esB, "PT", [128, 16, 128], BF16)
                    st = self.sb(esB, "at_st", [128, 4], F32)
                    rt = [self.sb(esB, f"rt{i}", [32, 512], F32) for i in range(2)]
                    maskblk = self.make_maskblk(esB)
                    wqd = self.W('mla_w_q_up', (o,), [384, 1536]).rearrange("(k p) (h d) -> p k h d", p=128, d=96)
                    wkvd = self.W('mla_w_kv_up', (o,), [256, 2048]).rearrange("(k p) (h d) -> p k h d", p=128, d=128)
                    for k in range(3):
                        sc.dma('pool', wq[:, k, :, 0:32], wqd[:, k, :, 64:96], writes=['wq'], chan='wq')
                        sc.dma('pool', wq[:, k, :, 32:96], wqd[:, k, :, 0:64], writes=['wq'], chan='wq')
                        sc.dma('pool', wq[:, k, :, 96:112], wqd[:, k, :, 80:96], writes=['wq'], chan='wq')
                        sc.dma('pool', wq[:, k, :, 112:128], wqd[:, k, :, 64:80], writes=['wq'], chan='wq')
                    sc.op('dve', lambda e: e.memset(wkv[:], 0.0), writes=['wkv'])
                    for k in range(2):
                        sc.dma('pool', wkv[:, k, :, 32:160], wkvd[:, k, :, :], writes=['wkv'], chan='wkv')
                    for h in range(16):
                        sl = h % 2
                        for g4 in range(4):
                            cols = slice(g4 * 512, (g4 + 1) * 512)
                            for k in range(3):
                                sc.op('pe', lambda e, k=k: e.matmul(self.ps[7][0:96, :], lhsT=wq[:, k, h, 0:96], rhs=cnT[:, k, cols],
                                                                    start=(k == 0), stop=(k == 2)),
                                      reads=['wq'] + [f'cnT{t}' for t in range(g4 * 4, g4 * 4 + 4)], writes=['ps7'])
                            for k in range(3):
                                sc.op('pe', lambda e, k=k: e.matmul(self.ps[6][0:32, :], lhsT=wq[:, k, h, 96:128], rhs=cnT[:, k, cols],
                                                                    start=(k == 0), stop=(k == 2)),
                                      reads=['wq'] + [f'cnT{t}' for t in range(g4 * 4, g4 * 4 + 4)], writes=['ps6'])
                            sc.op('dve', lambda e: e.tensor_tensor(out=rt[0][:], in0=self.ps[7][0:32, :], in1=COS[:, cols], op=ALU.mult),
                                  reads=['ps7', 'COS'], writes=['rt0'])
                            sc.op('dve', lambda e: e.tensor_tensor(out=rt[1][:], in0=self.ps[6][0:32, :], in1=SINS[:, cols], op=ALU.mult),
                                  reads=['ps6', 'SINS'], writes=['rt1'])
                            sc.op('dve', lambda e: e.tensor_tensor(out=qT[sl][0:32, cols], in0=rt[0][:], in1=rt[1][:], op=ALU.add),
                                  reads=['rt0', 'rt1'], writes=[f'qT{sl}'])
                            for (p0, p1) in ((32, 64), (64, 96)):
                                sc.op('act', lambda e: e.activation(out=qT[sl][p0:p1, cols], in_=self.ps[7][p0:p1, :], func=AF.Copy),
                                      reads=['ps7'], writes=[f'qT{sl}'])
                            for k in range(2):
                                sc.op('pe', lambda e, k=k: e.matmul(self.ps[7][0:96, :], lhsT=wkv[:, k, h, 0:96], rhs=cnT[:, 3 + k, cols],
                                                                    start=(k == 0), stop=(k == 1)),
                                      reads=['wkv'] + [f'cnT{t}' for t in range(g4 * 4, g4 * 4 + 4)], writes=['ps7'])
                            for (p0, p1) in ((32, 64), (64, 96)):
                                sc.op('act', lambda e: e.activation(out=kT[sl][p0:p1, cols], in_=self.ps[7][p0:p1, :], func=AF.Copy),
                                      reads=['ps7'], writes=[f'kT{sl}'])
                        sc.op('pool', lambda e: e.tensor_copy(out=kT[sl][0:32, :], in_=krT[:]), reads=['krT'], writes=[f'kT{sl}'])
                        for g8 in range(2):
                            pb = 4 + g8
                            for tl in range(8):
                                t = g8 * 8 + tl
                                for k in range(2):
                                    sc.op('pe', lambda e, k=k: e.matmul(self.ps[pb][:, tl * 64:(tl + 1) * 64], lhsT=cnT[:, 3 + k, t * 128:(t + 1) * 128],
                                                                        rhs=wkv[:, k, h, 96:160], start=(k == 0), stop=(k == 1)),
                                          reads=['wkv', f'cnT{t}'], writes=[f'ps{pb}'])
                            sc.op('act', lambda e: e.activation(out=Vh[sl][:, g8 * 8:(g8 + 1) * 8, :],
                                                                in_=self.ps[pb][:].rearrange("p (t d) -> p t d", d=64), func=AF.Copy),
                                  reads=[f'ps{pb}'], writes=[f'Vh{sl}'])
                        for qt in range(NT):
                            self.attn_tile(qT[sl][:, qt * 128:(qt + 1) * 128], kT[sl], f'kT{sl}', Vh[sl], f'Vh{sl}', (qt + 1) * 128, scale,
                                           O[:, qt, h * 64:(h + 1) * 64], f'O{qt}', (P, PT, st), maskblk, [f'qT{sl}'])
                    sc.barrier()
            self.mixer_tail(b, l, O, lambda t: f'O{t}', self.W('mla_w_out', (o,), [D, D]))


    def hyb_sublayer(self, b, l):
        sc = self.sc
        e_ = l // 2
        if not hasattr(self, 'xspill'):
            self.xspill = self.nc.dram_tensor("xspill", [S, D], F32, kind="Internal").ap()
        win_d = self.W('hyb_w_in', (e_,), [D, 4176])
        with ExitStack() as esO:
            O = self.sb(esO, "O", [128, NT, D], BF16)
            uT = self.sb(esO, "uT_all", [128, 8, S], BF16)
            with ExitStack() as esA:
                SH, SC1, _, _, _ = self.modulation(esA, b, l, 1, 1.0, which=('SH', 'SC1'))
                utmp = [self.sb(esA, f"utmp{i}", [128, D], F32) for i in range(2)]
                ub = [self.sb(esA, f"ub{i}", [128, D], BF16) for i in range(2)]
                for t in range(NT):
                    self.mod_transpose_tile(t, SC1, SH, utmp, ub, uT, f'uT{t}', t * 128, 4 + t % 2)
                    sc.dma('sp', self.xspill[t * 128:(t + 1) * 128, :], self.X[t][:], reads=[f'X{t}'], writes=[f'xsp{t}'], chan='xsp')
                sc.barrier()
            Xb = [self.X[t][:].bitcast(BF16) for t in range(NT)]
            uTk = [f'uT{t}' for t in range(NT)]
            if HYB_DSA:
                self.dsa_part(b, l, e_, win_d, O, uT, Xb, uTk)
            else:
                for t in range(NT):
                    sc.op('dve', lambda e: e.memset(O[:, t, 0:512], 0.0), writes=[f'O{t}'])
            if HYB_GDN:
                self.gdn_part(b, l, e_, win_d, O, uT, Xb, uTk)
            else:
                for t in range(NT):
                    sc.op('dve', lambda e: e.memset(O[:, t, 512:1024], 0.0), writes=[f'O{t}'])
            sc.barrier()
            for t in range(NT):
                sc.dma('sp', self.X[t][:], self.xspill[t * 128:(t + 1) * 128, :], writes=[f'X{t}'], chan='xin')
            with ExitStack() as esT:
                pass
            self.mixer_tail(b, l, O, lambda t: f'O{t}', self.W('hyb_w_out', (e_,), [D, D]))

    def proj_featmajor(self, wchunk, wkeys, loads, uT, uTk, dst_of, evac_scale=1.0):
        sc = self.sc
        for ci, (pieces, dst, dkey) in enumerate(loads):
            sl = self._wc % 2
            self._wc += 1
            for (d0, d1, srcap) in pieces:
                sc.dma('pool', wchunk[sl][:, :, d0:d1], srcap.rearrange("(k p) n -> p k n", p=128), writes=[wkeys[sl]], chan=wkeys[sl])
            for g4 in range(4):
                pb = 4 + (g4 % 2)
                for k in range(8):
                    sc.op('pe', lambda e, k=k: e.matmul(self.ps[pb][:], lhsT=wchunk[sl][:, k, :], rhs=uT[:, k, g4 * 512:(g4 + 1) * 512],
                                                        start=(k == 0), stop=(k == 7)),
                          reads=[wkeys[sl]] + uTk[g4 * 4:g4 * 4 + 4], writes=[f'ps{pb}'])
                if g4 % 2 == 0:
                    sc.op('act', lambda e: e.activation(out=dst[:, g4 * 512:(g4 + 1) * 512], in_=self.ps[pb][:], func=AF.Copy, scale=evac_scale),
                          reads=[f'ps{pb}'], writes=[dkey])
                else:
                    sc.op('dve', lambda e: e.tensor_scalar(out=dst[:, g4 * 512:(g4 + 1) * 512], in0=self.ps[pb][:], scalar1=evac_scale, scalar2=None,
                                                           op0=ALU.mult), reads=[f'ps{pb}'], writes=[dkey])

    def dsa_part(self, b, l, e_, win_d, O, uT, Xb, uTk):
        sc = self.sc
        self._wc = 0
        with ExitStack() as es:
            wchunk = [self.sb(es, f"wch{i}", [128, 8, 128], BF16) for i in range(2)]
            wkeys = ['wch0', 'wch1']
            ikT2 = self.sb(es, "ikT2", [128, S], BF16)
            iw = self.sb(es, "iw", [128, NT, 8], F32)
            posrow = self.sb(es, "posrow", [128, S], F32)
            poscol = self.sb(es, "poscol", [128, NT], F32)
            acc = self.sb(es, "acc", [128, S], F32)
            MB = self.sb(es, "MB", [128, S], F32)
            Dt = self.sb(es, "Dt", [128, S], F32)
            bh = [self.sb(es, f"bh{i}", [128, S], F32) for i in range(2)]
            rl = [self.sb(es, f"rl{i}", [128, 512], F32) for i in range(2)]
            P = self.sb(es, "P", [128, S], BF16)
            junk = P
            PT = self.sb(es, "PT", [128, 16, 128], BF16)
            st = self.sb(es, "at_st", [128, 4], F32)
            bs = self.sb(es, "bs", [128, 8], F32)
            maskf = self.sb(es, "maskf", [128, 128], F32)
            with ExitStack() as es2:
                pri = Dt[:].bitcast(I32)
                pci = self.sb(es2, "pci", [128, NT], I32)
                mi = self.sb(es2, "mi", [128, 128], I32)
                sc.dma('sp', pri, self.pos[b:b + 1, :].partition_broadcast(128), writes=['pri'], chan='misc')
                sc.dma('sp', pci[:], self.pos[b].rearrange("(t p) -> p t", p=128), writes=['pci'], chan='misc', allow_slow_non_contiguous=True)
                sc.op('dve', lambda e: e.tensor_copy(out=posrow[:], in_=pri), reads=['pri'], writes=['posrow'])
                sc.op('dve', lambda e: e.tensor_copy(out=poscol[:], in_=pci[:]), reads=['pci'], writes=['poscol'])
                sc.op('dve', lambda e: e.tensor_scalar(out=poscol[:], in0=poscol[:], scalar1=-1.0, scalar2=None, op0=ALU.mult),
                      reads=['poscol'], writes=['poscol'])
                sc.op('pool', lambda e: e.iota(mi[:], pattern=[[1, 128]], base=0, channel_multiplier=0), writes=['mi'])
                sc.op('dve', lambda e: e.tensor_scalar(out=maskf[:], in0=mi[:], scalar1=64.0, scalar2=-1e30, op0=ALU.is_ge, op1=ALU.mult),
                      reads=['mi'], writes=['maskf'])
                sc.op('dve', lambda e: e.memset(maskf[64:128, :], 0.0), reads=['maskf'], writes=['maskf'])
                sc.barrier()
            loads = []
            for c in range(4):
                loads.append(([(0, 128, win_d[:, c * 128:(c + 1) * 128])], Xb[c], f'X{c}'))
            self.proj_featmajor(wchunk, wkeys, loads, uT, uTk, None, evac_scale=0.125)
            loads = []
            for c in range(4):
                loads.append(([(0, 128, win_d[:, 512 + c * 128:512 + (c + 1) * 128])], Xb[4 + c], f'X{4 + c}'))
            for c in range(4):
                loads.append(([(0, 128, win_d[:, 1536 + c * 128:1536 + (c + 1) * 128])], Xb[8 + c], f'X{8 + c}'))
            loads.append(([(0, 64, win_d[:, 2048:2112]), (64, 128, win_d[:, 2048:2112])], ikT2, 'ikT2'))
            self.proj_featmajor(wchunk, wkeys, loads, uT, uTk, None)
            with ExitStack() as es2:
                wv = MB[:].bitcast(BF16).rearrange("p (k n) -> p k n", k=8)
                wiw = self.sb(es2, "wiw", [128, 8, 8], BF16)
                sc.dma('pool', wv, win_d[:, 1024:1536].rearrange("(k p) n -> p k n", p=128), writes=['wv'], chan='wv')
                sc.dma('pool', wiw[:], win_d[:, 2112:2120].rearrange("(k p) n -> p k n", p=128), writes=['wiw'], chan='wv')
                for t in range(NT):
                    pb = 4 + t % 2
                    for k in range(8):
                        sc.op('pe', lambda e, k=k: e.matmul(self.ps[pb][:], lhsT=uT[:, k, t * 128:(t + 1) * 128], rhs=wv[:, k, :],
                                                            start=(k == 0), stop=(k == 7)), reads=[uTk[t], 'wv'], writes=[f'ps{pb}'])
                    sc.op('act', lambda e: e.activation(out=Xb[12 + t // 4][:, (t % 4) * 512:(t % 4 + 1) * 512], in_=self.ps[pb][:], func=AF.Copy),
                          reads=[f'ps{pb}'], writes=[f'X{12 + t // 4}'])
                    pb2 = 6 + t % 2
                    for k in range(8):
                        sc.op('pe', lambda e, k=k: e.matmul(self.ps[pb2][:, 0:8], lhsT=uT[:, k, t * 128:(t + 1) * 128], rhs=wiw[:, k, :],
                                                            start=(k == 0), stop=(k == 7)), reads=[uTk[t], 'wiw'], writes=[f'ps{pb2}'])
                    sc.op('dve', lambda e: e.tensor_copy(out=iw[:, t, :], in_=self.ps[pb2][:, 0:8]), reads=[f'ps{pb2}'], writes=['iw'])
                sc.barrier()
            NIT = 14
            for qt in range(NT):
                KV = (qt + 1) * 128
                qs = slice(qt * 128, (qt + 1) * 128)
                nkc = (KV + 511) // 512
                bank = 0
                for kc in range(nkc):
                    c0 = kc * 512
                    c1 = min(KV, c0 + 512)
                    for h in range(8):
                        pb = bank % 4
                        bank += 1
                        hp = slice((h % 2) * 64, (h % 2) * 64 + 64)
                        sc.op('pe', lambda e: e.matmul(self.ps[pb][:, 0:c1 - c0], lhsT=Xb[8 + h // 2][hp, qs], rhs=ikT2[hp, c0:c1],
                                                       start=True, stop=True),
                              reads=[f'X{8 + h // 2}', 'ikT2'], writes=[f'ps{pb}', 'psS'])
                        rs_ = h % 2
                        sc.op('act', lambda e: e.activation(out=rl[rs_][:, 0:c1 - c0], in_=self.ps[pb][:, 0:c1 - c0], func=AF.Relu),
                              reads=[f'ps{pb}'], writes=[f'rl{rs_}'])
                        if h == 0:
                            sc.op('dve', lambda e: e.tensor_scalar(out=acc[:, c0:c1], in0=rl[rs_][:, 0:c1 - c0], scalar1=iw[:, qt, 0:1], scalar2=None,
                                                                   op0=ALU.mult), reads=[f'rl{rs_}', 'iw'], writes=['acc'])
                        else:
                            sc.op('dve', lambda e: e.scalar_tensor_tensor(out=acc[:, c0:c1], in0=rl[rs_][:, 0:c1 - c0], scalar=iw[:, qt, h:h + 1],
                                                                          in1=acc[:, c0:c1], op0=ALU.mult, op1=ALU.add),
                                  reads=[f'rl{rs_}', 'iw', 'acc'], writes=['acc'])
                if KV > 256:
                    sc.op('dve', lambda e: e.tensor_reduce(out=bs[:, 0:1], in_=acc[:, 0:KV], axis=AX.X, op=ALU.max), reads=['acc'], writes=['bs_hi'])
                    sc.op('dve', lambda e: e.tensor_reduce(out=bs[:, 1:2], in_=acc[:, 0:KV], axis=AX.X, op=ALU.min), reads=['acc'], writes=['bs_lo'])
                    sc.op('dve', lambda e: e.tensor_tensor(out=bs[:, 2:3], in0=bs[:, 0:1], in1=bs[:, 1:2], op=ALU.subtract),
                          reads=['bs_hi', 'bs_lo'], writes=['bs_h'])
                sc.op('dve', lambda e: e.tensor_tensor(out=acc[:, KV - 128:KV], in0=acc[:, KV - 128:KV], in1=maskf[:], op=ALU.add),
                      reads=['acc', 'maskf'], writes=['acc'])
                if KV > 256:
                    for it in range(NIT):
                        sc.op('dve', lambda e: e.tensor_scalar(out=bs[:, 2:3], in0=bs[:, 2:3], scalar1=0.5, scalar2=None, op0=ALU.mult),
                              reads=['bs_h'], writes=['bs_h'])
                        sc.op('dve', lambda e: e.tensor_tensor(out=bs[:, 3:4], in0=bs[:, 1:2], in1=bs[:, 2:3], op=ALU.add),
                              reads=['bs_lo', 'bs_h'], writes=['bs_mid'])
                        sc.op('dve', lambda e: e.tensor_scalar(out=junk[:, 0:KV], in0=acc[:, 0:KV], scalar1=bs[:, 3:4], scalar2=0.0,
                                                               op0=ALU.is_ge, op1=ALU.add, accum_out=bs[:, 4:5]),
                              reads=['acc', 'bs_mid'], writes=['at_P', 'bs_cnt'])
                        sc.op('dve', lambda e: e.tensor_scalar(out=bs[:, 5:6], in0=bs[:, 4:5], scalar1=255.5, scalar2=bs[:, 2:3],
                                                               op0=ALU.is_ge, op1=ALU.mult), reads=['bs_cnt', 'bs_h'], writes=['bs_g'])
                        sc.op('dve', lambda e: e.tensor_tensor(out=bs[:, 1:2], in0=bs[:, 1:2], in1=bs[:, 5:6], op=ALU.add),
                              reads=['bs_lo', 'bs_g'], writes=['bs_lo'])
                    thr = bs[:, 1:2]
                else:
                    sc.op('dve', lambda e: e.memset(bs[:, 1:2], -1e29), writes=['bs_lo'])
                    thr = bs[:, 1:2]
                sc.op('dve', lambda e: e.tensor_scalar(out=MB[:, 0:KV], in0=acc[:, 0:KV], scalar1=thr, scalar2=-1e30, op0=ALU.is_lt, op1=ALU.mult),
                      reads=['acc', 'bs_lo'], writes=['MB'])
                sc.op('act', lambda e: e.activation(out=Dt[:, 0:KV], in_=posrow[:, 0:KV], func=AF.Abs, bias=poscol[:, qt:qt + 1], scale=1.0),
                      reads=['posrow', 'poscol'], writes=['Dt'])
                for h in range(8):
                    sl = h % 2
                    slope = float(2.0 ** (-(h + 1)))
                    sc.op('dve', lambda e: e.scalar_tensor_tensor(out=bh[sl][:, 0:KV], in0=Dt[:, 0:KV], scalar=-slope, in1=MB[:, 0:KV],
                                                                  op0=ALU.mult, op1=ALU.add), reads=['Dt', 'MB'], writes=[f'bh{sl}'])
                    hp = slice((h % 2) * 64, (h % 2) * 64 + 64)
                    self.attn_tile(Xb[h // 2][hp, qs], Xb[4 + h // 2][hp, :], f'X{4 + h // 2}', None, None, KV, 1.0,
                                   O[:, qt, h * 64:(h + 1) * 64], f'O{qt}', (P, PT, st), None, [f'X{h // 2}'],
                                   bias=bh[sl], bias_key=f'bh{sl}',
                                   v_of=lambda blk, h=h: (Xb[12 + blk // 4][:, (blk % 4) * 512 + h * 64:(blk % 4) * 512 + (h + 1) * 64], f'X{12 + blk // 4}'),
                                   dv=64)
            sc.barrier()


    def gdn_part(self, b, l, e_, win_d, O, uT, Xb, uTk):
        sc = self.sc
        self._wc = 0
        T_ = slice
        with ExitStack() as es:
            U = self.sb(es, "gU", [128, 128], F32)
            Li = self.sb(es, "gLi", [128, 128], F32)
            Ls = self.sb(es, "gLs", [128, 128], F32)
            ones = self.sb(es, "gones", [128, 128], F32)
            NG = self.sb(es, "gNG", [128, 128], F32)
            cw = self.sb(es, "gcw", [128, 12, 4], F32)
            prm = self.sb(es, "gprm", [128, 16], F32)
            epsb = self.sb(es, "gepsb", [128, 4], F32)
            ab = self.sb(es, "gab", [128, NT, 8], F32)
            gg = self.sb(es, "ggg", [128, NT, 4], F32)
            bt = self.sb(es, "gbt", [128, NT, 4], F32)
            sc.op('dve', lambda e: e.tensor_scalar(out=U[:], in0=self.iot[:], scalar1=0.0, scalar2=None, op0=ALU.is_ge), reads=['iot'], writes=['gU'])
            sc.op('dve', lambda e: e.tensor_scalar(out=Li[:], in0=self.iot[:], scalar1=0.0, scalar2=None, op0=ALU.is_le), reads=['iot'], writes=['gLi'])
            sc.op('dve', lambda e: e.tensor_scalar(out=Ls[:], in0=self.iot[:], scalar1=0.0, scalar2=None, op0=ALU.is_lt), reads=['iot'], writes=['gLs'])
            sc.op('dve', lambda e: e.memset(ones[:], 1.0), writes=['gones'])
            sc.op('dve', lambda e: e.memset(epsb[:, 0:1], RMS_EPS), writes=['gepsb'])
            sc.op('dve', lambda e: e.memset(epsb[:, 1:2], 128.0 * RMS_EPS), writes=['gepsb'])
            sc.dma('sp', NG[:], self.W('gdn_norm_g', (e_,), [1, 128]).partition_broadcast(128), writes=['gNG'], chan='misc')
            for j in range(4):
                sc.dma('sp', cw[:, :, j], self.W('gdn_conv_w', (e_,), [4, 1536])[j].rearrange("(c p) -> p c", p=128), writes=['gcw'], chan='misc',
                       allow_slow_non_contiguous=True)
            sc.dma('sp', prm[:, 0:4], self.W('gdn_dt_bias', (e_,), [1, 4]).partition_broadcast(128), writes=['gprm'], chan='misc')
            sc.dma('sp', prm[:, 4:8], self.W('gdn_a_log', (e_,), [1, 4]).partition_broadcast(128), writes=['gprm'], chan='misc')
            sc.op('act', lambda e: e.activation(out=prm[:, 8:12], in_=prm[:, 4:8], func=AF.Exp), reads=['gprm'], writes=['gprm'])
            sc.op('dve', lambda e: e.tensor_scalar(out=prm[:, 8:12], in0=prm[:, 8:12], scalar1=-1.0, scalar2=None, op0=ALU.mult),
                  reads=['gprm'], writes=['gprm'])
            with ExitStack() as es1:
                wchunk = [self.sb(es1, f"wch{i}", [128, 8, 128], BF16) for i in range(2)]
                raw = self.sb(es1, "graw", [128, S + 3], F32)
                y = self.sb(es1, "gy", [128, S], F32)
                sq = [self.sb(es1, f"gsq{i}", [128, 512], F32) for i in range(2)]
                rin = [self.sb(es1, f"grin{i}", [128, 512], F32) for i in range(2)]
                sc.op('dve', lambda e: e.memset(raw[:, 0:3], 0.0), writes=['graw'])
                for ci in range(12):
                    sl = ci % 2
                    c0 = 2120 + ci * 128
                    sc.dma('pool', wchunk[sl][:], win_d[:, c0:c0 + 128].rearrange("(k p) n -> p k n", p=128), writes=[f'wch{sl}'], chan=f'wch{sl}')
                    for g4 in range(4):
                        pb = 4 + (g4 % 2)
                        for k in range(8):
                            sc.op('pe', lambda e, k=k: e.matmul(self.ps[pb][:], lhsT=wchunk[sl][:, k, :], rhs=uT[:, k, g4 * 512:(g4 + 1) * 512],
                                                                start=(k == 0), stop=(k == 7)),
                                  reads=[f'wch{sl}'] + uTk[g4 * 4:g4 * 4 + 4], writes=[f'ps{pb}'])
                        sc.op('act', lambda e: e.activation(out=raw[:, 3 + g4 * 512:3 + (g4 + 1) * 512], in_=self.ps[pb][:], func=AF.Copy),
                              reads=[f'ps{pb}'], writes=['graw'])
                    sc.op('dve', lambda e: e.tensor_scalar(out=y[:], in0=raw[:, 0:S], scalar1=cw[:, ci, 0:1], scalar2=None, op0=ALU.mult),
                          reads=['graw', 'gcw'], writes=['gy'])
                    for j in range(1, 4):
                        sc.op('dve', lambda e, j=j: e.scalar_tensor_tensor(out=y[:], in0=raw[:, j:S + j], scalar=cw[:, ci, j:j + 1], in1=y[:],
                                                                           op0=ALU.mult, op1=ALU.add), reads=['graw', 'gcw', 'gy'], writes=['gy'])
                    dst = Xb[ci]
                    if ci >= 8:
                        sc.op('act', lambda e: e.activation(out=dst[:, :], in_=y[:], func=AF.Silu), reads=['gy'], writes=[f'X{ci}'])
                    else:
                        sc.op('act', lambda e: e.activation(out=y[:], in_=y[:], func=AF.Silu), reads=['gy'], writes=['gy'])
                        for g4 in range(4):
                            cs_ = slice(g4 * 512, (g4 + 1) * 512)
                            s2 = g4 % 2
                            sc.op('pool', lambda e: e.tensor_tensor(out=sq[s2][:], in0=y[:, cs_], in1=y[:, cs_], op=ALU.mult),
                                  reads=['gy'], writes=[f'gsq{s2}'])
                            pb = 6 + s2
                            sc.op('pe', lambda e: e.matmul(self.ps[pb][:], lhsT=ones[:], rhs=sq[s2][:], start=True, stop=True),
                                  reads=['gones', f'gsq{s2}'], writes=[f'ps{pb}'])
                            if ci < 4:
                                sc.op('act', lambda e: e.activation(out=rin[s2][:], in_=self.ps[pb][:], func=AF.Sqrt, bias=epsb[:, 1:2], scale=128.0),
                                      reads=[f'ps{pb}', 'gepsb'], writes=[f'grin{s2}'])
                            else:
                                sc.op('act', lambda e: e.activation(out=rin[s2][:], in_=self.ps[pb][:], func=AF.Sqrt, bias=epsb[:, 0:1], scale=1.0),
                                      reads=[f'ps{pb}', 'gepsb'], writes=[f'grin{s2}'])
                            sc.op('dve', lambda e: e.reciprocal(out=rin[s2][:], in_=rin[s2][:]), reads=[f'grin{s2}'], writes=[f'grin{s2}'])
                            sc.op('dve', lambda e: e.tensor_tensor(out=dst[:, cs_], in0=y[:, cs_], in1=rin[s2][:], op=ALU.mult),
                                  reads=['gy', f'grin{s2}'], writes=[f'X{ci}'])
                sc.barrier()
            if GDN_STOP <= 1:
                for t in range(NT):
                    sc.op('dve', lambda e: e.memset(O[:, t, 512:1024], 0.0), writes=[f'O{t}'])
                sc.barrier()
                return
            with ExitStack() as es2:
                wz = self.sb(es2, "gwz", [128, 8, 512], BF16)
                wab = self.sb(es2, "gwab", [128, 8, 8], BF16)
                tmp = self.sb(es2, "gtmp", [128, NT, 4], F32)
                sc.dma('pool', wz[:], win_d[:, 3664:4176].rearrange("(k p) n -> p k n", p=128), writes=['gwz'], chan='wv')
                sc.dma('pool', wab[:], win_d[:, 3656:3664].rearrange("(k p) n -> p k n", p=128), writes=['gwab'], chan='wv')
                for t in range(NT):
                    pb = 4 + t % 2
                    for k in range(8):
                        sc.op('pe', lambda e, k=k: e.matmul(self.ps[pb][:], lhsT=uT[:, k, t * 128:(t + 1) * 128], rhs=wz[:, k, :],
                                                            start=(k == 0), stop=(k == 7)), reads=[uTk[t], 'gwz'], writes=[f'ps{pb}'])
                    sc.op('act', lambda e: e.activation(out=Xb[12 + t // 4][:, (t % 4) * 512:(t % 4 + 1) * 512], in_=self.ps[pb][:], func=AF.Silu),
                          reads=[f'ps{pb}'], writes=[f'X{12 + t // 4}'])
                    pb2 = 6 + t % 2
                    for k in range(8):
                        sc.op('pe', lambda e, k=k: e.matmul(self.ps[pb2][:, 0:8], lhsT=uT[:, k, t * 128:(t + 1) * 128], rhs=wab[:, k, :],
                                                            start=(k == 0), stop=(k == 7)), reads=[uTk[t], 'gwab'], writes=[f'ps{pb2}'])
                    sc.op('dve', lambda e: e.tensor_copy(out=ab[:, t, :], in_=self.ps[pb2][:, 0:8]), reads=[f'ps{pb2}'], writes=['gab'])
                bc4 = lambda ap: ap.unsqueeze(1).to_broadcast([128, NT, 4])
                sc.op('dve', lambda e: e.tensor_tensor(out=tmp[:], in0=ab[:, :, 0:4], in1=bc4(prm[:, 0:4]), op=ALU.add),
                      reads=['gab', 'gprm'], writes=['gtmp'])
                sc.op('act', lambda e: e.activation(out=tmp[:], in_=tmp[:], func=AF.Exp), reads=['gtmp'], writes=['gtmp'])
                sc.op('act', lambda e: e.activation(out=tmp[:], in_=tmp[:], func=AF.Ln, bias=1.0, scale=1.0), reads=['gtmp'], writes=['gtmp'])
                sc.op('dve', lambda e: e.tensor_tensor(out=gg[:], in0=tmp[:], in1=bc4(prm[:, 8:12]), op=ALU.mult),
                      reads=['gtmp', 'gprm'], writes=['ggg'])
                sc.op('act', lambda e: e.activation(out=bt[:], in_=ab[:, :, 4:8], func=AF.Sigmoid), reads=['gab'], writes=['gbt'])
                sc.barrier()
            if GDN_STOP <= 2:
                for t in range(NT):
                    sc.op('dve', lambda e: e.memset(O[:, t, 512:1024], 0.0), writes=[f'O{t}'])
                sc.barrier()
                return
            with ExitStack() as es3:
                f32t = lambda n: self.sb(es3, n, [128, 128], F32)
                b16t = lambda n: self.sb(es3, n, [128, 128], BF16)
                NS = 2
                B = []
                for i in range(NS):
                    B.append(dict(gb=f32t(f"g_gb{i}"), dd=f32t(f"g_dd{i}"), E=f32t(f"g_E{i}"), ET=f32t(f"g_ET{i}"),
                                  Pa=f32t(f"g_Pa{i}"), Pb=f32t(f"g_Pb{i}"), Qa=f32t(f"g_Qa{i}"), Qb=f32t(f"g_Qb{i}"),
                                  R=f32t(f"g_R{i}"), Rb=b16t(f"g_Rb{i}"), AT=b16t(f"g_AT{i}"), vb=b16t(f"g_vb{i}"),
                                  kbg=b16t(f"g_kbg{i}"), kdec=b16t(f"g_kdec{i}"), u=f32t(f"g_u{i}"), wT=b16t(f"g_wT{i}"),
                                  vnb=b16t(f"g_vnb{i}"), o=f32t(f"g_o{i}"), o2=f32t(f"g_o2{i}"), sm=self.sb(es3, f"g_sm{i}", [128, 16], F32)))
                Sst = [f32t(f"g_S{h}") for h in range(4)]
                Sbf = [b16t(f"g_Sb{h}") for h in range(4)]
                for h in range(4):
                    sc.op('dve', lambda e: e.memset(Sst[h][:], 0.0), writes=[f'gS{h}'])
                    sc.op('dve', lambda e: e.memset(Sbf[h][:], 0.0), writes=[f'gSb{h}'])
                q = lambda pb, j: self.ps[pb][:, j * 128:(j + 1) * 128]
                for t in range(0 if G3_LEVEL < 5 else GDN_NT, NT):
                    sc.op('dve', lambda e: e.memset(O[:, t, 512:1024], 0.0), writes=[f'O{t}'])
                for t in range(GDN_NT):
                    ts_ = slice(t * 128, (t + 1) * 128)
                    for h in range(4):
                        i = (t * 4 + h) % NS
                        bb = B[i]
                        K = lambda n: f'g_{n}{i}'
                        sm = bb['sm']
                        qT, kT, vT = Xb[h][:, ts_], Xb[4 + h][:, ts_], Xb[8 + h][:, ts_]
                        kq, kk_, kv = f'X{h}', f'X{4 + h}', f'X{8 + h}'
                        gcol = gg[:, t, h:h + 1]
                        bcol = bt[:, t, h:h + 1]
                        sc.op('dve', lambda e: e.tensor_copy(out=bb['gb'][:], in_=gcol.to_broadcast([128, 128])), reads=['ggg'], writes=[K('gb')])
                        sc.op('pe', lambda e: e.matmul(q(0, 0), lhsT=U[:], rhs=bb['gb'][:], start=True, stop=True),
                              reads=['gU', K('gb')], writes=['ps0'])
                        sc.op('pe', lambda e: e.matmul(q(0, 1), lhsT=bb['gb'][:], rhs=U[:], start=True, stop=True),
                              reads=['gU', K('gb')], writes=['ps0'])
                        sc.op('act', lambda e: e.activation(out=sm[:, 0:1], in_=q(0, 0)[:, 0:1], func=AF.Copy), reads=['ps0'], writes=[K('sm0')])
                        sc.op('dve', lambda e: e.tensor_scalar(out=bb['dd'][:], in0=q(0, 1), scalar1=sm[:, 0:1], scalar2=0.0,
                                                               op0=ALU.subtract, op1=ALU.max), reads=['ps0', K('sm0')], writes=[K('dd')])
                        sc.op('act', lambda e: e.activation(out=bb['E'][:], in_=bb['dd'][:], func=AF.Exp, scale=-1.0), reads=[K('dd')], writes=[K('E')])
                        sc.op('dve', lambda e: e.tensor_scalar(out=bb['dd'][:], in0=q(0, 1), scalar1=sm[:, 0:1], scalar2=0.0,
                                                               op0=ALU.subtract, op1=ALU.min), reads=['ps0', K('sm0'), K('dd')], writes=[K('dd')])
                        sc.op('act', lambda e: e.activation(out=bb['ET'][:], in_=bb['dd'][:], func=AF.Exp), reads=[K('dd')], writes=[K('ET')])
                        sc.op('pool', lambda e: e.tensor_tensor(out=bb['ET'][:], in0=bb['ET'][:], in1=U[:], op=ALU.mult), reads=[K('ET'), 'gU'], writes=[K('ET')])
                        sc.op('pool', lambda e: e.tensor_tensor(out=bb['E'][:], in0=bb['E'][:], in1=Ls[:], op=ALU.mult), reads=[K('E'), 'gLs'], writes=[K('E')])
                        sc.op('act', lambda e: e.activation(out=sm[:, 1:2], in_=sm[:, 0:1], func=AF.Exp), reads=[K('sm0')], writes=[K('sm1')])
                        sc.op('dve', lambda e: e.tensor_tensor(out=sm[:, 2:3], in0=sm[:, 1:2], in1=bcol, op=ALU.mult), reads=[K('sm1'), 'gbt'], writes=[K('sm2')])
                        sc.op('dve', lambda e: e.tensor_tensor(out=sm[:, 3:4], in0=q(0, 1)[:, 127:128], in1=sm[:, 0:1], op=ALU.subtract),
                              reads=['ps0', K('sm0')], writes=[K('sm3')])
                        sc.op('act', lambda e: e.activation(out=sm[:, 3:4], in_=sm[:, 3:4], func=AF.Exp), reads=[K('sm3')], writes=[K('sm3')])
                        sc.op('act', lambda e: e.activation(out=sm[:, 4:5], in_=q(0, 1)[:, 127:128], func=AF.Exp), reads=['ps0'], writes=[K('sm4')])
                        if G3_LEVEL < 2:
                            continue
                        sc.op('pe', lambda e: e.matmul(q(1, 0), lhsT=kT, rhs=kT, start=True, stop=True), reads=[kk_], writes=['ps1'])
                        sc.op('pe', lambda e: e.matmul(q(1, 1), lhsT=kT, rhs=qT, start=True, stop=True), reads=[kk_, kq], writes=['ps1'])
                        if G3_LEVEL < 2.07:
                            continue
                        sc.op('dve', lambda e: e.scalar_tensor_tensor(out=bb['Qa'][:], in0=q(1, 0), scalar=bcol, in1=bb['E'][:], op0=ALU.mult, op1=ALU.mult),
                              reads=['ps1', 'gbt', K('E')], writes=[K('Qa')])
                        sc.op('dve', lambda e: e.tensor_tensor(out=bb['AT'][:], in0=q(1, 1), in1=bb['ET'][:], op=ALU.mult),
                              reads=['ps1', K('ET')], writes=[K('AT')])
                        if G3_LEVEL < 2.15:
                            continue
                        sc.op('pe', lambda e: e.matmul(q(2, 0), lhsT=bb['Qa'][:], rhs=self.ident_f[:], start=True, stop=True), reads=[K('Qa'), 'ident_f'], writes=['ps2'])
                        sc.op('act', lambda e: e.activation(out=bb['Pa'][:], in_=q(2, 0), func=AF.Copy), reads=['ps2'], writes=[K('Pa')])
                        sc.op('dve', lambda e: e.tensor_tensor(out=bb['R'][:], in0=self.ident_f[:], in1=bb['Pa'][:], op=ALU.subtract),
                              reads=[K('Pa'), 'ident_f'], writes=[K('R')])
                        if G3_LEVEL < 2.25:
                            continue
                        pk = self.ps[3][:].bitcast(BF16)
                        sc.op('pe', lambda e: e.transpose(out=pk[:, 0:128], in_=kT, identity=self.ident_b[:]), reads=[kk_, 'ident_b'], writes=['ps3'])
                        sc.op('pe', lambda e: e.transpose(out=pk[:, 128:256], in_=vT, identity=self.ident_b[:]), reads=[kv, 'ident_b'], writes=['ps3'])
                        sc.op('act', lambda e: e.activation(out=bb['kbg'][:], in_=pk[:, 0:128], func=AF.Copy, scale=sm[:, 2:3]), reads=['ps3', K('sm2')], writes=[K('kbg')])
                        sc.op('act', lambda e: e.activation(out=bb['kdec'][:], in_=pk[:, 0:128], func=AF.Copy, scale=sm[:, 3:4]), reads=['ps3', K('sm3')], writes=[K('kdec')])
                        sc.op('act', lambda e: e.activation(out=bb['vb'][:], in_=pk[:, 128:256], func=AF.Copy, scale=bcol),
                              reads=['ps3', 'gbt'], writes=[K('vb')])
                        if G3_LEVEL < 3:
                            continue
                        P_, Q_ = bb['Pa'], bb['Qa']
                        Pn, Qn = bb['Pb'], bb['Qb']
                        kP, kQ, kPn, kQn = K('Pa'), K('Qa'), K('Pb'), K('Qb')
                        for lev in range(6):
                            lastl = (lev == 5)
                            sc.op('pe', lambda e: e.matmul(q(4, 0), lhsT=P_[:], rhs=Q_[:], start=True, stop=True), reads=[kP, kQ], writes=['ps4'])
                            sc.op('act', lambda e: e.activation(out=Qn[:], in_=q(4, 0), func=AF.Copy), reads=['ps4'], writes=[kQn])
                            if not lastl:
                                sc.op('pe', lambda e: e.matmul(q(2, 0), lhsT=Q_[:], rhs=P_[:], start=True, stop=True), reads=[kP, kQ], writes=['ps2'])
                                sc.op('act', lambda e: e.activation(out=Pn[:], in_=q(2, 0), func=AF.Copy), reads=['ps2'], writes=[kPn])
                            sc.op('pe', lambda e: e.matmul(q(5, 0), lhsT=Qn[:], rhs=bb['R'][:], start=True, stop=True), reads=[kQn, K('R')], writes=['ps5'])
                            sc.op('dve', lambda e: e.tensor_tensor(out=bb['R'][:], in0=bb['R'][:], in1=q(5, 0), op=ALU.add), reads=['ps5', K('R')], writes=[K('R')])
                            P_, Pn = Pn, P_
                            Q_, Qn = Qn, Q_
                            kP, kPn = kPn, kP
                            kQ, kQn = kQn, kQ
                        if G3_LEVEL < 4:
                            continue
                        sc.op('act', lambda e: e.activation(out=bb['Rb'][:], in_=bb['R'][:], func=AF.Copy), reads=[K('R')], writes=[K('Rb')])
                        sc.op('pe', lambda e: e.matmul(q(5, 1), lhsT=bb['Rb'][:], rhs=bb['vb'][:], start=True, stop=True), reads=[K('Rb'), K('vb')], writes=['ps5'])
                        sc.op('pe', lambda e: e.matmul(q(5, 2), lhsT=bb['kbg'][:], rhs=bb['Rb'][:], start=True, stop=True), reads=[K('Rb'), K('kbg')], writes=['ps5'])
                        sc.op('act', lambda e: e.activation(out=bb['u'][:], in_=q(5, 1), func=AF.Copy), reads=['ps5'], writes=[K('u')])
                        sc.op('act', lambda e: e.activation(out=bb['wT'][:], in_=q(5, 2), func=AF.Copy), reads=['ps5'], writes=[K('wT')])
                        sc.op('pe', lambda e: e.matmul(q(6, 0), lhsT=bb['wT'][:], rhs=Sbf[h][:], start=True, stop=True), reads=[K('wT'), f'gSb{h}'], writes=['ps6'])
                        sc.op('pe', lambda e: e.matmul(q(6, 1), lhsT=qT, rhs=Sbf[h][:], start=True, stop=True), reads=[kq, f'gSb{h}'], writes=['ps6'])
                        sc.op('dve', lambda e: e.tensor_tensor(out=bb['vnb'][:], in0=bb['u'][:], in1=q(6, 0), op=ALU.subtract), reads=['ps6', K('u')], writes=[K('vnb')])
                        sc.op('pe', lambda e: e.matmul(q(7, 0), lhsT=bb['AT'][:], rhs=bb['vnb'][:], start=True, stop=True), reads=[K('AT'), K('vnb')], writes=['ps7'])
                        sc.op('pe', lambda e: e.matmul(q(7, 1), lhsT=bb['kdec'][:], rhs=bb['vnb'][:], start=True, stop=True), reads=[K('kdec'), K('vnb')], writes=['ps7'])
                        sc.op('dve', lambda e: e.tensor_scalar(out=bb['o'][:], in0=q(6, 1), scalar1=sm[:, 1:2], scalar2=None, op0=ALU.mult),
                              reads=['ps6', K('sm1')], writes=[K('o')])
                        sc.op('dve', lambda e: e.tensor_tensor(out=bb['o'][:], in0=bb['o'][:], in1=q(7, 0), op=ALU.add), reads=['ps7', K('o')], writes=[K('o')])
                        sc.op('dve', lambda e: e.scalar_tensor_tensor(out=Sst[h][:], in0=Sst[h][:], scalar=sm[:, 4:5], in1=q(7, 1), op0=ALU.mult, op1=ALU.add),
                              reads=['ps7', K('sm4'), f'gS{h}'], writes=[f'gS{h}'])
                        sc.op('act', lambda e: e.activation(out=Sbf[h][:], in_=Sst[h][:], func=AF.Copy), reads=[f'gS{h}'], writes=[f'gSb{h}'])
                        if G3_LEVEL < 5:
                            continue
                        sc.op('act', lambda e: e.activation(out=bb['o2'][:], in_=bb['o'][:], func=AF.Square, accum_out=sm[:, 5:6]),
                              reads=[K('o')], writes=[K('o2'), K('sm5')])
                        sc.op('act', lambda e: e.activation(out=sm[:, 6:7], in_=sm[:, 5:6], func=AF.Sqrt, bias=epsb[:, 0:1], scale=1.0 / 128),
                              reads=[K('sm5'), 'gepsb'], writes=[K('sm6')])
                        sc.op('dve', lambda e: e.reciprocal(out=sm[:, 7:8], in_=sm[:, 6:7]), reads=[K('sm6')], writes=[K('sm7')])
                        sc.op('dve', lambda e: e.scalar_tensor_tensor(out=bb['o2'][:], in0=bb['o'][:], scalar=sm[:, 7:8], in1=NG[:], op0=ALU.mult, op1=ALU.mult),
                              reads=[K('o'), K('sm7'), 'gNG', K('o2')], writes=[K('o2')])
                        zs = Xb[12 + t // 4][:, (t % 4) * 512 + h * 128:(t % 4) * 512 + (h + 1) * 128]
                        sc.op('pool', lambda e: e.tensor_tensor(out=O[:, t, 512 + h * 128:512 + (h + 1) * 128], in0=bb['o2'][:], in1=zs, op=ALU.mult),
                              reads=[K('o2'), f'X{12 + t // 4}'], writes=[f'O{t}'])
                sc.barrier()


_PLAN = FULL_PLAN
POOL_ELEM = 'pool'
DEBUG = False
HYB_DSA = True
HYB_GDN = True
GDN_STOP = 99
GDN_NT = NT
G3_LEVEL = 5
_LAST = None
_NINS = 0


def kernel(**inputs):
    n = 8
    bld = Builder(_PLAN)
    nc = bld.build()
    global _NINS
    _NINS = bld.sc.n_ins
    wviews = {}
    for name, (key, idx, cols) in bld.wsrc.items():
        a = np.asarray(inputs[key])[idx]
        if cols is not None:
            a = a[..., cols[0]:cols[1]]
        wviews[name] = np.ascontiguousarray(a).reshape(bld.win[name].shape)
    in_maps = []
    for i in range(n):
        m = {}
        for k in ("x", "c", "positions"):
            m[k] = np.ascontiguousarray(np.asarray(inputs[k])[i * NB:(i + 1) * NB])
        for name, arr in wviews.items():
            m[name] = arr
        in_maps.append(m)
    res = run_bass_kernel_spmd(nc, in_maps, core_ids=list(range(n)))
    global _LAST
    _LAST = res
    return np.concatenate([r["out"] for r in res.results], axis=0)
```

```python
import numpy as np
from contextlib import ExitStack
import concourse.bass as bass
import concourse.mybir as mybir
from concourse.bass_utils import run_bass_kernel_spmd

F32 = mybir.dt.float32
BF16 = mybir.dt.bfloat16
I32 = mybir.dt.int32
AF = mybir.ActivationFunctionType
ALU = mybir.AluOpType
AX = mybir.AxisListType

D = 1024
S = 2048
DFF = 2816
NT = S // 128
DEPTH = 4
ALPHA = float((2.0 * DEPTH) ** 0.25)
LN_EPS = 1e-5
RMS_EPS = 1e-6
NB = 2

FULL_PLAN = [(l, s) for l in range(DEPTH) for s in range(3)]


class Sched:
    NDMA = 32

    def __init__(self, nc, es):
        self.nc = nc
        self.es = es
        self.eng = {'pe': nc.tensor, 'act': nc.scalar, 'dve': nc.vector, 'pool': nc.gpsimd, 'sp': nc.sync}
        self.sem = {}
        self.val = {}
        self.waited = {e: {} for e in self.eng}
        for e in self.eng:
            self._mksem(e)
        self.dsem = [f'd{i}' for i in range(self.NDMA)]
        for d in self.dsem:
            self._mksem(d)
        self.qpool = {'sp': self.dsem[:12], 'pool': self.dsem[12:]}
        self.rr = {'sp': 0, 'pool': 0}
        self.lastw = {}
        self.readers = {}
        self.n_ins = 0

    def _mksem(self, name):
        self.sem[name] = self.es.enter_context(self.nc.semaphore('s_' + name))
        self.val[name] = 0

    def _deps(self, reads, writes):
        deps = {}

        def add(evs):
            for s, v in evs.items():
                if deps.get(s, 0) < v:
                    deps[s] = v
        for k in reads:
            add(self.lastw.get(k, {}))
        for k in writes:
            add(self.lastw.get(k, {}))
            add(self.readers.get(k, {}))
        return deps

    def _wait(self, e, deps):
        for s, v in deps.items():
            if s == e and e == 'pe':
                continue
            if self.waited[e].get(s, 0) >= v:
                continue
            self.eng[e].wait_ge(self.sem[s], v)
            self.waited[e][s] = v
            self.n_ins += 1

    def op(self, e, fn, reads=(), writes=()):
        self._wait(e, self._deps(reads, writes))
        ins = fn(self.eng[e])
        self.val[e] += 1
        v = self.val[e]
        ins.then_inc(self.sem[e], 1)
        self.n_ins += 1
        for k in reads:
            r = self.readers.setdefault(k, {})
            if r.get(e, 0) < v:
                r[e] = v
        for k in writes:
            self.lastw[k] = {e: v}
            self.readers[k] = {}

    def dma(self, q, out, in_, reads=(), writes=(), chan=None, **kw):
        pool_ = self.qpool[q]
        s = pool_[self.rr[q] % len(pool_)]
        self.rr[q] += 1
        deps = self._deps(reads, writes)
        if self.val[s] > 0:
            deps[s] = self.val[s]
        self._wait(q, deps)
        ins = self.eng[q].dma_start(out=out, in_=in_, **kw)
        self.val[s] += 16
        v = self.val[s]
        ins.then_inc(self.sem[s], 16)
        self.n_ins += 1
        for k in reads:
            self.readers.setdefault(k, {})[s] = v
        for k in writes:
            prev = self.lastw.get(k, {})
            keep = {a: b for a, b in prev.items() if a in self.val and a.startswith('d') and a[1:].isdigit()}
            keep[s] = v
            self.lastw[k] = keep
            self.readers[k] = {}

    def barrier(self):
        for e in self.eng:
            for s, v in self.val.items():
                if (s == e and e == 'pe') or v == 0:
                    continue
                if self.waited[e].get(s, 0) >= v:
                    continue
                self.eng[e].wait_ge(self.sem[s], v)
                self.waited[e][s] = v
        self.lastw.clear()
        self.readers.clear()

    def wait_all_dma(self, e):
        for s in self.dsem:
            v = self.val[s]
            if v > 0 and self.waited[e].get(s, 0) < v:
                self.eng[e].wait_ge(self.sem[s], v)
                self.waited[e][s] = v


class Builder:
    def __init__(self, plan):
        self.plan = plan
        self.nc = bass.Bass("TRN2", target_bir_lowering=False)
        nc = self.nc
        di = lambda n, sh, dt=F32: nc.dram_tensor(n, sh, dt, kind="ExternalInput").ap()
        self.x = di("x", [NB, S, D])
        self.c = di("c", [NB, D])
        self.pos = di("positions", [NB, S], I32)
        self.win = {}
        self.wsrc = {}
        self.out = nc.dram_tensor("out", [NB, S, D], F32, kind="ExternalOutput").ap()
        self.dbg = nc.dram_tensor("dbg", [128, 4096], F32, kind="ExternalOutput").ap() if DEBUG else None
        self.dumped = set()

    def W(self, key, idx, shape, cols=None):
        name = key + "_" + "_".join(str(i) for i in idx) + ("" if cols is None else f"_c{cols[0]}")
        if name not in self.win:
            self.win[name] = self.nc.dram_tensor(name, list(shape), F32, kind="ExternalInput").ap()
            self.wsrc[name] = (key, tuple(idx), cols)
        return self.win[name]

    def sb(self, es, name, shape, dt):
        self._uid = getattr(self, '_uid', 0) + 1
        return es.enter_context(self.nc.sbuf_tensor(f"{name}_{self._uid}", shape, dt))

    def dump(self, tag, ap, key, col0, shape=None):
        if not DEBUG or tag in self.dumped:
            return
        self.dumped.add(tag)
        n = int(np.prod(ap.shape[1:]))
        dst = self.dbg[:, col0:col0 + n]
        if len(ap.shape) == 3:
            dst = dst.rearrange("p (a b) -> p a b", a=ap.shape[1])
        self.sc.dma('pool', dst, ap, reads=[key] if isinstance(key, str) else key, chan='dbg')

    def build(self):
        nc = self.nc
        with ExitStack() as es:
            self.sc = Sched(nc, es)
            sc = self.sc
            self.psbig = es.enter_context(nc.psum_tensor("psbig", [128, 2048], F32))
            self.ps = [self.psbig[:, i * 512:(i + 1) * 512] for i in range(4)] + \
                      [es.enter_context(nc.psum_tensor(f"ps{i}", [128, 512], F32)) for i in range(4, 8)]
            self.ident_f = self.sb(es, "ident_f", [128, 128], F32)
            self.ident_b = self.sb(es, "ident_b", [128, 128], BF16)
            self.iot = self.sb(es, "iot", [128, 128], I32)
            self.eps_ln = self.sb(es, "eps_ln", [128, 1], F32)
            sc.op('pool', lambda e: e.iota(self.iot[:], pattern=[[1, 128]], base=0, channel_multiplier=-1),
                  writes=['iot'])
            sc.op('dve', lambda e: e.tensor_scalar(out=self.ident_f[:], in0=self.iot[:], scalar1=0.0, scalar2=None,
                                                   op0=ALU.is_equal), reads=['iot'], writes=['ident_f'])
            sc.op('dve', lambda e: e.tensor_copy(out=self.ident_b[:], in_=self.ident_f[:]),
                  reads=['ident_f'], writes=['ident_b'])
            sc.op('dve', lambda e: e.memset(self.eps_ln[:], LN_EPS), writes=['eps_ln'])
            self.X = [self.sb(es, f"X{t}", [128, D], F32) for t in range(NT)]
            self.csrep = self.sb(es, "csrep", [128, 8, 128], BF16)
            self.cs_f = self.sb(es, "cs_f", [128, 8], F32)
            self.cs_b = self.sb(es, "cs_b", [128, 8], BF16)
            for b in range(NB):
                self.run_sequence(b)
            sc.wait_all_dma('sp')
        return nc

    def run_sequence(self, b):
        sc = self.sc
        nc = self.nc
        for t in range(NT):
            sc.dma('sp', self.X[t][:], self.x[b, t * 128:(t + 1) * 128, :], writes=[f'X{t}'], chan='xin')
        sc.dma('sp', self.cs_f[:], self.c[b].rearrange("(k p) -> p k", p=128), writes=['cs_f'], chan='misc',
               allow_slow_non_contiguous=True)
        sc.op('act', lambda e: e.activation(out=self.cs_b[:], in_=self.cs_f[:], func=AF.Silu),
              reads=['cs_f'], writes=['cs_b'])
        sc.op('dve', lambda e: e.tensor_copy(out=self.csrep[:], in_=self.cs_b[:].unsqueeze(2).to_broadcast([128, 8, 128])),
              reads=['cs_b'], writes=['csrep'])
        for (l, s) in self.plan:
            if s in (0, 2):
                self.ffn_sublayer(b, l, s)
            elif l % 2 == 1:
                self.mla_sublayer(b, l)
            else:
                self.hyb_sublayer(b, l)
        for t in range(NT):
            sc.dma('sp', self.out[b, t * 128:(t + 1) * 128, :], self.X[t][:], reads=[f'X{t}'], chan='out')

    def modulation(self, es, b, l, s, gate_mul, which=('SH', 'SC1', 'G1', 'LG', 'LB')):
        sc = self.sc
        mk = lambda n: self.sb(es, n, [128, D], F32) if n in which else None
        SH, SC1, G1, LG, LB = mk('SH'), mk('SC1'), mk('G1'), mk('LG'), mk('LB')
        with ExitStack() as es2:
            self._modulation(es2, b, l, s, gate_mul, which, SH, SC1, G1, LG, LB)
            sc.barrier()
        return SH, SC1, G1, LG, LB

    def _modulation(self, es, b, l, s, gate_mul, which, SH, SC1, G1, LG, LB):
        sc = self.sc
        mw = [self.sb(es, f"mw{i}", [128, 8, 512], BF16) for i in range(2)]
        mb = [self.sb(es, f"mb{i}", [128, 512], F32) for i in range(2)]
        if LG is not None:
            sc.dma('sp', LG[:], self.W('ln_g', (l, s), [1, D]).partition_broadcast(128), writes=['LG'], chan='misc')
        if LB is not None:
            sc.dma('sp', LB[:], self.W('ln_b', (l, s), [1, D]).partition_broadcast(128), writes=['LB'], chan='misc')
        dst = [SH, SC1, G1]
        names = ['SH', 'SC1', 'G1']
        i = 0
        for j in range(3):
            if dst[j] is None:
                continue
            for hf in range(2):
                c0 = j * D + hf * 512
                mwd = self.W('mod_w', (l,), [D, 3 * D], cols=(s * 3 * D, (s + 1) * 3 * D))
                mbd = self.W('mod_b', (l,), [1, 3 * D], cols=(s * 3 * D, (s + 1) * 3 * D))
                sl = i % 2
                sc.dma('pool', mw[sl][:], mwd[:, c0:c0 + 512].rearrange("(k p) n -> p k n", p=128),
                       writes=[f'mw{sl}'], chan=f'mw{sl}')
                sc.dma('sp', mb[sl][:], mbd[:, c0:c0 + 512].partition_broadcast(128),
                       writes=[f'mb{sl}'], chan=f'mb{sl}')
                pb = 6 + sl
                for k in range(8):
                    sc.op('pe', lambda e, k=k: e.matmul(self.ps[pb][:], lhsT=self.csrep[:, k, :], rhs=mw[sl][:, k, :],
                                                        start=(k == 0), stop=(k == 7)),
                          reads=['csrep', f'mw{sl}'], writes=[f'ps{pb}'])
                addc = 0.0 if j == 0 else 1.0
                sc.op('dve', lambda e: e.scalar_tensor_tensor(out=dst[j][:, hf * 512:(hf + 1) * 512], in0=self.ps[pb][:],
                                                              scalar=addc, in1=mb[sl][:], op0=ALU.add, op1=ALU.add),
                      reads=[f'ps{pb}', f'mb{sl}'], writes=[names[j]])
                i += 1
        if gate_mul != 1.0 and G1 is not None:
            sc.op('dve', lambda e: e.tensor_scalar(out=G1[:], in0=G1[:], scalar1=gate_mul, scalar2=None, op0=ALU.mult),
                  reads=['G1'], writes=['G1'])

    def layer_norm_tile(self, t, LG, LB, lnbuf):
        sc = self.sc
        X = self.X[t]
        st, mv, sd, rstd, nb = lnbuf
        k = f'X{t}'
        for hf in range(2):
            sc.op('dve', lambda e, hf=hf: e.bn_stats(out=st[:, hf, :], in_=X[:, hf * 512:(hf + 1) * 512]),
                  reads=[k], writes=['ln_st'])
        sc.op('dve', lambda e: e.bn_aggr(out=mv[:], in_=st[:].rearrange("p a b -> p (a b)")), reads=['ln_st'], writes=['ln_mv'])
        sc.op('act', lambda e: e.activation(out=sd[:], in_=mv[:, 1:2], func=AF.Sqrt, bias=self.eps_ln[:], scale=1.0),
              reads=['ln_mv', 'eps_ln'], writes=['ln_sd'])
        sc.op('dve', lambda e: e.reciprocal(out=rstd[:], in_=sd[:]), reads=['ln_sd'], writes=['ln_rstd'])
        sc.op('dve', lambda e: e.tensor_scalar(out=nb[:], in0=mv[:, 0:1], scalar1=rstd[:], scalar2=-1.0,
                                               op0=ALU.mult, op1=ALU.mult), reads=['ln_mv', 'ln_rstd'], writes=['ln_nb'])
        sc.op('act', lambda e: e.activation(out=X[:], in_=X[:], func=AF.Identity, bias=nb[:], scale=rstd[:]),
              reads=[k, 'ln_nb', 'ln_rstd'], writes=[k])
        sc.op('dve', lambda e: e.tensor_tensor(out=X[:], in0=X[:], in1=LG[:], op=ALU.mult), reads=[k, 'LG'], writes=[k])
        sc.op(POOL_ELEM, lambda e: e.tensor_tensor(out=X[:], in0=X[:], in1=LB[:], op=ALU.add), reads=[k, 'LB'], writes=[k])

    def ln_bufs(self, es):
        return (self.sb(es, "ln_st", [128, 2, 6], F32), self.sb(es, "ln_mv", [128, 2], F32),
                self.sb(es, "ln_sd", [128, 1], F32), self.sb(es, "ln_rstd", [128, 1], F32),
                self.sb(es, "ln_nb", [128, 1], F32))

    def mod_transpose_tile(self, t, SC1, SH, utmp, ub, dstT, dst_key, col0, pbank):
        sc = self.sc
        sl = t % 2
        sc.op('dve', lambda e: e.tensor_tensor(out=utmp[sl][:], in0=self.X[t][:], in1=SC1[:], op=ALU.mult),
              reads=[f'X{t}', 'SC1'], writes=[f'utmp{sl}'])
        sc.op(POOL_ELEM, lambda e: e.tensor_tensor(out=ub[sl][:], in0=utmp[sl][:], in1=SH[:], op=ALU.add),
              reads=[f'utmp{sl}', 'SH'], writes=[f'ub{sl}'])
        pv = self.ps[pbank][:].bitcast(BF16)
        for k in range(8):
            sc.op('pe', lambda e, k=k: e.transpose(out=pv[:, k * 128:(k + 1) * 128], in_=ub[sl][:, k * 128:(k + 1) * 128],
                                                   identity=self.ident_b[:]),
                  reads=[f'ub{sl}', 'ident_b'], writes=[f'ps{pbank}'])
        sc.op('act', lambda e: e.activation(out=dstT[:, :, col0:col0 + 128], in_=pv.rearrange("p (k n) -> p k n", k=8),
                                            func=AF.Copy),
              reads=[f'ps{pbank}'], writes=[dst_key])

    def ffn_sublayer(self, b, l, s):
        sc = self.sc
        fi = 0 if s == 0 else 1
        with ExitStack() as es:
            SH, SC1, G1, LG, LB = self.modulation(es, b, l, s, 0.5)
            lnbuf = self.ln_bufs(es)
            utmp = [self.sb(es, f"utmp{i}", [128, D], F32) for i in range(2)]
            ub = [self.sb(es, f"ub{i}", [128, D], BF16) for i in range(2)]
            uT = self.sb(es, "uT", [128, 8, 1024], BF16)
            hT = self.sb(es, "hT", [128, 12, 1024], BF16)
            wgb = [self.sb(es, f"wgb{i}", [128, 8, 256], BF16) for i in range(2)]
            wub = [self.sb(es, f"wub{i}", [128, 8, 256], BF16) for i in range(2)]
            wdb = [self.sb(es, f"wdb{i}", [128, 2, D], BF16) for i in range(6)]
            sg = [self.sb(es, f"sg{i}", [128, 512], F32) for i in range(2)]
            t1 = [self.sb(es, f"t1_{i}", [128, 512], F32) for i in range(2)]
            cnt = 0
            self.dump('SC1', SC1[:], 'SC1', 0)
            self.dump('G1', G1[:], 'G1', 1024)
            for grp in range(2):
                for tl in range(8):
                    t = grp * 8 + tl
                    self.mod_transpose_tile(t, SC1, SH, utmp, ub, uT, f'uT{tl}', tl * 128, tl % 2)
                self.dump('uT', uT[:, :, 0:128], 'uT0', 2048)
                for ph in range(2):
                    blocks = list(range(0, 6)) if ph == 0 else list(range(6, 11))
                    for bi, blk in enumerate(blocks):
                        sl = cnt % 2
                        cnt += 1
                        c0 = blk * 256
                        sc.dma('pool', wgb[sl][:], self.W('ffn_w_gate', (l, fi), [D, DFF])[:, c0:c0 + 256].rearrange("(k p) n -> p k n", p=128),
                               writes=[f'wgb{sl}'], chan=f'wgb{sl}')
                        sc.dma('pool', wub[sl][:], self.W('ffn_w_up', (l, fi), [D, DFF])[:, c0:c0 + 256].rearrange("(k p) n -> p k n", p=128),
                               writes=[f'wub{sl}'], chan=f'wub{sl}')
                        sc.dma('pool', wdb[bi][:], self.W('ffn_w_down', (l, fi), [DFF, D])[c0:c0 + 256, :].rearrange("(k p) n -> p k n", p=128),
                               writes=[f'wdb{bi}'], chan=f'wdb{bi}')
                        for cc in range(2):
                            fl = bi * 2 + cc
                            for hf in range(2):
                                pg = 2 + hf
                                pu = 4 + hf
                                for k in range(8):
                                    sc.op('pe', lambda e, k=k: e.matmul(self.ps[pg][:], lhsT=wgb[sl][:, k, cc * 128:(cc + 1) * 128],
                                                                        rhs=uT[:, k, hf * 512:(hf + 1) * 512],
                                                                        start=(k == 0), stop=(k == 7)),
                                          reads=[f'wgb{sl}'] + [f'uT{i}' for i in range(hf * 4, hf * 4 + 4)], writes=[f'ps{pg}'])
                                for k in range(8):
                                    sc.op('pe', lambda e, k=k: e.matmul(self.ps[pu][:], lhsT=wub[sl][:, k, cc * 128:(cc + 1) * 128],
                                                                        rhs=uT[:, k, hf * 512:(hf + 1) * 512],
                                                                        start=(k == 0), stop=(k == 7)),
                                          reads=[f'wub{sl}'] + [f'uT{i}' for i in range(hf * 4, hf * 4 + 4)], writes=[f'ps{pu}'])
                                sc.op('act', lambda e: e.activation(out=sg[hf][:], in_=self.ps[pg][:], func=AF.Silu),
                                      reads=[f'ps{pg}'], writes=[f'sg{hf}'])
                                sc.op('dve', lambda e: e.tensor_tensor(out=hT[:, fl, hf * 512:(hf + 1) * 512], in0=sg[hf][:],
                                                                       in1=self.ps[pu][:], op=ALU.mult),
                                      reads=[f'sg{hf}', f'ps{pu}'], writes=[f'hT{fl}_{hf}'])
                    nfl = len(blocks) * 2
                    self.dump('hT', hT[:, 0, 0:512], 'hT0_0', 3072)
                    for tl in range(8):
                        t = grp * 8 + tl
                        for hf in range(2):
                            py = 6 + hf
                            for fl in range(nfl):
                                sc.op('pe', lambda e, fl=fl: e.matmul(self.ps[py][:], lhsT=hT[:, fl, tl * 128:(tl + 1) * 128],
                                                                      rhs=wdb[fl // 2][:, fl % 2, hf * 512:(hf + 1) * 512],
                                                                      start=(fl == 0), stop=(fl == nfl - 1)),
                                      reads=[f'hT{fl}_{tl // 4}', f'wdb{fl // 2}'], writes=[f'ps{py}'])
                            Xs = self.X[t][:, hf * 512:(hf + 1) * 512]
                            sc.op('dve', lambda e: e.tensor_tensor(out=t1[hf][:], in0=self.ps[py][:], in1=G1[:, hf * 512:(hf + 1) * 512],
                                                                   op=ALU.mult),
                                  reads=[f'ps{py}', 'G1'], writes=[f't1_{hf}'])
                            if hf == 0:
                                self.dump('t1', t1[0][:], 't1_0', 3584)
                            if ph == 0:
                                sc.op('dve', lambda e: e.scalar_tensor_tensor(out=Xs, in0=Xs, scalar=ALPHA, in1=t1[hf][:],
                                                                              op0=ALU.mult, op1=ALU.add),
                                      reads=[f'X{t}', f't1_{hf}'], writes=[f'X{t}'])
                            else:
                                sc.op(POOL_ELEM, lambda e: e.tensor_tensor(out=Xs, in0=Xs, in1=t1[hf][:], op=ALU.add),
                                      reads=[f'X{t}', f't1_{hf}'], writes=[f'X{t}'])
                        if ph == 1:
                            self.layer_norm_tile(t, LG, LB, lnbuf)
            sc.barrier()


    def mixer_tail(self, b, l, O, okey, wout_d):
        sc = self.sc
        with ExitStack() as es:
            _, _, G1, LG, LB = self.modulation(es, b, l, 1, 1.0, which=('G1', 'LG', 'LB'))
            lnbuf = self.ln_bufs(es)
            wo = self.sb(es, "wo", [128, 8, D], BF16)
            OT = [self.sb(es, f"OT{i}", [128, 8, 128], BF16) for i in range(2)]
            t1 = [self.sb(es, f"t1_{i}", [128, 512], F32) for i in range(2)]
            for hf in range(2):
                sc.dma('pool', wo[:, :, hf * 512:(hf + 1) * 512],
                       wout_d[:, hf * 512:(hf + 1) * 512].rearrange("(k p) n -> p k n", p=128), writes=[f'wo{hf}'], chan=f'wo{hf}')
            for t in range(NT):
                sl = t % 2
                pb = 4 + sl
                pv = self.ps[pb][:].bitcast(BF16)
                for k in range(8):
                    sc.op('pe', lambda e, k=k: e.transpose(out=pv[:, k * 128:(k + 1) * 128], in_=O[:, t, k * 128:(k + 1) * 128],
                                                           identity=self.ident_b[:]),
                          reads=[okey(t), 'ident_b'], writes=[f'ps{pb}'])
                sc.op('act', lambda e: e.activation(out=OT[sl][:], in_=pv.rearrange("p (k n) -> p k n", k=8), func=AF.Copy),
                      reads=[f'ps{pb}'], writes=[f'OT{sl}'])
                for hf in range(2):
                    py = 6 + hf
                    for k in range(8):
                        sc.op('pe', lambda e, k=k: e.matmul(self.ps[py][:], lhsT=OT[sl][:, k, :], rhs=wo[:, k, hf * 512:(hf + 1) * 512],
                                                            start=(k == 0), stop=(k == 7)),
                              reads=[f'OT{sl}', f'wo{hf}'], writes=[f'ps{py}'])
                    Xs = self.X[t][:, hf * 512:(hf + 1) * 512]
                    sc.op('dve', lambda e: e.tensor_tensor(out=t1[hf][:], in0=self.ps[py][:], in1=G1[:, hf * 512:(hf + 1) * 512], op=ALU.mult),
                          reads=[f'ps{py}', 'G1'], writes=[f't1_{hf}'])
                    sc.op('dve', lambda e: e.scalar_tensor_tensor(out=Xs, in0=Xs, scalar=ALPHA, in1=t1[hf][:], op0=ALU.mult, op1=ALU.add),
                          reads=[f'X{t}', f't1_{hf}'], writes=[f'X{t}'])
                self.layer_norm_tile(t, LG, LB, lnbuf)
            sc.barrier()

    def attn_tile(self, qT_ap, kT, kT_key, V, V_key, KV, scale, out_ap, out_key, bufs, maskblk, q_keys, bias=None, bias_key=None,
                  v_of=None, dv=None):
        sc = self.sc
        P, PT, st = bufs
        if v_of is None:
            dv = V.shape[2]
            v_of = lambda blk: (V[:, blk, :], V_key)
        Sp = self.psbig
        PSK = ['psS', 'ps0', 'ps1', 'ps2', 'ps3']
        nkc = (KV + 511) // 512
        for kc in range(nkc):
            c0 = kc * 512
            c1 = min(KV, c0 + 512)
            last = (kc == nkc - 1)
            sc.op('pe', lambda e: e.matmul(Sp[:, c0:c1], lhsT=qT_ap, rhs=kT[:, c0:c1], start=True,
                                           stop=(not last) or (maskblk is None)),
                  reads=list(q_keys) + [kT_key], writes=PSK)
        if maskblk is not None:
            sc.op('pe', lambda e: e.matmul(Sp[:, KV - 128:KV], lhsT=self.ident_b[:], rhs=maskblk[:], start=False, stop=True),
                  reads=['ident_b', 'maskblk'], writes=PSK)
        src = Sp[:, 0:KV]
        skeys = PSK
        if bias is not None:
            sc.op('dve', lambda e: e.tensor_tensor(out=bias[:, 0:KV], in0=Sp[:, 0:KV], in1=bias[:, 0:KV], op=ALU.add),
                  reads=PSK + [bias_key], writes=[bias_key])
            src = bias[:, 0:KV]
            skeys = [bias_key]
        sc.op('dve', lambda e: e.tensor_reduce(out=st[:, 0:1], in_=src, axis=AX.X, op=ALU.max), reads=skeys, writes=['at_m'])
        sc.op('dve', lambda e: e.tensor_scalar(out=st[:, 1:2], in0=st[:, 0:1], scalar1=-scale, scalar2=None, op0=ALU.mult),
              reads=['at_m'], writes=['at_nm'])
        sc.op('act', lambda e: e.activation(out=P[:, 0:KV], in_=src, func=AF.Exp, bias=st[:, 1:2], scale=scale, accum_out=st[:, 2:3]),
              reads=skeys + ['at_nm'], writes=['at_P', 'at_sum'])
        sc.op('dve', lambda e: e.reciprocal(out=st[:, 3:4], in_=st[:, 2:3]), reads=['at_sum'], writes=['at_rinv'])
        nblk = KV // 128
        for g in range((nblk + 7) // 8):
            pb = 4 + (g % 2)
            pv = self.ps[pb][:].bitcast(BF16)
            nb_ = min(8, nblk - g * 8)
            for j in range(nb_):
                blk = g * 8 + j
                sc.op('pe', lambda e, j=j, blk=blk: e.transpose(out=pv[:, j * 128:(j + 1) * 128], in_=P[:, blk * 128:(blk + 1) * 128],
                                                                identity=self.ident_b[:]),
                      reads=['at_P', 'ident_b'], writes=[f'ps{pb}'])
            eng = 'act' if g % 2 == 0 else 'dve'
            if eng == 'act':
                sc.op('act', lambda e: e.activation(out=PT[:, g * 8:g * 8 + nb_, :], in_=pv[:, 0:nb_ * 128].rearrange("p (k n) -> p k n", n=128),
                                                    func=AF.Copy), reads=[f'ps{pb}'], writes=[f'at_PT{g}'])
            else:
                sc.op('dve', lambda e: e.tensor_copy(out=PT[:, g * 8:g * 8 + nb_, :], in_=pv[:, 0:nb_ * 128].rearrange("p (k n) -> p k n", n=128)),
                      reads=[f'ps{pb}'], writes=[f'at_PT{g}'])
        po = self.ps[6]
        for blk in range(nblk):
            vap, vkey = v_of(blk)
            sc.op('pe', lambda e, blk=blk: e.matmul(po[:, 0:dv], lhsT=PT[:, blk, :], rhs=vap, start=(blk == 0), stop=(blk == nblk - 1)),
                  reads=[f'at_PT{blk // 8}', vkey], writes=['ps6'])
        sc.op('act', lambda e: e.activation(out=out_ap, in_=po[:, 0:dv], func=AF.Copy, scale=st[:, 3:4]),
              reads=['ps6', 'at_rinv'], writes=[out_key])

    def make_maskblk(self, es):
        sc = self.sc
        mi = self.sb(es, "mask_i", [128, 128], I32)
        mf = self.sb(es, "mask_f", [128, 128], F32)
        mb = self.sb(es, "maskblk", [128, 128], BF16)
        sc.op('pool', lambda e: e.iota(mi[:], pattern=[[1, 128]], base=0, channel_multiplier=0), writes=['mask_i'])
        sc.op('dve', lambda e: e.tensor_scalar(out=mf[:], in0=mi[:], scalar1=64.0, scalar2=None, op0=ALU.is_ge),
              reads=['mask_i'], writes=['mask_f'])
        sc.op('dve', lambda e: e.memset(mf[64:128, :], 0.0), reads=['mask_f'], writes=['mask_f'])
        sc.op('dve', lambda e: e.tensor_scalar(out=mb[:], in0=mf[:], scalar1=-1e30, scalar2=None, op0=ALU.mult),
              reads=['mask_f'], writes=['maskblk'])
        return mb

    def rope_tables(self, es, b):
        sc = self.sc
        COS = self.sb(es, "COS", [32, S], F32)
        SINS = self.sb(es, "SINS", [32, S], F32)
        with ExitStack() as es2:
            pi_ = self.sb(es2, "rp_pi", [32, S], I32)
            ang = self.sb(es2, "rp_ang", [32, S], F32)
            kf = self.sb(es2, "rp_kf", [32, S], F32)
            ki = self.sb(es2, "rp_ki", [32, S], I32)
            r = self.sb(es2, "rp_r", [32, S], F32)
            m = self.sb(es2, "rp_m", [32, S], F32)
            fi = self.sb(es2, "rp_fi", [32, 1], I32)
            ff = self.sb(es2, "rp_ff", [32, 4], F32)
            sc.dma('sp', pi_[:], self.pos[b:b + 1, :].partition_broadcast(32), writes=['rp_pi'], chan='misc')
            sc.op('pool', lambda e: e.iota(fi[:], pattern=[[0, 1]], base=0, channel_multiplier=1), writes=['rp_fi'])
            sc.op('dve', lambda e: e.tensor_copy(out=ff[:, 0:1], in_=fi[:]), reads=['rp_fi'], writes=['rp_ff'])
            sc.op('dve', lambda e: e.tensor_scalar(out=ff[:, 1:2], in0=ff[:, 0:1], scalar1=16.0, scalar2=-16.0, op0=ALU.is_ge, op1=ALU.mult),
                  reads=['rp_ff'], writes=['rp_ff'])
            sc.op('dve', lambda e: e.tensor_tensor(out=ff[:, 2:3], in0=ff[:, 0:1], in1=ff[:, 1:2], op=ALU.add), reads=['rp_ff'], writes=['rp_ff'])
            sc.op('act', lambda e: e.activation(out=ff[:, 3:4], in_=ff[:, 2:3], func=AF.Exp, scale=-float(np.log(10000.0)) / 16.0),
                  reads=['rp_ff'], writes=['rp_ff'])
            sc.op('dve', lambda e: e.tensor_copy(out=ang[:], in_=pi_[:]), reads=['rp_pi'], writes=['rp_ang'])
            sc.op('dve', lambda e: e.tensor_scalar(out=ang[:], in0=ang[:], scalar1=ff[:, 3:4], scalar2=None, op0=ALU.mult),
                  reads=['rp_ang', 'rp_ff'], writes=['rp_ang'])
            TWO_PI = float(2 * np.pi)
            C1 = 6.28125
            C2 = float(2 * np.pi - 6.28125)
            sc.op('dve', lambda e: e.tensor_scalar(out=kf[:], in0=ang[:], scalar1=1.0 / TWO_PI, scalar2=None, op0=ALU.mult),
                  reads=['rp_ang'], writes=['rp_kf'])
            sc.op('dve', lambda e: e.tensor_copy(out=ki[:], in_=kf[:]), reads=['rp_kf'], writes=['rp_ki'])
            sc.op('dve', lambda e: e.tensor_copy(out=kf[:], in_=ki[:]), reads=['rp_ki'], writes=['rp_kf'])
            sc.op('dve', lambda e: e.scalar_tensor_tensor(out=r[:], in0=kf[:], scalar=-C1, in1=ang[:], op0=ALU.mult, op1=ALU.add),
                  reads=['rp_kf', 'rp_ang'], writes=['rp_r'])
            sc.op('dve', lambda e: e.scalar_tensor_tensor(out=r[:], in0=kf[:], scalar=-C2, in1=r[:], op0=ALU.mult, op1=ALU.add),
                  reads=['rp_kf', 'rp_r'], writes=['rp_r'])

            def wrap(buf, key):
                sc.op('dve', lambda e: e.tensor_scalar(out=m[:], in0=buf[:], scalar1=float(np.pi), scalar2=-TWO_PI, op0=ALU.is_gt, op1=ALU.mult),
                      reads=[key], writes=['rp_m'])
                sc.op('dve', lambda e: e.tensor_tensor(out=buf[:], in0=buf[:], in1=m[:], op=ALU.add), reads=[key, 'rp_m'], writes=[key])
                sc.op('dve', lambda e: e.tensor_scalar(out=m[:], in0=buf[:], scalar1=-float(np.pi), scalar2=TWO_PI, op0=ALU.is_lt, op1=ALU.mult),
                      reads=[key], writes=['rp_m'])
                sc.op('dve', lambda e: e.tensor_tensor(out=buf[:], in0=buf[:], in1=m[:], op=ALU.add), reads=[key, 'rp_m'], writes=[key])
            wrap(r, 'rp_r')
            sc.op('act', lambda e: e.activation(out=SINS[:], in_=r[:], func=AF.Sin), reads=['rp_r'], writes=['SINS'])
            sc.op('dve', lambda e: e.tensor_scalar(out=ff[:, 1:2], in0=ff[:, 0:1], scalar1=16.0, scalar2=2.0, op0=ALU.is_ge, op1=ALU.mult),
                  reads=['rp_ff'], writes=['rp_ff'])
            sc.op('dve', lambda e: e.tensor_scalar(out=ff[:, 1:2], in0=ff[:, 1:2], scalar1=-1.0, scalar2=None, op0=ALU.add),
                  reads=['rp_ff'], writes=['rp_ff'])
            sc.op('dve', lambda e: e.tensor_scalar(out=SINS[:], in0=SINS[:], scalar1=ff[:, 1:2], scalar2=None, op0=ALU.mult),
                  reads=['SINS', 'rp_ff'], writes=['SINS'])
            sc.op('dve', lambda e: e.tensor_scalar(out=r[:], in0=r[:], scalar1=float(np.pi / 2), scalar2=None, op0=ALU.add),
                  reads=['rp_r'], writes=['rp_r'])
            wrap(r, 'rp_r')
            sc.op('act', lambda e: e.activation(out=COS[:], in_=r[:], func=AF.Sin), reads=['rp_r'], writes=['COS'])
            sc.barrier()
        return COS, SINS

    def mla_sublayer(self, b, l):
        sc = self.sc
        o = l // 2
        HD = 96
        scale = float(HD ** -0.5)
        with ExitStack() as esO:
            O = self.sb(esO, "O", [128, NT, D], BF16)
            with ExitStack() as es:
                cnT = self.sb(es, "cnT", [128, 5, S], BF16)
                krT = self.sb(es, "krT", [32, S], BF16)
                COS, SINS = self.rope_tables(es, b)
                with ExitStack() as esA:
                    SH, SC1, _, _, _ = self.modulation(esA, b, l, 1, 1.0, which=('SH', 'SC1'))
                    utmp = [self.sb(esA, f"utmp{i}", [128, D], F32) for i in range(2)]
                    ub = [self.sb(esA, f"ub{i}", [128, D], BF16) for i in range(2)]
                    uT4 = self.sb(esA, "uT4", [128, 8, 512], BF16)
                    win = self.sb(esA, "win", [128, 8, 672], BF16)
                    winsw = self.sb(esA, "winsw", [128, 8, 32], BF16)
                    GQ = self.sb(esA, "GQ", [128, 640], F32)
                    cn = [self.sb(esA, f"cn{i}", [128, 640], BF16) for i in range(2)]
                    junk = self.sb(esA, "junk", [128, 384], F32)
                    rs = self.sb(esA, "rs", [128, 8], F32)
                    eps_r = self.sb(esA, "eps_r", [128, 1], F32)
                    rt = [self.sb(esA, f"rt{i}", [32, 512], F32) for i in range(2)]
                    wd_ = self.W('mla_w_in', (o,), [D, 672])
                    sc.dma('pool', win[:], wd_.rearrange("(k p) n -> p k n", p=128), writes=['win'], chan='win')
                    sc.dma('pool', winsw[:, :, 0:16], wd_[:, 656:672].rearrange("(k p) n -> p k n", p=128), writes=['winsw'], chan='win')
                    sc.dma('pool', winsw[:, :, 16:32], wd_[:, 640:656].rearrange("(k p) n -> p k n", p=128), writes=['winsw'], chan='win')
                    sc.dma('sp', GQ[:, 0:384], self.W('mla_q_norm_g', (o,), [1, 384]).partition_broadcast(128), writes=['GQ'], chan='misc')
                    sc.dma('sp', GQ[:, 384:640], self.W('mla_kv_norm_g', (o,), [1, 256]).partition_broadcast(128), writes=['GQ'], chan='misc')
                    sc.op('dve', lambda e: e.memset(eps_r[:], RMS_EPS), writes=['eps_r'])
                    for g4 in range(4):
                        for tl in range(4):
                            t = g4 * 4 + tl
                            self.mod_transpose_tile(t, SC1, SH, utmp, ub, uT4, f'uT{tl}', tl * 128, 4 + t % 2)
                            for (c0, c1, pb) in ((0, 512, 0), (512, 640, 1)):
                                for k in range(8):
                                    sc.op('pe', lambda e, k=k: e.matmul(self.ps[pb][:, 0:c1 - c0], lhsT=uT4[:, k, tl * 128:(tl + 1) * 128],
                                                                        rhs=win[:, k, c0:c1], start=(k == 0), stop=(k == 7)),
                                          reads=[f'uT{tl}', 'win'], writes=[f'ps{pb}'])
                            cps = self.psbig[:, 0:640]
                            sc.op('act', lambda e: e.activation(out=junk[:, 0:384], in_=cps[:, 0:384], func=AF.Square, accum_out=rs[:, 0:1]),
                                  reads=['ps0'], writes=['junk', 'rs0'])
                            sc.op('act', lambda e: e.activation(out=junk[:, 0:256], in_=cps[:, 384:640], func=AF.Square, accum_out=rs[:, 1:2]),
                                  reads=['ps0', 'ps1'], writes=['junk', 'rs1'])
                            sc.op('act', lambda e: e.activation(out=rs[:, 2:3], in_=rs[:, 0:1], func=AF.Sqrt, bias=eps_r[:], scale=1.0 / 384),
                                  reads=['rs0', 'eps_r'], writes=['rs2'])
                            sc.op('act', lambda e: e.activation(out=rs[:, 3:4], in_=rs[:, 1:2], func=AF.Sqrt, bias=eps_r[:], scale=1.0 / 256),
                                  reads=['rs1', 'eps_r'], writes=['rs3'])
                            sc.op('dve', lambda e: e.reciprocal(out=rs[:, 4:6], in_=rs[:, 2:4]), reads=['rs2', 'rs3'], writes=['rs4'])
                            sl = t % 2
                            sc.op('dve', lambda e: e.scalar_tensor_tensor(out=cn[sl][:, 0:384], in0=cps[:, 0:384], scalar=rs[:, 4:5], in1=GQ[:, 0:384],
                                                                          op0=ALU.mult, op1=ALU.mult),
                                  reads=['ps0', 'rs4', 'GQ'], writes=[f'cn{sl}'])
                            sc.op('dve', lambda e: e.scalar_tensor_tensor(out=cn[sl][:, 384:640], in0=cps[:, 384:640], scalar=rs[:, 5:6], in1=GQ[:, 384:640],
                                                                          op0=ALU.mult, op1=ALU.mult),
                                  reads=['ps0', 'ps1', 'rs4', 'GQ'], writes=[f'cn{sl}'])
                            pb = 2 + sl
                            pv = self.ps[pb][:].bitcast(BF16)
                            for k in range(5):
                                sc.op('pe', lambda e, k=k: e.transpose(out=pv[:, k * 128:(k + 1) * 128], in_=cn[sl][:, k * 128:(k + 1) * 128],
                                                                       identity=self.ident_b[:]),
                                      reads=[f'cn{sl}', 'ident_b'], writes=[f'ps{pb}'])
                            sc.op('act', lambda e: e.activation(out=cnT[:, :, t * 128:(t + 1) * 128],
                                                                in_=pv[:, 0:640].rearrange("p (k n) -> p k n", k=5), func=AF.Copy),
                                  reads=[f'ps{pb}'], writes=[f'cnT{t}'])
                        cols = slice(g4 * 512, (g4 + 1) * 512)
                        for k in range(8):
                            sc.op('pe', lambda e, k=k: e.matmul(self.ps[6][0:32, :], lhsT=win[:, k, 640:672], rhs=uT4[:, k, :],
                                                                start=(k == 0), stop=(k == 7)),
                                  reads=['win'] + [f'uT{i}' for i in range(4)], writes=['ps6'])
                        for k in range(8):
                            sc.op('pe', lambda e, k=k: e.matmul(self.ps[7][0:32, :], lhsT=winsw[:, k, :], rhs=uT4[:, k, :],
                                                                start=(k == 0), stop=(k == 7)),
                                  reads=['winsw'] + [f'uT{i}' for i in range(4)], writes=['ps7'])
                        sc.op('dve', lambda e: e.tensor_tensor(out=rt[0][:], in0=self.ps[6][0:32, :], in1=COS[:, cols], op=ALU.mult),
                              reads=['ps6', 'COS'], writes=['rt0'])
                        sc.op('dve', lambda e: e.tensor_tensor(out=rt[1][:], in0=self.ps[7][0:32, :], in1=SINS[:, cols], op=ALU.mult),
                              reads=['ps7', 'SINS'], writes=['rt1'])
                        sc.op('dve', lambda e: e.tensor_tensor(out=krT[:, cols], in0=rt[0][:], in1=rt[1][:], op=ALU.add),
                              reads=['rt0', 'rt1'], writes=['krT'])
                    sc.barrier()
                with ExitStack() as esB:
                    wq = self.sb(esB, "wq", [128, 3, 16, 128], BF16)
                    wkv = self.sb(esB, "wkv", [128, 2, 16, 160], BF16)
                    qT = [self.sb(esB, f"qT{i}", [96, S], BF16) for i in range(2)]
                    kT = [self.sb(esB, f"kT{i}", [96, S], BF16) for i in range(2)]
                    Vh = [self.sb(esB, f"Vh{i}", [128, NT, 64], BF16) for i in range(2)]
                    P = self.sb(esB, "P", [128, S], BF16)
                    PT = self.sb(esB, "PT", [128, 16, 128], BF16)
                    st = self.sb(esB, "at_st", [128, 4], F32)
                    rt = [self.sb(esB, f"rt{i}", [32, 512], F32) for i in range(2)]
                    maskblk = self.make_maskblk(esB)
                    wqd = self.W('mla_w_q_up', (o,), [384, 1536]).rearrange("(k p) (h d) -> p k h d", p=128, d=96)
                    wkvd = self.W('mla_w_kv_up', (o,), [256, 2048]).rearrange("(k p) (h d) -> p k h d", p=128, d=128)
                    for k in range(3):
                        sc.dma('pool', wq[:, k, :, 0:32], wqd[:, k, :, 64:96], writes=['wq'], chan='wq')
                        sc.dma('pool', wq[:, k, :, 32:96], wqd[:, k, :, 0:64], writes=['wq'], chan='wq')
                        sc.dma('pool', wq[:, k, :, 96:112], wqd[:, k, :, 80:96], writes=['wq'], chan='wq')
                        sc.dma('pool', wq[:, k, :, 112:128], wqd[:, k, :, 64:80], writes=['wq'], chan='wq')
                    sc.op('dve', lambda e: e.memset(wkv[:], 0.0), writes=['wkv'])
                    for k in range(2):
                        sc.dma('pool', wkv[:, k, :, 32:160], wkvd[:, k, :, :], writes=['wkv'], chan='wkv')
                    for h in range(16):
                        sl = h % 2
                        for g4 in range(4):
                            cols = slice(g4 * 512, (g4 + 1) * 512)
                            for k in range(3):
                                sc.op('pe', lambda e, k=k: e.matmul(self.ps[7][0:96, :], lhsT=wq[:, k, h, 0:96], rhs=cnT[:, k, cols],
                                                                    start=(k == 0), stop=(k == 2)),
                                      reads=['wq'] + [f'cnT{t}' for t in range(g4 * 4, g4 * 4 + 4)], writes=['ps7'])
                            for k in range(3):
                                sc.op('pe', lambda e, k=k: e.matmul(self.ps[6][0:32, :], lhsT=wq[:, k, h, 96:128], rhs=cnT[:, k, cols],
                                                                    start=(k == 0), stop=(k == 2)),
                                      reads=['wq'] + [f'cnT{t}' for t in range(g4 * 4, g4 * 4 + 4)], writes=['ps6'])
                            sc.op('dve', lambda e: e.tensor_tensor(out=rt[0][:], in0=self.ps[7][0:32, :], in1=COS[:, cols], op=ALU.mult),
                                  reads=['ps7', 'COS'], writes=['rt0'])
                            sc.op('dve', lambda e: e.tensor_tensor(out=rt[1][:], in0=self.ps[6][0:32, :], in1=SINS[:, cols], op=ALU.mult),
                                  reads=['ps6', 'SINS'], writes=['rt1'])
                            sc.op('dve', lambda e: e.tensor_tensor(out=qT[sl][0:32, cols], in0=rt[0][:], in1=rt[1][:], op=ALU.add),
                                  reads=['rt0', 'rt1'], writes=[f'qT{sl}'])
                            for (p0, p1) in ((32, 64), (64, 96)):
                                sc.op('act', lambda e: e.activation(out=qT[sl][p0:p1, cols], in_=self.ps[7][p0:p1, :], func=AF.Copy),
                                      reads=['ps7'], writes=[f'qT{sl}'])
                            for k in range(2):
                                sc.op('pe', lambda e, k=k: e.matmul(self.ps[7][0:96, :], lhsT=wkv[:, k, h, 0:96], rhs=cnT[:, 3 + k, cols],
                                                                    start=(k == 0), stop=(k == 1)),
                                      reads=['wkv'] + [f'cnT{t}' for t in range(g4 * 4, g4 * 4 + 4)], writes=['ps7'])
                            for (p0, p1) in ((32, 64), (64, 96)):
                                sc.op('act', lambda e: e.activation(out=kT[sl][p0:p1, cols], in_=self.ps[7][p0:p1, :], func=AF.Copy),
                                      reads=['ps7'], writes=[f'kT{sl}'])
                        sc.op('pool', lambda e: e.tensor_copy(out=kT[sl][0:32, :], in_=krT[:]), reads=['krT'], writes=[f'kT{sl}'])
                        for g8 in range(2):
                            pb = 4 + g8
                            for tl in range(8):
                                t = g8 * 8 + tl
                                for k in range(2):
                                    sc.op('pe', lambda e, k=k: e.matmul(self.ps[pb][:, tl * 64:(tl + 1) * 64], lhsT=cnT[:, 3 + k, t * 128:(t + 1) * 128],
                                                                        rhs=wkv[:, k, h, 96:160], start=(k == 0), stop=(k == 1)),
                                          reads=['wkv', f'cnT{t}'], writes=[f'ps{pb}'])
                            sc.op('act', lambda e: e.activation(out=Vh[sl][:, g8 * 8:(g8 + 1) * 8, :],
                                                                in_=self.ps[pb][:].rearrange("p (t d) -> p t d", d=64), func=AF.Copy),
                                  reads=[f'ps{pb}'], writes=[f'Vh{sl}'])
                        for qt in range(NT):
                            self.attn_tile(qT[sl][:, qt * 128:(qt + 1) * 128], kT[sl], f'kT{sl}', Vh[sl], f'Vh{sl}', (qt + 1) * 128, scale,
                                           O[:, qt, h * 64:(h + 1) * 64], f'O{qt}', (P, PT, st), maskblk, [f'qT{sl}'])
                    sc.barrier()
            self.mixer_tail(b, l, O, lambda t: f'O{t}', self.W('mla_w_out', (o,), [D, D]))


    def hyb_sublayer(self, b, l):
        sc = self.sc
        e_ = l // 2
        if not hasattr(self, 'xspill'):
            self.xspill = self.nc.dram_tensor("xspill", [S, D], F32, kind="Internal").ap()
        win_d = self.W('hyb_w_in', (e_,), [D, 4176])
        with ExitStack() as esO:
            O = self.sb(esO, "O", [128, NT, D], BF16)
            uT = self.sb(esO, "uT_all", [128, 8, S], BF16)
            with ExitStack() as esA:
                SH, SC1, _, _, _ = self.modulation(esA, b, l, 1, 1.0, which=('SH', 'SC1'))
                utmp = [self.sb(esA, f"utmp{i}", [128, D], F32) for i in range(2)]
                ub = [self.sb(esA, f"ub{i}", [128, D], BF16) for i in range(2)]
                for t in range(NT):
                    self.mod_transpose_tile(t, SC1, SH, utmp, ub, uT, f'uT{t}', t * 128, 4 + t % 2)
                    sc.dma('sp', self.xspill[t * 128:(t + 1) * 128, :], self.X[t][:], reads=[f'X{t}'], writes=[f'xsp{t}'], chan='xsp')
                sc.barrier()
            Xb = [self.X[t][:].bitcast(BF16) for t in range(NT)]
            uTk = [f'uT{t}' for t in range(NT)]
            if HYB_DSA:
                self.dsa_part(b, l, e_, win_d, O, uT, Xb, uTk)
            else:
                for t in range(NT):
                    sc.op('dve', lambda e: e.memset(O[:, t, 0:512], 0.0), writes=[f'O{t}'])
            if HYB_GDN:
                self.gdn_part(b, l, e_, win_d, O, uT, Xb, uTk)
            else:
                for t in range(NT):
                    sc.op('dve', lambda e: e.memset(O[:, t, 512:1024], 0.0), writes=[f'O{t}'])
            sc.barrier()
            for t in range(NT):
                sc.dma('sp', self.X[t][:], self.xspill[t * 128:(t + 1) * 128, :], writes=[f'X{t}'], chan='xin')
            with ExitStack() as esT:
                pass
            self.mixer_tail(b, l, O, lambda t: f'O{t}', self.W('hyb_w_out', (e_,), [D, D]))

    def proj_featmajor(self, wchunk, wkeys, loads, uT, uTk, dst_of, evac_scale=1.0):
        sc = self.sc
        for ci, (pieces, dst, dkey) in enumerate(loads):
            sl = self._wc % 2
            self._wc += 1
            for (d0, d1, srcap) in pieces:
                sc.dma('pool', wchunk[sl][:, :, d0:d1], srcap.rearrange("(k p) n -> p k n", p=128), writes=[wkeys[sl]], chan=wkeys[sl])
            for g4 in range(4):
                pb = 4 + (g4 % 2)
                for k in range(8):
                    sc.op('pe', lambda e, k=k: e.matmul(self.ps[pb][:], lhsT=wchunk[sl][:, k, :], rhs=uT[:, k, g4 * 512:(g4 + 1) * 512],
                                                        start=(k == 0), stop=(k == 7)),
                          reads=[wkeys[sl]] + uTk[g4 * 4:g4 * 4 + 4], writes=[f'ps{pb}'])
                if g4 % 2 == 0:
                    sc.op('act', lambda e: e.activation(out=dst[:, g4 * 512:(g4 + 1) * 512], in_=self.ps[pb][:], func=AF.Copy, scale=evac_scale),
                          reads=[f'ps{pb}'], writes=[dkey])
                else:
                    sc.op('dve', lambda e: e.tensor_scalar(out=dst[:, g4 * 512:(g4 + 1) * 512], in0=self.ps[pb][:], scalar1=evac_scale, scalar2=None,
                                                           op0=ALU.mult), reads=[f'ps{pb}'], writes=[dkey])

    def dsa_part(self, b, l, e_, win_d, O, uT, Xb, uTk):
        sc = self.sc
        self._wc = 0
        with ExitStack() as es:
            wchunk = [self.sb(es, f"wch{i}", [128, 8, 128], BF16) for i in range(2)]
            wkeys = ['wch0', 'wch1']
            ikT2 = self.sb(es, "ikT2", [128, S], BF16)
            iw = self.sb(es, "iw", [128, NT, 8], F32)
            posrow = self.sb(es, "posrow", [128, S], F32)
            poscol = self.sb(es, "poscol", [128, NT], F32)
            acc = self.sb(es, "acc", [128, S], F32)
            MB = self.sb(es, "MB", [128, S], F32)
            Dt = self.sb(es, "Dt", [128, S], F32)
            bh = [self.sb(es, f"bh{i}", [128, S], F32) for i in range(2)]
            rl = [self.sb(es, f"rl{i}", [128, 512], F32) for i in range(2)]
            P = self.sb(es, "P", [128, S], BF16)
            junk = P
            PT = self.sb(es, "PT", [128, 16, 128], BF16)
            st = self.sb(es, "at_st", [128, 4], F32)
            bs = self.sb(es, "bs", [128, 8], F32)
            maskf = self.sb(es, "maskf", [128, 128], F32)
            with ExitStack() as es2:
                pri = Dt[:].bitcast(I32)
                pci = self.sb(es2, "pci", [128, NT], I32)
                mi = self.sb(es2, "mi", [128, 128], I32)
                sc.dma('sp', pri, self.pos[b:b + 1, :].partition_broadcast(128), writes=['pri'], chan='misc')
                sc.dma('sp', pci[:], self.pos[b].rearrange("(t p) -> p t", p=128), writes=['pci'], chan='misc', allow_slow_non_contiguous=True)
                sc.op('dve', lambda e: e.tensor_copy(out=posrow[:], in_=pri), reads=['pri'], writes=['posrow'])
                sc.op('dve', lambda e: e.tensor_copy(out=poscol[:], in_=pci[:]), reads=['pci'], writes=['poscol'])
                sc.op('dve', lambda e: e.tensor_scalar(out=poscol[:], in0=poscol[:], scalar1=-1.0, scalar2=None, op0=ALU.mult),
                      reads=['poscol'], writes=['poscol'])
                sc.op('pool', lambda e: e.iota(mi[:], pattern=[[1, 128]], base=0, channel_multiplier=0), writes=['mi'])
                sc.op('dve', lambda e: e.tensor_scalar(out=maskf[:], in0=mi[:], scalar1=64.0, scalar2=-1e30, op0=ALU.is_ge, op1=ALU.mult),
                      reads=['mi'], writes=['maskf'])
                sc.op('dve', lambda e: e.memset(maskf[64:128, :], 0.0), reads=['maskf'], writes=['maskf'])
                sc.barrier()
            loads = []
            for c in range(4):
                loads.append(([(0, 128, win_d[:, c * 128:(c + 1) * 128])], Xb[c], f'X{c}'))
            self.proj_featmajor(wchunk, wkeys, loads, uT, uTk, None, evac_scale=0.125)
            loads = []
            for c in range(4):
                loads.append(([(0, 128, win_d[:, 512 + c * 128:512 + (c + 1) * 128])], Xb[4 + c], f'X{4 + c}'))
            for c in range(4):
                loads.append(([(0, 128, win_d[:, 1536 + c * 128:1536 + (c + 1) * 128])], Xb[8 + c], f'X{8 + c}'))
            loads.append(([(0, 64, win_d[:, 2048:2112]), (64, 128, win_d[:, 2048:2112])], ikT2, 'ikT2'))
            self.proj_featmajor(wchunk, wkeys, loads, uT, uTk, None)
            with ExitStack() as es2:
                wv = MB[:].bitcast(BF16).rearrange("p (k n) -> p k n", k=8)
                wiw = self.sb(es2, "wiw", [128, 8, 8], BF16)
                sc.dma('pool', wv, win_d[:, 1024:1536].rearrange("(k p) n -> p k n", p=128), writes=['wv'], chan='wv')
                sc.dma('pool', wiw[:], win_d[:, 2112:2120].rearrange("(k p) n -> p k n", p=128), writes=['wiw'], chan='wv')
                for t in range(NT):
                    pb = 4 + t % 2
                    for k in range(8):
                        sc.op('pe', lambda e, k=k: e.matmul(self.ps[pb][:], lhsT=uT[:, k, t * 128:(t + 1) * 128], rhs=wv[:, k, :],
                                                            start=(k == 0), stop=(k == 7)), reads=[uTk[t], 'wv'], writes=[f'ps{pb}'])
                    sc.op('act', lambda e: e.activation(out=Xb[12 + t // 4][:, (t % 4) * 512:(t % 4 + 1) * 512], in_=self.ps[pb][:], func=AF.Copy),
                          reads=[f'ps{pb}'], writes=[f'X{12 + t // 4}'])
                    pb2 = 6 + t % 2
                    for k in range(8):
                        sc.op('pe', lambda e, k=k: e.matmul(self.ps[pb2][:, 0:8], lhsT=uT[:, k, t * 128:(t + 1) * 128], rhs=wiw[:, k, :],
                                                            start=(k == 0), stop=(k == 7)), reads=[uTk[t], 'wiw'], writes=[f'ps{pb2}'])
                    sc.op('dve', lambda e: e.tensor_copy(out=iw[:, t, :], in_=self.ps[pb2][:, 0:8]), reads=[f'ps{pb2}'], writes=['iw'])
                sc.barrier()
            NIT = 14
            for qt in range(NT):
                KV = (qt + 1) * 128
                qs = slice(qt * 128, (qt + 1) * 128)
                nkc = (KV + 511) // 512
                bank = 0
                for kc in range(nkc):
                    c0 = kc * 512
                    c1 = min(KV, c0 + 512)
                    for h in range(8):
                        pb = bank % 4
                        bank += 1
                        hp = slice((h % 2) * 64, (h % 2) * 64 + 64)
                        sc.op('pe', lambda e: e.matmul(self.ps[pb][:, 0:c1 - c0], lhsT=Xb[8 + h // 2][hp, qs], rhs=ikT2[hp, c0:c1],
                                                       start=True, stop=True),
                              reads=[f'X{8 + h // 2}', 'ikT2'], writes=[f'ps{pb}', 'psS'])
                        rs_ = h % 2
                        sc.op('act', lambda e: e.activation(out=rl[rs_][:, 0:c1 - c0], in_=self.ps[pb][:, 0:c1 - c0], func=AF.Relu),
                              reads=[f'ps{pb}'], writes=[f'rl{rs_}'])
                        if h == 0:
                            sc.op('dve', lambda e: e.tensor_scalar(out=acc[:, c0:c1], in0=rl[rs_][:, 0:c1 - c0], scalar1=iw[:, qt, 0:1], scalar2=None,
                                                                   op0=ALU.mult), reads=[f'rl{rs_}', 'iw'], writes=['acc'])
                        else:
                            sc.op('dve', lambda e: e.scalar_tensor_tensor(out=acc[:, c0:c1], in0=rl[rs_][:, 0:c1 - c0], scalar=iw[:, qt, h:h + 1],
                                                                          in1=acc[:, c0:c1], op0=ALU.mult, op1=ALU.add),
                                  reads=[f'rl{rs_}', 'iw', 'acc'], writes=['acc'])
                if KV > 256:
                    sc.op('dve', lambda e: e.tensor_reduce(out=bs[:, 0:1], in_=acc[:, 0:KV], axis=AX.X, op=ALU.max), reads=['acc'], writes=['bs_hi'])
                    sc.op('dve', lambda e: e.tensor_reduce(out=bs[:, 1:2], in_=acc[:, 0:KV], axis=AX.X, op=ALU.min), reads=['acc'], writes=['bs_lo'])
                    sc.op('dve', lambda e: e.tensor_tensor(out=bs[:, 2:3], in0=bs[:, 0:1], in1=bs[:, 1:2], op=ALU.subtract),
                          reads=['bs_hi', 'bs_lo'], writes=['bs_h'])
                sc.op('dve', lambda e: e.tensor_tensor(out=acc[:, KV - 128:KV], in0=acc[:, KV - 128:KV], in1=maskf[:], op=ALU.add),
                      reads=['acc', 'maskf'], writes=['acc'])
                if KV > 256:
                    for it in range(NIT):
                        sc.op('dve', lambda e: e.tensor_scalar(out=bs[:, 2:3], in0=bs[:, 2:3], scalar1=0.5, scalar2=None, op0=ALU.mult),
                              reads=['bs_h'], writes=['bs_h'])
                        sc.op('dve', lambda e: e.tensor_tensor(out=bs[:, 3:4], in0=bs[:, 1:2], in1=bs[:, 2:3], op=ALU.add),
                              reads=['bs_lo', 'bs_h'], writes=['bs_mid'])
                        sc.op('dve', lambda e: e.tensor_scalar(out=junk[:, 0:KV], in0=acc[:, 0:KV], scalar1=bs[:, 3:4], scalar2=0.0,
                                                               op0=ALU.is_ge, op1=ALU.add, accum_out=bs[:, 4:5]),
                              reads=['acc', 'bs_mid'], writes=['at_P', 'bs_cnt'])
                        sc.op('dve', lambda e: e.tensor_scalar(out=bs[:, 5:6], in0=bs[:, 4:5], scalar1=255.5, scalar2=bs[:, 2:3],
                                                               op0=ALU.is_ge, op1=ALU.mult), reads=['bs_cnt', 'bs_h'], writes=['bs_g'])
                        sc.op('dve', lambda e: e.tensor_tensor(out=bs[:, 1:2], in0=bs[:, 1:2], in1=bs[:, 5:6], op=ALU.add),
                              reads=['bs_lo', 'bs_g'], writes=['bs_lo'])
                    thr = bs[:, 1:2]
                else:
                    sc.op('dve', lambda e: e.memset(bs[:, 1:2], -1e29), writes=['bs_lo'])
                    thr = bs[:, 1:2]
                sc.op('dve', lambda e: e.tensor_scalar(out=MB[:, 0:KV], in0=acc[:, 0:KV], scalar1=thr, scalar2=-1e30, op0=ALU.is_lt, op1=ALU.mult),
                      reads=['acc', 'bs_lo'], writes=['MB'])
                sc.op('act', lambda e: e.activation(out=Dt[:, 0:KV], in_=posrow[:, 0:KV], func=AF.Abs, bias=poscol[:, qt:qt + 1], scale=1.0),
                      reads=['posrow', 'poscol'], writes=['Dt'])
                for h in range(8):
                    sl = h % 2
                    slope = float(2.0 ** (-(h + 1)))
                    sc.op('dve', lambda e: e.scalar_tensor_tensor(out=bh[sl][:, 0:KV], in0=Dt[:, 0:KV], scalar=-slope, in1=MB[:, 0:KV],
                                                                  op0=ALU.mult, op1=ALU.add), reads=['Dt', 'MB'], writes=[f'bh{sl}'])
                    hp = slice((h % 2) * 64, (h % 2) * 64 + 64)
                    self.attn_tile(Xb[h // 2][hp, qs], Xb[4 + h // 2][hp, :], f'X{4 + h // 2}', None, None, KV, 1.0,
                                   O[:, qt, h * 64:(h + 1) * 64], f'O{qt}', (P, PT, st), None, [f'X{h // 2}'],
                                   bias=bh[sl], bias_key=f'bh{sl}',
                                   v_of=lambda blk, h=h: (Xb[12 + blk // 4][:, (blk % 4) * 512 + h * 64:(blk % 4) * 512 + (h + 1) * 64], f'X{12 + blk // 4}'),
                                   dv=64)
            sc.barrier()


    def gdn_part(self, b, l, e_, win_d, O, uT, Xb, uTk):
        sc = self.sc
        self._wc = 0
        T_ = slice
        with ExitStack() as es:
            U = self.sb(es, "gU", [128, 128], F32)
            Li = self.sb(es, "gLi", [128, 128], F32)
            Ls = self.sb(es, "gLs", [128, 128], F32)
            ones = self.sb(es, "gones", [128, 128], F32)
            NG = self.sb(es, "gNG", [128, 128], F32)
            cw = self.sb(es, "gcw", [128, 12, 4], F32)
            prm = self.sb(es, "gprm", [128, 16], F32)
            epsb = self.sb(es, "gepsb", [128, 4], F32)
            ab = self.sb(es, "gab", [128, NT, 8], F32)
            gg = self.sb(es, "ggg", [128, NT, 4], F32)
            bt = self.sb(es, "gbt", [128, NT, 4], F32)
            sc.op('dve', lambda e: e.tensor_scalar(out=U[:], in0=self.iot[:], scalar1=0.0, scalar2=None, op0=ALU.is_ge), reads=['iot'], writes=['gU'])
            sc.op('dve', lambda e: e.tensor_scalar(out=Li[:], in0=self.iot[:], scalar1=0.0, scalar2=None, op0=ALU.is_le), reads=['iot'], writes=['gLi'])
            sc.op('dve', lambda e: e.tensor_scalar(out=Ls[:], in0=self.iot[:], scalar1=0.0, scalar2=None, op0=ALU.is_lt), reads=['iot'], writes=['gLs'])
            sc.op('dve', lambda e: e.memset(ones[:], 1.0), writes=['gones'])
            sc.op('dve', lambda e: e.memset(epsb[:, 0:1], RMS_EPS), writes=['gepsb'])
            sc.op('dve', lambda e: e.memset(epsb[:, 1:2], 128.0 * RMS_EPS), writes=['gepsb'])
            sc.dma('sp', NG[:], self.W('gdn_norm_g', (e_,), [1, 128]).partition_broadcast(128), writes=['gNG'], chan='misc')
            for j in range(4):
                sc.dma('sp', cw[:, :, j], self.W('gdn_conv_w', (e_,), [4, 1536])[j].rearrange("(c p) -> p c", p=128), writes=['gcw'], chan='misc',
                       allow_slow_non_contiguous=True)
            sc.dma('sp', prm[:, 0:4], self.W('gdn_dt_bias', (e_,), [1, 4]).partition_broadcast(128), writes=['gprm'], chan='misc')
            sc.dma('sp', prm[:, 4:8], self.W('gdn_a_log', (e_,), [1, 4]).partition_broadcast(128), writes=['gprm'], chan='misc')
            sc.op('act', lambda e: e.activation(out=prm[:, 8:12], in_=prm[:, 4:8], func=AF.Exp), reads=['gprm'], writes=['gprm'])
            sc.op('dve', lambda e: e.tensor_scalar(out=prm[:, 8:12], in0=prm[:, 8:12], scalar1=-1.0, scalar2=None, op0=ALU.mult),
                  reads=['gprm'], writes=['gprm'])
            with ExitStack() as es1:
                wchunk = [self.sb(es1, f"wch{i}", [128, 8, 128], BF16) for i in range(2)]
                raw = self.sb(es1, "graw", [128, S + 3], F32)
                y = self.sb(es1, "gy", [128, S], F32)
                sq = [self.sb(es1, f"gsq{i}", [128, 512], F32) for i in range(2)]
                rin = [self.sb(es1, f"grin{i}", [128, 512], F32) for i in range(2)]
                sc.op('dve', lambda e: e.memset(raw[:, 0:3], 0.0), writes=['graw'])
                for ci in range(12):
                    sl = ci % 2
                    c0 = 2120 + ci * 128
                    sc.dma('pool', wchunk[sl][:], win_d[:, c0:c0 + 128].rearrange("(k p) n -> p k n", p=128), writes=[f'wch{sl}'], chan=f'wch{sl}')
                    for g4 in range(4):
                        pb = 4 + (g4 % 2)
                        for k in range(8):
                            sc.op('pe', lambda e, k=k: e.matmul(self.ps[pb][:], lhsT=wchunk[sl][:, k, :], rhs=uT[:, k, g4 * 512:(g4 + 1) * 512],
                                                                start=(k == 0), stop=(k == 7)),
                                  reads=[f'wch{sl}'] + uTk[g4 * 4:g4 * 4 + 4], writes=[f'ps{pb}'])
                        sc.op('act', lambda e: e.activation(out=raw[:, 3 + g4 * 512:3 + (g4 + 1) * 512], in_=self.ps[pb][:], func=AF.Copy),
                              reads=[f'ps{pb}'], writes=['graw'])
                    sc.op('dve', lambda e: e.tensor_scalar(out=y[:], in0=raw[:, 0:S], scalar1=cw[:, ci, 0:1], scalar2=None, op0=ALU.mult),
                          reads=['graw', 'gcw'], writes=['gy'])
                    for j in range(1, 4):
                        sc.op('dve', lambda e, j=j: e.scalar_tensor_tensor(out=y[:], in0=raw[:, j:S + j], scalar=cw[:, ci, j:j + 1], in1=y[:],
                                                                           op0=ALU.mult, op1=ALU.add), reads=['graw', 'gcw', 'gy'], writes=['gy'])
                    dst = Xb[ci]
                    if ci >= 8:
                        sc.op('act', lambda e: e.activation(out=dst[:, :], in_=y[:], func=AF.Silu), reads=['gy'], writes=[f'X{ci}'])
                    else:
                        sc.op('act', lambda e: e.activation(out=y[:], in_=y[:], func=AF.Silu), reads=['gy'], writes=['gy'])
                        for g4 in range(4):
                            cs_ = slice(g4 * 512, (g4 + 1) * 512)
                            s2 = g4 % 2
                            sc.op('pool', lambda e: e.tensor_tensor(out=sq[s2][:], in0=y[:, cs_], in1=y[:, cs_], op=ALU.mult),
                                  reads=['gy'], writes=[f'gsq{s2}'])
                            pb = 6 + s2
                            sc.op('pe', lambda e: e.matmul(self.ps[pb][:], lhsT=ones[:], rhs=sq[s2][:], start=True, stop=True),
                                  reads=['gones', f'gsq{s2}'], writes=[f'ps{pb}'])
                            if ci < 4:
                                sc.op('act', lambda e: e.activation(out=rin[s2][:], in_=self.ps[pb][:], func=AF.Sqrt, bias=epsb[:, 1:2], scale=128.0),
                                      reads=[f'ps{pb}', 'gepsb'], writes=[f'grin{s2}'])
                            else:
                                sc.op('act', lambda e: e.activation(out=rin[s2][:], in_=self.ps[pb][:], func=AF.Sqrt, bias=epsb[:, 0:1], scale=1.0),
                                      reads=[f'ps{pb}', 'gepsb'], writes=[f'grin{s2}'])
                            sc.op('dve', lambda e: e.reciprocal(out=rin[s2][:], in_=rin[s2][:]), reads=[f'grin{s2}'], writes=[f'grin{s2}'])
                            sc.op('dve', lambda e: e.tensor_tensor(out=dst[:, cs_], in0=y[:, cs_], in1=rin[s2][:], op=ALU.mult),
                                  reads=['gy', f'grin{s2}'], writes=[f'X{ci}'])
                sc.barrier()
            if GDN_STOP <= 1:
                for t in range(NT):
                    sc.op('dve', lambda e: e.memset(O[:, t, 512:1024], 0.0), writes=[f'O{t}'])
                sc.barrier()
                return
            with ExitStack() as es2:
                wz = self.sb(es2, "gwz", [128, 8, 512], BF16)
                wab = self.sb(es2, "gwab", [128, 8, 8], BF16)
                tmp = self.sb(es2, "gtmp", [128, NT, 4], F32)
                sc.dma('pool', wz[:], win_d[:, 3664:4176].rearrange("(k p) n -> p k n", p=128), writes=['gwz'], chan='wv')
                sc.dma('pool', wab[:], win_d[:, 3656:3664].rearrange("(k p) n -> p k n", p=128), writes=['gwab'], chan='wv')
                for t in range(NT):
                    pb = 4 + t % 2
                    for k in range(8):
                        sc.op('pe', lambda e, k=k: e.matmul(self.ps[pb][:], lhsT=uT[:, k, t * 128:(t + 1) * 128], rhs=wz[:, k, :],
                                                            start=(k == 0), stop=(k == 7)), reads=[uTk[t], 'gwz'], writes=[f'ps{pb}'])
                    sc.op('act', lambda e: e.activation(out=Xb[12 + t // 4][:, (t % 4) * 512:(t % 4 + 1) * 512], in_=self.ps[pb][:], func=AF.Silu),
                          reads=[f'ps{pb}'], writes=[f'X{12 + t // 4}'])
                    pb2 = 6 + t % 2
                    for k in range(8):
                        sc.op('pe', lambda e, k=k: e.matmul(self.ps[pb2][:, 0:8], lhsT=uT[:, k, t * 128:(t + 1) * 128], rhs=wab[:, k, :],
                                                            start=(k == 0), stop=(k == 7)), reads=[uTk[t], 'gwab'], writes=[f'ps{pb2}'])
                    sc.op('dve', lambda e: e.tensor_copy(out=ab[:, t, :], in_=self.ps[pb2][:, 0:8]), reads=[f'ps{pb2}'], writes=['gab'])
                bc4 = lambda ap: ap.unsqueeze(1).to_broadcast([128, NT, 4])
                sc.op('dve', lambda e: e.tensor_tensor(out=tmp[:], in0=ab[:, :, 0:4], in1=bc4(prm[:, 0:4]), op=ALU.add),
                      reads=['gab', 'gprm'], writes=['gtmp'])
                sc.op('act', lambda e: e.activation(out=tmp[:], in_=tmp[:], func=AF.Exp), reads=['gtmp'], writes=['gtmp'])
                sc.op('act', lambda e: e.activation(out=tmp[:], in_=tmp[:], func=AF.Ln, bias=1.0, scale=1.0), reads=['gtmp'], writes=['gtmp'])
                sc.op('dve', lambda e: e.tensor_tensor(out=gg[:], in0=tmp[:], in1=bc4(prm[:, 8:12]), op=ALU.mult),
                      reads=['gtmp', 'gprm'], writes=['ggg'])
                sc.op('act', lambda e: e.activation(out=bt[:], in_=ab[:, :, 4:8], func=AF.Sigmoid), reads=['gab'], writes=['gbt'])
                sc.barrier()
            if GDN_STOP <= 2:
                for t in range(NT):
                    sc.op('dve', lambda e: e.memset(O[:, t, 512:1024], 0.0), writes=[f'O{t}'])
                sc.barrier()
                return
            with ExitStack() as es3:
                f32t = lambda n: self.sb(es3, n, [128, 128], F32)
                b16t = lambda n: self.sb(es3, n, [128, 128], BF16)
                NS = 4
                B = []
                for i in range(NS):
                    B.append(dict(gb=f32t(f"g_gb{i}"), dd=f32t(f"g_dd{i}"), E=f32t(f"g_E{i}"), ET=f32t(f"g_ET{i}"),
                                  Pa=f32t(f"g_Pa{i}"), Pb=f32t(f"g_Pb{i}"), Qa=f32t(f"g_Qa{i}"), Qb=f32t(f"g_Qb{i}"),
                                  R=f32t(f"g_R{i}"), Rb=b16t(f"g_Rb{i}"), AT=b16t(f"g_AT{i}"), vb=b16t(f"g_vb{i}"),
                                  kbg=b16t(f"g_kbg{i}"), kdec=b16t(f"g_kdec{i}"), u=f32t(f"g_u{i}"), wT=b16t(f"g_wT{i}"),
                                  vnb=b16t(f"g_vnb{i}"), o=f32t(f"g_o{i}"), o2=f32t(f"g_o2{i}"), sm=self.sb(es3, f"g_sm{i}", [128, 16], F32)))
                Sst = [f32t(f"g_S{h}") for h in range(4)]
                Sbf = [b16t(f"g_Sb{h}") for h in range(4)]
                for h in range(4):
                    sc.op('dve', lambda e: e.memset(Sst[h][:], 0.0), writes=[f'gS{h}'])
                    sc.op('dve', lambda e: e.memset(Sbf[h][:], 0.0), writes=[f'gSb{h}'])
                q = lambda pb, j: self.ps[pb][:, j * 128:(j + 1) * 128]
                for t in range(0 if G3_LEVEL < 5 else GDN_NT, NT):
                    sc.op('dve', lambda e: e.memset(O[:, t, 512:1024], 0.0), writes=[f'O{t}'])
                def step(t, h):
                    ts_ = slice(t * 128, (t + 1) * 128)
                    PA = lambda j: self.ps[2 * h][:, j * 128:(j + 1) * 128]
                    PB = lambda j: self.ps[2 * h + 1][:, j * 128:(j + 1) * 128]
                    kA, kB = f'ps{2 * h}', f'ps{2 * h + 1}'
                    i = h % NS
                    bb = B[i]
                    K = lambda n: f'g_{n}{i}'
                    sm = bb['sm']
                    qT, kT, vT = Xb[h][:, ts_], Xb[4 + h][:, ts_], Xb[8 + h][:, ts_]
                    kq, kk_, kv = f'X{h}', f'X{4 + h}', f'X{8 + h}'
                    gcol = gg[:, t, h:h + 1]
                    bcol = bt[:, t, h:h + 1]
                    sc.op('dve', lambda e: e.tensor_copy(out=bb['gb'][:], in_=gcol.to_broadcast([128, 128])), reads=['ggg'], writes=[K('gb')])
                    yield
                    sc.op('pe', lambda e: e.matmul(PB(0), lhsT=U[:], rhs=bb['gb'][:], start=True, stop=True),
                          reads=['gU', K('gb')], writes=[kB])
                    yield
                    sc.op('pe', lambda e: e.matmul(PA(0), lhsT=bb['gb'][:], rhs=U[:], start=True, stop=True),
                          reads=['gU', K('gb')], writes=[kA])
                    yield
                    sc.op('act', lambda e: e.activation(out=sm[:, 0:1], in_=PB(0)[:, 0:1], func=AF.Copy), reads=[kB], writes=[K('sm0')])
                    yield
                    sc.op('dve', lambda e: e.tensor_scalar(out=bb['dd'][:], in0=PA(0), scalar1=sm[:, 0:1], scalar2=0.0,
                                                           op0=ALU.subtract, op1=ALU.max), reads=[kA, K('sm0')], writes=[K('dd')])
                    yield
                    sc.op('act', lambda e: e.activation(out=bb['E'][:], in_=bb['dd'][:], func=AF.Exp, scale=-1.0), reads=[K('dd')], writes=[K('E')])
                    yield
                    sc.op('dve', lambda e: e.tensor_scalar(out=bb['dd'][:], in0=PA(0), scalar1=sm[:, 0:1], scalar2=0.0,
                                                           op0=ALU.subtract, op1=ALU.min), reads=[kA, K('sm0'), K('dd')], writes=[K('dd')])
                    yield
                    sc.op('act', lambda e: e.activation(out=bb['ET'][:], in_=bb['dd'][:], func=AF.Exp), reads=[K('dd')], writes=[K('ET')])
                    yield
                    sc.op('pool', lambda e: e.tensor_tensor(out=bb['ET'][:], in0=bb['ET'][:], in1=U[:], op=ALU.mult), reads=[K('ET'), 'gU'], writes=[K('ET')])
                    yield
                    sc.op('pool', lambda e: e.tensor_tensor(out=bb['E'][:], in0=bb['E'][:], in1=Ls[:], op=ALU.mult), reads=[K('E'), 'gLs'], writes=[K('E')])
                    yield
                    sc.op('act', lambda e: e.activation(out=sm[:, 1:2], in_=sm[:, 0:1], func=AF.Exp), reads=[K('sm0')], writes=[K('sm1')])
                    yield
                    sc.op('dve', lambda e: e.tensor_tensor(out=sm[:, 2:3], in0=sm[:, 1:2], in1=bcol, op=ALU.mult), reads=[K('sm1'), 'gbt'], writes=[K('sm2')])
                    yield
                    sc.op('dve', lambda e: e.tensor_tensor(out=sm[:, 3:4], in0=PA(0)[:, 127:128], in1=sm[:, 0:1], op=ALU.subtract),
                          reads=[kA, K('sm0')], writes=[K('sm3')])
                    yield
                    sc.op('act', lambda e: e.activation(out=sm[:, 3:4], in_=sm[:, 3:4], func=AF.Exp), reads=[K('sm3')], writes=[K('sm3')])
                    yield
                    sc.op('dve', lambda e: e.tensor_copy(out=sm[:, 8:9], in_=PA(0)[:, 127:128]), reads=[kA], writes=[K('sm8')])
                    yield
                    sc.op('act', lambda e: e.activation(out=sm[:, 4:5], in_=sm[:, 8:9], func=AF.Exp), reads=[K('sm8')], writes=[K('sm4')])
                    yield
                    if G3_LEVEL < 2:
                        return
                    sc.op('pe', lambda e: e.matmul(PA(1), lhsT=kT, rhs=kT, start=True, stop=True), reads=[kk_], writes=[kA])
                    yield
                    sc.op('pe', lambda e: e.matmul(PA(2), lhsT=kT, rhs=qT, start=True, stop=True), reads=[kk_, kq], writes=[kA])
                    yield
                    if G3_LEVEL < 2.07:
                        return
                    sc.op('dve', lambda e: e.scalar_tensor_tensor(out=bb['Qa'][:], in0=PA(1), scalar=bcol, in1=bb['E'][:], op0=ALU.mult, op1=ALU.mult),
                          reads=[kA, 'gbt', K('E')], writes=[K('Qa')])
                    yield
                    sc.op('dve', lambda e: e.tensor_tensor(out=bb['AT'][:], in0=PA(2), in1=bb['ET'][:], op=ALU.mult),
                          reads=[kA, K('ET')], writes=[K('AT')])
                    yield
                    if G3_LEVEL < 2.15:
                        return
                    sc.op('pe', lambda e: e.matmul(PB(1), lhsT=bb['Qa'][:], rhs=self.ident_f[:], start=True, stop=True), reads=[K('Qa'), 'ident_f'], writes=[kB])
                    yield
                    sc.op('act', lambda e: e.activation(out=bb['Pa'][:], in_=PB(1), func=AF.Copy), reads=[kB], writes=[K('Pa')])
                    yield
                    sc.op('dve', lambda e: e.tensor_tensor(out=bb['R'][:], in0=self.ident_f[:], in1=bb['Pa'][:], op=ALU.subtract),
                          reads=[K('Pa'), 'ident_f'], writes=[K('R')])
                    yield
                    if G3_LEVEL < 2.25:
                        return
                    pk = PB(2).bitcast(BF16)
                    sc.op('pe', lambda e: e.transpose(out=pk[:, 0:128], in_=kT, identity=self.ident_b[:]), reads=[kk_, 'ident_b'], writes=[kB])
                    yield
                    sc.op('pe', lambda e: e.transpose(out=pk[:, 128:256], in_=vT, identity=self.ident_b[:]), reads=[kv, 'ident_b'], writes=[kB])
                    yield
                    sc.op('act', lambda e: e.activation(out=bb['kbg'][:], in_=pk[:, 0:128], func=AF.Copy, scale=sm[:, 2:3]), reads=[kB, K('sm2')], writes=[K('kbg')])
                    yield
                    sc.op('act', lambda e: e.activation(out=bb['kdec'][:], in_=pk[:, 0:128], func=AF.Copy, scale=sm[:, 3:4]), reads=[kB, K('sm3')], writes=[K('kdec')])
                    yield
                    sc.op('act', lambda e: e.activation(out=bb['vb'][:], in_=pk[:, 128:256], func=AF.Copy, scale=bcol),
                          reads=[kB, 'gbt'], writes=[K('vb')])
                    yield
                    if G3_LEVEL < 3:
                        return
                    P_, Q_ = bb['Pa'], bb['Qa']
                    Pn, Qn = bb['Pb'], bb['Qb']
                    kP, kQ, kPn, kQn = K('Pa'), K('Qa'), K('Pb'), K('Qb')
                    for lev in range(6):
                        lastl = (lev == 5)
                        sc.op('pe', lambda e: e.matmul(PB(3), lhsT=P_[:], rhs=Q_[:], start=True, stop=True), reads=[kP, kQ], writes=[kB])
                        yield
                        sc.op('act', lambda e: e.activation(out=Qn[:], in_=PB(3), func=AF.Copy), reads=[kB], writes=[kQn])
                        yield
                        if not lastl:
                            sc.op('pe', lambda e: e.matmul(PB(1), lhsT=Q_[:], rhs=P_[:], start=True, stop=True), reads=[kP, kQ], writes=[kB])
                            yield
                            sc.op('act', lambda e: e.activation(out=Pn[:], in_=PB(1), func=AF.Copy), reads=[kB], writes=[kPn])
                            yield
                        sc.op('pe', lambda e: e.matmul(PA(3), lhsT=Qn[:], rhs=bb['R'][:], start=True, stop=True), reads=[kQn, K('R')], writes=[kA])
                        yield
                        sc.op('dve', lambda e: e.tensor_tensor(out=bb['R'][:], in0=bb['R'][:], in1=PA(3), op=ALU.add), reads=[kA, K('R')], writes=[K('R')])
                        yield
                        P_, Pn = Pn, P_
                        Q_, Qn = Qn, Q_
                        kP, kPn = kPn, kP
                        kQ, kQn = kQn, kQ
                    if G3_LEVEL < 4:
                        return
                    sc.op('act', lambda e: e.activation(out=bb['Rb'][:], in_=bb['R'][:], func=AF.Copy), reads=[K('R')], writes=[K('Rb')])
                    yield
                    sc.op('pe', lambda e: e.matmul(PB(0), lhsT=bb['Rb'][:], rhs=bb['vb'][:], start=True, stop=True), reads=[K('Rb'), K('vb')], writes=[kB])
                    yield
                    sc.op('pe', lambda e: e.matmul(PB(1), lhsT=bb['kbg'][:], rhs=bb['Rb'][:], start=True, stop=True), reads=[K('Rb'), K('kbg')], writes=[kB])
                    yield
                    sc.op('act', lambda e: e.activation(out=bb['u'][:], in_=PB(0), func=AF.Copy), reads=[kB], writes=[K('u')])
                    yield
                    sc.op('act', lambda e: e.activation(out=bb['wT'][:], in_=PB(1), func=AF.Copy), reads=[kB], writes=[K('wT')])
                    yield
                    sc.op('pe', lambda e: e.matmul(PA(0), lhsT=bb['wT'][:], rhs=Sbf[h][:], start=True, stop=True), reads=[K('wT'), f'gSb{h}'], writes=[kA])
                    yield
                    sc.op('pe', lambda e: e.matmul(PA(1), lhsT=qT, rhs=Sbf[h][:], start=True, stop=True), reads=[kq, f'gSb{h}'], writes=[kA])
                    yield
                    sc.op('dve', lambda e: e.tensor_tensor(out=bb['vnb'][:], in0=bb['u'][:], in1=PA(0), op=ALU.subtract), reads=[kA, K('u')], writes=[K('vnb')])
                    yield
                    sc.op('pe', lambda e: e.matmul(PA(2), lhsT=bb['AT'][:], rhs=bb['vnb'][:], start=True, stop=True), reads=[K('AT'), K('vnb')], writes=[kA])
                    yield
                    sc.op('pe', lambda e: e.matmul(PA(3), lhsT=bb['kdec'][:], rhs=bb['vnb'][:], start=True, stop=True), reads=[K('kdec'), K('vnb')], writes=[kA])
                    yield
                    sc.op('dve', lambda e: e.tensor_scalar(out=bb['o'][:], in0=PA(1), scalar1=sm[:, 1:2], scalar2=None, op0=ALU.mult),
                          reads=[kA, K('sm1')], writes=[K('o')])
                    yield
                    sc.op('dve', lambda e: e.tensor_tensor(out=bb['o'][:], in0=bb['o'][:], in1=PA(2), op=ALU.add), reads=[kA, K('o')], writes=[K('o')])
                    yield
                    sc.op('dve', lambda e: e.scalar_tensor_tensor(out=Sst[h][:], in0=Sst[h][:], scalar=sm[:, 4:5], in1=PA(3), op0=ALU.mult, op1=ALU.add),
                          reads=[kA, K('sm4'), f'gS{h}'], writes=[f'gS{h}'])
                    yield
                    sc.op('act', lambda e: e.activation(out=Sbf[h][:], in_=Sst[h][:], func=AF.Copy), reads=[f'gS{h}'], writes=[f'gSb{h}'])
                    yield
                    if G3_LEVEL < 5:
                        return
                    sc.op('act', lambda e: e.activation(out=bb['o2'][:], in_=bb['o'][:], func=AF.Square, accum_out=sm[:, 5:6]),
                          reads=[K('o')], writes=[K('o2'), K('sm5')])
                    yield
                    sc.op('act', lambda e: e.activation(out=sm[:, 6:7], in_=sm[:, 5:6], func=AF.Sqrt, bias=epsb[:, 0:1], scale=1.0 / 128),
                          reads=[K('sm5'), 'gepsb'], writes=[K('sm6')])
                    yield
                    sc.op('dve', lambda e: e.reciprocal(out=sm[:, 7:8], in_=sm[:, 6:7]), reads=[K('sm6')], writes=[K('sm7')])
                    yield
                    sc.op('dve', lambda e: e.scalar_tensor_tensor(out=bb['o2'][:], in0=bb['o'][:], scalar=sm[:, 7:8], in1=NG[:], op0=ALU.mult, op1=ALU.mult),
                          reads=[K('o'), K('sm7'), 'gNG', K('o2')], writes=[K('o2')])
                    yield
                    zs = Xb[12 + t // 4][:, (t % 4) * 512 + h * 128:(t % 4) * 512 + (h + 1) * 128]
                    sc.op('pool', lambda e: e.tensor_tensor(out=O[:, t, 512 + h * 128:512 + (h + 1) * 128], in0=bb['o2'][:], in1=zs, op=ALU.mult),
                          reads=[K('o2'), f'X{12 + t // 4}'], writes=[f'O{t}'])
                    yield
                for t in range(GDN_NT):
                    gens = [step(t, h) for h in range(4)]
                    while gens:
                        for g_ in list(gens):
                            try:
                                next(g_)
                            except StopIteration:
                                gens.remove(g_)
                sc.barrier()


_PLAN = FULL_PLAN
POOL_ELEM = 'pool'
DEBUG = False
HYB_DSA = True
HYB_GDN = True
GDN_STOP = 99
GDN_NT = NT
G3_LEVEL = 5
_LAST = None
_NINS = 0


def kernel(**inputs):
    n = 8
    bld = Builder(_PLAN)
    nc = bld.build()
    global _NINS
    _NINS = bld.sc.n_ins
    wviews = {}
    for name, (key, idx, cols) in bld.wsrc.items():
        a = np.asarray(inputs[key])[idx]
        if cols is not None:
            a = a[..., cols[0]:cols[1]]
        wviews[name] = np.ascontiguousarray(a).reshape(bld.win[name].shape)
    in_maps = []
    for i in range(n):
        m = {}
        for k in ("x", "c", "positions"):
            m[k] = np.ascontiguousarray(np.asarray(inputs[k])[i * NB:(i + 1) * NB])
        for name, arr in wviews.items():
            m[name] = arr
        in_maps.append(m)
    res = run_bass_kernel_spmd(nc, in_maps, core_ids=list(range(n)))
    global _LAST
    _LAST = res
    return np.concatenate([r["out"] for r in res.results], axis=0)
```

```python
import numpy as np
from contextlib import ExitStack
import concourse.bass as bass
import concourse.mybir as mybir
from concourse.bass_utils import run_bass_kernel_spmd

F32 = mybir.dt.float32
BF16 = mybir.dt.bfloat16
I32 = mybir.dt.int32
AF = mybir.ActivationFunctionType
ALU = mybir.AluOpType
AX = mybir.AxisListType

D = 1024
S = 2048
DFF = 2816
NT = S // 128
DEPTH = 4
ALPHA = float((2.0 * DEPTH) ** 0.25)
LN_EPS = 1e-5
RMS_EPS = 1e-6
NB = 2

FULL_PLAN = [(l, s) for l in range(DEPTH) for s in range(3)]


class Sched:
    NDMA = 32

    def __init__(self, nc, es):
        self.nc = nc
        self.es = es
        self.eng = {'pe': nc.tensor, 'act': nc.scalar, 'dve': nc.vector, 'pool': nc.gpsimd, 'sp': nc.sync}
        self.sem = {}
        self.val = {}
        self.waited = {e: {} for e in self.eng}
        for e in self.eng:
            self._mksem(e)
        self.dsem = [f'd{i}' for i in range(self.NDMA)]
        for d in self.dsem:
            self._mksem(d)
        self.qpool = {'sp': self.dsem[:12], 'pool': self.dsem[12:]}
        self.rr = {'sp': 0, 'pool': 0}
        self.lastw = {}
        self.readers = {}
        self.n_ins = 0

    def _mksem(self, name):
        self.sem[name] = self.es.enter_context(self.nc.semaphore('s_' + name))
        self.val[name] = 0

    def _deps(self, reads, writes):
        deps = {}

        def add(evs):
            for s, v in evs.items():
                if deps.get(s, 0) < v:
                    deps[s] = v
        for k in reads:
            add(self.lastw.get(k, {}))
        for k in writes:
            add(self.lastw.get(k, {}))
            add(self.readers.get(k, {}))
        return deps

    def _wait(self, e, deps):
        for s, v in deps.items():
            if s == e and e == 'pe':
                continue
            if self.waited[e].get(s, 0) >= v:
                continue
            self.eng[e].wait_ge(self.sem[s], v)
            self.waited[e][s] = v
            self.n_ins += 1

    def op(self, e, fn, reads=(), writes=()):
        self._wait(e, self._deps(reads, writes))
        ins = fn(self.eng[e])
        self.val[e] += 1
        v = self.val[e]
        ins.then_inc(self.sem[e], 1)
        self.n_ins += 1
        for k in reads:
            r = self.readers.setdefault(k, {})
            if r.get(e, 0) < v:
                r[e] = v
        for k in writes:
            self.lastw[k] = {e: v}
            self.readers[k] = {}

    def dma(self, q, out, in_, reads=(), writes=(), chan=None, **kw):
        pool_ = self.qpool[q]
        s = pool_[self.rr[q] % len(pool_)]
        self.rr[q] += 1
        deps = self._deps(reads, writes)
        if self.val[s] > 0:
            deps[s] = self.val[s]
        self._wait(q, deps)
        ins = self.eng[q].dma_start(out=out, in_=in_, **kw)
        self.val[s] += 16
        v = self.val[s]
        ins.then_inc(self.sem[s], 16)
        self.n_ins += 1
        for k in reads:
            self.readers.setdefault(k, {})[s] = v
        for k in writes:
            prev = self.lastw.get(k, {})
            keep = {a: b for a, b in prev.items() if a in self.val and a.startswith('d') and a[1:].isdigit()}
            keep[s] = v
            self.lastw[k] = keep
            self.readers[k] = {}

    def barrier(self):
        for e in self.eng:
            for s, v in self.val.items():
                if (s == e and e == 'pe') or v == 0:
                    continue
                if self.waited[e].get(s, 0) >= v:
                    continue
                self.eng[e].wait_ge(self.sem[s], v)
                self.waited[e][s] = v
        self.lastw.clear()
        self.readers.clear()

    def wait_all_dma(self, e):
        for s in self.dsem:
            v = self.val[s]
            if v > 0 and self.waited[e].get(s, 0) < v:
                self.eng[e].wait_ge(self.sem[s], v)
                self.waited[e][s] = v


class Builder:
    def __init__(self, plan):
        self.plan = plan
        self.nc = bass.Bass("TRN2", target_bir_lowering=False)
        nc = self.nc
        di = lambda n, sh, dt=F32: nc.dram_tensor(n, sh, dt, kind="ExternalInput").ap()
        self.x = di("x", [NB, S, D])
        self.c = di("c", [NB, D])
        self.pos = di("positions", [NB, S], I32)
        self.win = {}
        self.wsrc = {}
        self.out = nc.dram_tensor("out", [NB, S, D], F32, kind="ExternalOutput").ap()
        self.dbg = nc.dram_tensor("dbg", [128, 4096], F32, kind="ExternalOutput").ap() if DEBUG else None
        self.dumped = set()

    def W(self, key, idx, shape, cols=None):
        name = key + "_" + "_".join(str(i) for i in idx) + ("" if cols is None else f"_c{cols[0]}")
        if name not in self.win:
            self.win[name] = self.nc.dram_tensor(name, list(shape), F32, kind="ExternalInput").ap()
            self.wsrc[name] = (key, tuple(idx), cols)
        return self.win[name]

    def sb(self, es, name, shape, dt):
        self._uid = getattr(self, '_uid', 0) + 1
        return es.enter_context(self.nc.sbuf_tensor(f"{name}_{self._uid}", shape, dt))

    def dump(self, tag, ap, key, col0, shape=None):
        if not DEBUG or tag in self.dumped:
            return
        self.dumped.add(tag)
        n = int(np.prod(ap.shape[1:]))
        dst = self.dbg[:, col0:col0 + n]
        if len(ap.shape) == 3:
            dst = dst.rearrange("p (a b) -> p a b", a=ap.shape[1])
        self.sc.dma('pool', dst, ap, reads=[key] if isinstance(key, str) else key, chan='dbg')

    def build(self):
        nc = self.nc
        with ExitStack() as es:
            self.sc = Sched(nc, es)
            sc = self.sc
            self.psbig = es.enter_context(nc.psum_tensor("psbig", [128, 2048], F32))
            self.ps = [self.psbig[:, i * 512:(i + 1) * 512] for i in range(4)] + \
                      [es.enter_context(nc.psum_tensor(f"ps{i}", [128, 512], F32)) for i in range(4, 8)]
            self.ident_f = self.sb(es, "ident_f", [128, 128], F32)
            self.ident_b = self.sb(es, "ident_b", [128, 128], BF16)
            self.iot = self.sb(es, "iot", [128, 128], I32)
            self.eps_ln = self.sb(es, "eps_ln", [128, 1], F32)
            sc.op('pool', lambda e: e.iota(self.iot[:], pattern=[[1, 128]], base=0, channel_multiplier=-1),
                  writes=['iot'])
            sc.op('dve', lambda e: e.tensor_scalar(out=self.ident_f[:], in0=self.iot[:], scalar1=0.0, scalar2=None,
                                                   op0=ALU.is_equal), reads=['iot'], writes=['ident_f'])
            sc.op('dve', lambda e: e.tensor_copy(out=self.ident_b[:], in_=self.ident_f[:]),
                  reads=['ident_f'], writes=['ident_b'])
            sc.op('dve', lambda e: e.memset(self.eps_ln[:], LN_EPS), writes=['eps_ln'])
            self.X = [self.sb(es, f"X{t}", [128, D], F32) for t in range(NT)]
            self.csrep = self.sb(es, "csrep", [128, 8, 128], BF16)
            self.cs_f = self.sb(es, "cs_f", [128, 8], F32)
            self.cs_b = self.sb(es, "cs_b", [128, 8], BF16)
            for b in range(NB):
                self.run_sequence(b)
            sc.wait_all_dma('sp')
        return nc

    def run_sequence(self, b):
        sc = self.sc
        nc = self.nc
        for t in range(NT):
            sc.dma('sp', self.X[t][:], self.x[b, t * 128:(t + 1) * 128, :], writes=[f'X{t}'], chan='xin')
        sc.dma('sp', self.cs_f[:], self.c[b].rearrange("(k p) -> p k", p=128), writes=['cs_f'], chan='misc',
               allow_slow_non_contiguous=True)
        sc.op('act', lambda e: e.activation(out=self.cs_b[:], in_=self.cs_f[:], func=AF.Silu),
              reads=['cs_f'], writes=['cs_b'])
        sc.op('dve', lambda e: e.tensor_copy(out=self.csrep[:], in_=self.cs_b[:].unsqueeze(2).to_broadcast([128, 8, 128])),
              reads=['cs_b'], writes=['csrep'])
        for (l, s) in self.plan:
            if s in (0, 2):
                self.ffn_sublayer(b, l, s)
            elif l % 2 == 1:
                self.mla_sublayer(b, l)
            else:
                self.hyb_sublayer(b, l)
        for t in range(NT):
            sc.dma('sp', self.out[b, t * 128:(t + 1) * 128, :], self.X[t][:], reads=[f'X{t}'], chan='out')

    def modulation(self, es, b, l, s, gate_mul, which=('SH', 'SC1', 'G1', 'LG', 'LB')):
        sc = self.sc
        mk = lambda n: self.sb(es, n, [128, D], F32) if n in which else None
        SH, SC1, G1, LG, LB = mk('SH'), mk('SC1'), mk('G1'), mk('LG'), mk('LB')
        with ExitStack() as es2:
            self._modulation(es2, b, l, s, gate_mul, which, SH, SC1, G1, LG, LB)
            sc.barrier()
        return SH, SC1, G1, LG, LB

    def _modulation(self, es, b, l, s, gate_mul, which, SH, SC1, G1, LG, LB):
        sc = self.sc
        mw = [self.sb(es, f"mw{i}", [128, 8, 512], BF16) for i in range(2)]
        mb = [self.sb(es, f"mb{i}", [128, 512], F32) for i in range(2)]
        if LG is not None:
            sc.dma('sp', LG[:], self.W('ln_g', (l, s), [1, D]).partition_broadcast(128), writes=['LG'], chan='misc')
        if LB is not None:
            sc.dma('sp', LB[:], self.W('ln_b', (l, s), [1, D]).partition_broadcast(128), writes=['LB'], chan='misc')
        dst = [SH, SC1, G1]
        names = ['SH', 'SC1', 'G1']
        i = 0
        for j in range(3):
            if dst[j] is None:
                continue
            for hf in range(2):
                c0 = j * D + hf * 512
                mwd = self.W('mod_w', (l,), [D, 3 * D], cols=(s * 3 * D, (s + 1) * 3 * D))
                mbd = self.W('mod_b', (l,), [1, 3 * D], cols=(s * 3 * D, (s + 1) * 3 * D))
                sl = i % 2
                sc.dma('pool', mw[sl][:], mwd[:, c0:c0 + 512].rearrange("(k p) n -> p k n", p=128),
                       writes=[f'mw{sl}'], chan=f'mw{sl}')
                sc.dma('sp', mb[sl][:], mbd[:, c0:c0 + 512].partition_broadcast(128),
                       writes=[f'mb{sl}'], chan=f'mb{sl}')
                pb = 6 + sl
                for k in range(8):
                    sc.op('pe', lambda e, k=k: e.matmul(self.ps[pb][:], lhsT=self.csrep[:, k, :], rhs=mw[sl][:, k, :],
                                                        start=(k == 0), stop=(k == 7)),
                          reads=['csrep', f'mw{sl}'], writes=[f'ps{pb}'])
                addc = 0.0 if j == 0 else 1.0
                sc.op('dve', lambda e: e.scalar_tensor_tensor(out=dst[j][:, hf * 512:(hf + 1) * 512], in0=self.ps[pb][:],
                                                              scalar=addc, in1=mb[sl][:], op0=ALU.add, op1=ALU.add),
                      reads=[f'ps{pb}', f'mb{sl}'], writes=[names[j]])
                i += 1
        if gate_mul != 1.0 and G1 is not None:
            sc.op('dve', lambda e: e.tensor_scalar(out=G1[:], in0=G1[:], scalar1=gate_mul, scalar2=None, op0=ALU.mult),
                  reads=['G1'], writes=['G1'])

    def layer_norm_tile(self, t, LG, LB, lnbuf):
        sc = self.sc
        X = self.X[t]
        st, mv, sd, rstd, nb = lnbuf
        k = f'X{t}'
        for hf in range(2):
            sc.op('dve', lambda e, hf=hf: e.bn_stats(out=st[:, hf, :], in_=X[:, hf * 512:(hf + 1) * 512]),
                  reads=[k], writes=['ln_st'])
        sc.op('dve', lambda e: e.bn_aggr(out=mv[:], in_=st[:].rearrange("p a b -> p (a b)")), reads=['ln_st'], writes=['ln_mv'])
        sc.op('act', lambda e: e.activation(out=sd[:], in_=mv[:, 1:2], func=AF.Sqrt, bias=self.eps_ln[:], scale=1.0),
              reads=['ln_mv', 'eps_ln'], writes=['ln_sd'])
        sc.op('dve', lambda e: e.reciprocal(out=rstd[:], in_=sd[:]), reads=['ln_sd'], writes=['ln_rstd'])
        sc.op('dve', lambda e: e.tensor_scalar(out=nb[:], in0=mv[:, 0:1], scalar1=rstd[:], scalar2=-1.0,
                                               op0=ALU.mult, op1=ALU.mult), reads=['ln_mv', 'ln_rstd'], writes=['ln_nb'])
        sc.op('act', lambda e: e.activation(out=X[:], in_=X[:], func=AF.Identity, bias=nb[:], scale=rstd[:]),
              reads=[k, 'ln_nb', 'ln_rstd'], writes=[k])
        sc.op('dve', lambda e: e.tensor_tensor(out=X[:], in0=X[:], in1=LG[:], op=ALU.mult), reads=[k, 'LG'], writes=[k])
        sc.op(POOL_ELEM, lambda e: e.tensor_tensor(out=X[:], in0=X[:], in1=LB[:], op=ALU.add), reads=[k, 'LB'], writes=[k])

    def ln_bufs(self, es):
        return (self.sb(es, "ln_st", [128, 2, 6], F32), self.sb(es, "ln_mv", [128, 2], F32),
                self.sb(es, "ln_sd", [128, 1], F32), self.sb(es, "ln_rstd", [128, 1], F32),
                self.sb(es, "ln_nb", [128, 1], F32))

    def mod_transpose_tile(self, t, SC1, SH, utmp, ub, dstT, dst_key, col0, pbank):
        sc = self.sc
        sl = t % 2
        sc.op('dve', lambda e: e.tensor_tensor(out=utmp[sl][:], in0=self.X[t][:], in1=SC1[:], op=ALU.mult),
              reads=[f'X{t}', 'SC1'], writes=[f'utmp{sl}'])
        sc.op(POOL_ELEM, lambda e: e.tensor_tensor(out=ub[sl][:], in0=utmp[sl][:], in1=SH[:], op=ALU.add),
              reads=[f'utmp{sl}', 'SH'], writes=[f'ub{sl}'])
        pv = self.ps[pbank][:].bitcast(BF16)
        for k in range(8):
            sc.op('pe', lambda e, k=k: e.transpose(out=pv[:, k * 128:(k + 1) * 128], in_=ub[sl][:, k * 128:(k + 1) * 128],
                                                   identity=self.ident_b[:]),
                  reads=[f'ub{sl}', 'ident_b'], writes=[f'ps{pbank}'])
        sc.op('act', lambda e: e.activation(out=dstT[:, :, col0:col0 + 128], in_=pv.rearrange("p (k n) -> p k n", k=8),
                                            func=AF.Copy),
              reads=[f'ps{pbank}'], writes=[dst_key])

    def ffn_sublayer(self, b, l, s):
        sc = self.sc
        fi = 0 if s == 0 else 1
        with ExitStack() as es:
            SH, SC1, G1, LG, LB = self.modulation(es, b, l, s, 0.5)
            lnbuf = self.ln_bufs(es)
            utmp = [self.sb(es, f"utmp{i}", [128, D], F32) for i in range(2)]
            ub = [self.sb(es, f"ub{i}", [128, D], BF16) for i in range(2)]
            uT = self.sb(es, "uT", [128, 8, 1024], BF16)
            hT = self.sb(es, "hT", [128, 12, 1024], BF16)
            wgb = [self.sb(es, f"wgb{i}", [128, 8, 256], BF16) for i in range(3)]
            wub = [self.sb(es, f"wub{i}", [128, 8, 256], BF16) for i in range(3)]
            wdb = [self.sb(es, f"wdb{i}", [128, 2, D], BF16) for i in range(6)]
            sg = [self.sb(es, f"sg{i}", [128, 512], F32) for i in range(2)]
            t1 = [self.sb(es, f"t1_{i}", [128, 512], F32) for i in range(2)]
            cnt = 0
            self.dump('SC1', SC1[:], 'SC1', 0)
            self.dump('G1', G1[:], 'G1', 1024)
            for grp in range(2):
                for tl in range(8):
                    t = grp * 8 + tl
                    self.mod_transpose_tile(t, SC1, SH, utmp, ub, uT, f'uT{tl}', tl * 128, tl % 2)
                self.dump('uT', uT[:, :, 0:128], 'uT0', 2048)
                for ph in range(2):
                    blocks = list(range(0, 6)) if ph == 0 else list(range(6, 11))
                    for bi, blk in enumerate(blocks):
                        sl = cnt % 3
                        cnt += 1
                        c0 = blk * 256
                        sc.dma('pool', wgb[sl][:], self.W('ffn_w_gate', (l, fi), [D, DFF])[:, c0:c0 + 256].rearrange("(k p) n -> p k n", p=128),
                               writes=[f'wgb{sl}'], chan=f'wgb{sl}')
                        sc.dma('pool', wub[sl][:], self.W('ffn_w_up', (l, fi), [D, DFF])[:, c0:c0 + 256].rearrange("(k p) n -> p k n", p=128),
                               writes=[f'wub{sl}'], chan=f'wub{sl}')
                        sc.dma('pool', wdb[bi][:], self.W('ffn_w_down', (l, fi), [DFF, D])[c0:c0 + 256, :].rearrange("(k p) n -> p k n", p=128),
                               writes=[f'wdb{bi}'], chan=f'wdb{bi}')
                        for cc in range(2):
                            fl = bi * 2 + cc
                            for hf in range(2):
                                pg = 2 + hf
                                pu = 4 + hf
                                for k in range(8):
                                    sc.op('pe', lambda e, k=k: e.matmul(self.ps[pg][:], lhsT=wgb[sl][:, k, cc * 128:(cc + 1) * 128],
                                                                        rhs=uT[:, k, hf * 512:(hf + 1) * 512],
                                                                        start=(k == 0), stop=(k == 7)),
                                          reads=[f'wgb{sl}'] + [f'uT{i}' for i in range(hf * 4, hf * 4 + 4)], writes=[f'ps{pg}'])
                                for k in range(8):
                                    sc.op('pe', lambda e, k=k: e.matmul(self.ps[pu][:], lhsT=wub[sl][:, k, cc * 128:(cc + 1) * 128],
                                                                        rhs=uT[:, k, hf * 512:(hf + 1) * 512],
                                                                        start=(k == 0), stop=(k == 7)),
                                          reads=[f'wub{sl}'] + [f'uT{i}' for i in range(hf * 4, hf * 4 + 4)], writes=[f'ps{pu}'])
                                sc.op('act', lambda e: e.activation(out=sg[hf][:], in_=self.ps[pg][:], func=AF.Silu),
                                      reads=[f'ps{pg}'], writes=[f'sg{hf}'])
                                sc.op('dve', lambda e: e.tensor_tensor(out=hT[:, fl, hf * 512:(hf + 1) * 512], in0=sg[hf][:],
                                                                       in1=self.ps[pu][:], op=ALU.mult),
                                      reads=[f'sg{hf}', f'ps{pu}'], writes=[f'hT{fl}_{hf}'])
                    nfl = len(blocks) * 2
                    self.dump('hT', hT[:, 0, 0:512], 'hT0_0', 3072)
                    for tl in range(8):
                        t = grp * 8 + tl
                        for hf in range(2):
                            py = 6 + hf
                            for fl in range(nfl):
                                sc.op('pe', lambda e, fl=fl: e.matmul(self.ps[py][:], lhsT=hT[:, fl, tl * 128:(tl + 1) * 128],
                                                                      rhs=wdb[fl // 2][:, fl % 2, hf * 512:(hf + 1) * 512],
                                                                      start=(fl == 0), stop=(fl == nfl - 1)),
                                      reads=[f'hT{fl}_{tl // 4}', f'wdb{fl // 2}'], writes=[f'ps{py}'])
                            Xs = self.X[t][:, hf * 512:(hf + 1) * 512]
                            sc.op('dve', lambda e: e.tensor_tensor(out=t1[hf][:], in0=self.ps[py][:], in1=G1[:, hf * 512:(hf + 1) * 512],
                                                                   op=ALU.mult),
                                  reads=[f'ps{py}', 'G1'], writes=[f't1_{hf}'])
                            if hf == 0:
                                self.dump('t1', t1[0][:], 't1_0', 3584)
                            if ph == 0:
                                sc.op('dve', lambda e: e.scalar_tensor_tensor(out=Xs, in0=Xs, scalar=ALPHA, in1=t1[hf][:],
                                                                              op0=ALU.mult, op1=ALU.add),
                                      reads=[f'X{t}', f't1_{hf}'], writes=[f'X{t}'])
                            else:
                                sc.op(POOL_ELEM, lambda e: e.tensor_tensor(out=Xs, in0=Xs, in1=t1[hf][:], op=ALU.add),
                                      reads=[f'X{t}', f't1_{hf}'], writes=[f'X{t}'])
                        if ph == 1:
                            self.layer_norm_tile(t, LG, LB, lnbuf)
            sc.barrier()


    def mixer_tail(self, b, l, O, okey, wout_d):
        sc = self.sc
        with ExitStack() as es:
            _, _, G1, LG, LB = self.modulation(es, b, l, 1, 1.0, which=('G1', 'LG', 'LB'))
            lnbuf = self.ln_bufs(es)
            wo = self.sb(es, "wo", [128, 8, D], BF16)
            OT = [self.sb(es, f"OT{i}", [128, 8, 128], BF16) for i in range(2)]
            t1 = [self.sb(es, f"t1_{i}", [128, 512], F32) for i in range(2)]
            for hf in range(2):
                sc.dma('pool', wo[:, :, hf * 512:(hf + 1) * 512],
                       wout_d[:, hf * 512:(hf + 1) * 512].rearrange("(k p) n -> p k n", p=128), writes=[f'wo{hf}'], chan=f'wo{hf}')
            for t in range(NT):
                sl = t % 2
                pb = 4 + sl
                pv = self.ps[pb][:].bitcast(BF16)
                for k in range(8):
                    sc.op('pe', lambda e, k=k: e.transpose(out=pv[:, k * 128:(k + 1) * 128], in_=O[:, t, k * 128:(k + 1) * 128],
                                                           identity=self.ident_b[:]),
                          reads=[okey(t), 'ident_b'], writes=[f'ps{pb}'])
                sc.op('act', lambda e: e.activation(out=OT[sl][:], in_=pv.rearrange("p (k n) -> p k n", k=8), func=AF.Copy),
                      reads=[f'ps{pb}'], writes=[f'OT{sl}'])
                for hf in range(2):
                    py = 6 + hf
                    for k in range(8):
                        sc.op('pe', lambda e, k=k: e.matmul(self.ps[py][:], lhsT=OT[sl][:, k, :], rhs=wo[:, k, hf * 512:(hf + 1) * 512],
                                                            start=(k == 0), stop=(k == 7)),
                              reads=[f'OT{sl}', f'wo{hf}'], writes=[f'ps{py}'])
                    Xs = self.X[t][:, hf * 512:(hf + 1) * 512]
                    sc.op('dve', lambda e: e.tensor_tensor(out=t1[hf][:], in0=self.ps[py][:], in1=G1[:, hf * 512:(hf + 1) * 512], op=ALU.mult),
                          reads=[f'ps{py}', 'G1'], writes=[f't1_{hf}'])
                    sc.op('dve', lambda e: e.scalar_tensor_tensor(out=Xs, in0=Xs, scalar=ALPHA, in1=t1[hf][:], op0=ALU.mult, op1=ALU.add),
                          reads=[f'X{t}', f't1_{hf}'], writes=[f'X{t}'])
                self.layer_norm_tile(t, LG, LB, lnbuf)
            sc.barrier()

    def attn_tile(self, qT_ap, kT, kT_key, V, V_key, KV, scale, out_ap, out_key, bufs, maskblk, q_keys, bias=None, bias_key=None,
                  v_of=None, dv=None):
        sc = self.sc
        P, PT, st = bufs
        if v_of is None:
            dv = V.shape[2]
            v_of = lambda blk: (V[:, blk, :], V_key)
        Sp = self.psbig
        PSK = ['psS', 'ps0', 'ps1', 'ps2', 'ps3']
        nkc = (KV + 511) // 512
        for kc in range(nkc):
            c0 = kc * 512
            c1 = min(KV, c0 + 512)
            last = (kc == nkc - 1)
            sc.op('pe', lambda e: e.matmul(Sp[:, c0:c1], lhsT=qT_ap, rhs=kT[:, c0:c1], start=True,
                                           stop=(not last) or (maskblk is None)),
                  reads=list(q_keys) + [kT_key], writes=PSK)
        if maskblk is not None:
            sc.op('pe', lambda e: e.matmul(Sp[:, KV - 128:KV], lhsT=self.ident_b[:], rhs=maskblk[:], start=False, stop=True),
                  reads=['ident_b', 'maskblk'], writes=PSK)
        src = Sp[:, 0:KV]
        skeys = PSK
        if bias is not None:
            sc.op('dve', lambda e: e.tensor_tensor(out=bias[:, 0:KV], in0=Sp[:, 0:KV], in1=bias[:, 0:KV], op=ALU.add),
                  reads=PSK + [bias_key], writes=[bias_key])
            src = bias[:, 0:KV]
            skeys = [bias_key]
        sc.op('dve', lambda e: e.tensor_reduce(out=st[:, 0:1], in_=src, axis=AX.X, op=ALU.max), reads=skeys, writes=['at_m'])
        sc.op('dve', lambda e: e.tensor_scalar(out=st[:, 1:2], in0=st[:, 0:1], scalar1=-scale, scalar2=None, op0=ALU.mult),
              reads=['at_m'], writes=['at_nm'])
        sc.op('act', lambda e: e.activation(out=P[:, 0:KV], in_=src, func=AF.Exp, bias=st[:, 1:2], scale=scale, accum_out=st[:, 2:3]),
              reads=skeys + ['at_nm'], writes=['at_P', 'at_sum'])
        sc.op('dve', lambda e: e.reciprocal(out=st[:, 3:4], in_=st[:, 2:3]), reads=['at_sum'], writes=['at_rinv'])
        nblk = KV // 128
        for g in range((nblk + 7) // 8):
            pb = 4 + (g % 2)
            pv = self.ps[pb][:].bitcast(BF16)
            nb_ = min(8, nblk - g * 8)
            for j in range(nb_):
                blk = g * 8 + j
                sc.op('pe', lambda e, j=j, blk=blk: e.transpose(out=pv[:, j * 128:(j + 1) * 128], in_=P[:, blk * 128:(blk + 1) * 128],
                                                                identity=self.ident_b[:]),
                      reads=['at_P', 'ident_b'], writes=[f'ps{pb}'])
            eng = 'act' if g % 2 == 0 else 'dve'
            if eng == 'act':
                sc.op('act', lambda e: e.activation(out=PT[:, g * 8:g * 8 + nb_, :], in_=pv[:, 0:nb_ * 128].rearrange("p (k n) -> p k n", n=128),
                                                    func=AF.Copy), reads=[f'ps{pb}'], writes=[f'at_PT{g}'])
            else:
                sc.op('dve', lambda e: e.tensor_copy(out=PT[:, g * 8:g * 8 + nb_, :], in_=pv[:, 0:nb_ * 128].rearrange("p (k n) -> p k n", n=128)),
                      reads=[f'ps{pb}'], writes=[f'at_PT{g}'])
        po = self.ps[6]
        for blk in range(nblk):
            vap, vkey = v_of(blk)
            sc.op('pe', lambda e, blk=blk: e.matmul(po[:, 0:dv], lhsT=PT[:, blk, :], rhs=vap, start=(blk == 0), stop=(blk == nblk - 1)),
                  reads=[f'at_PT{blk // 8}', vkey], writes=['ps6'])
        sc.op('act', lambda e: e.activation(out=out_ap, in_=po[:, 0:dv], func=AF.Copy, scale=st[:, 3:4]),
              reads=['ps6', 'at_rinv'], writes=[out_key])

    def make_maskblk(self, es):
        sc = self.sc
        mi = self.sb(es, "mask_i", [128, 128], I32)
        mf = self.sb(es, "mask_f", [128, 128], F32)
        mb = self.sb(es, "maskblk", [128, 128], BF16)
        sc.op('pool', lambda e: e.iota(mi[:], pattern=[[1, 128]], base=0, channel_multiplier=0), writes=['mask_i'])
        sc.op('dve', lambda e: e.tensor_scalar(out=mf[:], in0=mi[:], scalar1=64.0, scalar2=None, op0=ALU.is_ge),
              reads=['mask_i'], writes=['mask_f'])
        sc.op('dve', lambda e: e.memset(mf[64:128, :], 0.0), reads=['mask_f'], writes=['mask_f'])
        sc.op('dve', lambda e: e.tensor_scalar(out=mb[:], in0=mf[:], scalar1=-1e30, scalar2=None, op0=ALU.mult),
              reads=['mask_f'], writes=['maskblk'])
        return mb

    def rope_tables(self, es, b):
        sc = self.sc
        COS = self.sb(es, "COS", [32, S], F32)
        SINS = self.sb(es, "SINS", [32, S], F32)
        with ExitStack() as es2:
            pi_ = self.sb(es2, "rp_pi", [32, S], I32)
            ang = self.sb(es2, "rp_ang", [32, S], F32)
            kf = self.sb(es2, "rp_kf", [32, S], F32)
            ki = self.sb(es2, "rp_ki", [32, S], I32)
            r = self.sb(es2, "rp_r", [32, S], F32)
            m = self.sb(es2, "rp_m", [32, S], F32)
            fi = self.sb(es2, "rp_fi", [32, 1], I32)
            ff = self.sb(es2, "rp_ff", [32, 4], F32)
            sc.dma('sp', pi_[:], self.pos[b:b + 1, :].partition_broadcast(32), writes=['rp_pi'], chan='misc')
            sc.op('pool', lambda e: e.iota(fi[:], pattern=[[0, 1]], base=0, channel_multiplier=1), writes=['rp_fi'])
            sc.op('dve', lambda e: e.tensor_copy(out=ff[:, 0:1], in_=fi[:]), reads=['rp_fi'], writes=['rp_ff'])
            sc.op('dve', lambda e: e.tensor_scalar(out=ff[:, 1:2], in0=ff[:, 0:1], scalar1=16.0, scalar2=-16.0, op0=ALU.is_ge, op1=ALU.mult),
                  reads=['rp_ff'], writes=['rp_ff'])
            sc.op('dve', lambda e: e.tensor_tensor(out=ff[:, 2:3], in0=ff[:, 0:1], in1=ff[:, 1:2], op=ALU.add), reads=['rp_ff'], writes=['rp_ff'])
            sc.op('act', lambda e: e.activation(out=ff[:, 3:4], in_=ff[:, 2:3], func=AF.Exp, scale=-float(np.log(10000.0)) / 16.0),
                  reads=['rp_ff'], writes=['rp_ff'])
            sc.op('dve', lambda e: e.tensor_copy(out=ang[:], in_=pi_[:]), reads=['rp_pi'], writes=['rp_ang'])
            sc.op('dve', lambda e: e.tensor_scalar(out=ang[:], in0=ang[:], scalar1=ff[:, 3:4], scalar2=None, op0=ALU.mult),
                  reads=['rp_ang', 'rp_ff'], writes=['rp_ang'])
            TWO_PI = float(2 * np.pi)
            C1 = 6.28125
            C2 = float(2 * np.pi - 6.28125)
            sc.op('dve', lambda e: e.tensor_scalar(out=kf[:], in0=ang[:], scalar1=1.0 / TWO_PI, scalar2=None, op0=ALU.mult),
                  reads=['rp_ang'], writes=['rp_kf'])
            sc.op('dve', lambda e: e.tensor_copy(out=ki[:], in_=kf[:]), reads=['rp_kf'], writes=['rp_ki'])
            sc.op('dve', lambda e: e.tensor_copy(out=kf[:], in_=ki[:]), reads=['rp_ki'], writes=['rp_kf'])
            sc.op('dve', lambda e: e.scalar_tensor_tensor(out=r[:], in0=kf[:], scalar=-C1, in1=ang[:], op0=ALU.mult, op1=ALU.add),
                  reads=['rp_kf', 'rp_ang'], writes=['rp_r'])
            sc.op('dve', lambda e: e.scalar_tensor_tensor(out=r[:], in0=kf[:], scalar=-C2, in1=r[:], op0=ALU.mult, op1=ALU.add),
                  reads=['rp_kf', 'rp_r'], writes=['rp_r'])

            def wrap(buf, key):
                sc.op('dve', lambda e: e.tensor_scalar(out=m[:], in0=buf[:], scalar1=float(np.pi), scalar2=-TWO_PI, op0=ALU.is_gt, op1=ALU.mult),
                      reads=[key], writes=['rp_m'])
                sc.op('dve', lambda e: e.tensor_tensor(out=buf[:], in0=buf[:], in1=m[:], op=ALU.add), reads=[key, 'rp_m'], writes=[key])
                sc.op('dve', lambda e: e.tensor_scalar(out=m[:], in0=buf[:], scalar1=-float(np.pi), scalar2=TWO_PI, op0=ALU.is_lt, op1=ALU.mult),
                      reads=[key], writes=['rp_m'])
                sc.op('dve', lambda e: e.tensor_tensor(out=buf[:], in0=buf[:], in1=m[:], op=ALU.add), reads=[key, 'rp_m'], writes=[key])
            wrap(r, 'rp_r')
            sc.op('act', lambda e: e.activation(out=SINS[:], in_=r[:], func=AF.Sin), reads=['rp_r'], writes=['SINS'])
            sc.op('dve', lambda e: e.tensor_scalar(out=ff[:, 1:2], in0=ff[:, 0:1], scalar1=16.0, scalar2=2.0, op0=ALU.is_ge, op1=ALU.mult),
                  reads=['rp_ff'], writes=['rp_ff'])
            sc.op('dve', lambda e: e.tensor_scalar(out=ff[:, 1:2], in0=ff[:, 1:2], scalar1=-1.0, scalar2=None, op0=ALU.add),
                  reads=['rp_ff'], writes=['rp_ff'])
            sc.op('dve', lambda e: e.tensor_scalar(out=SINS[:], in0=SINS[:], scalar1=ff[:, 1:2], scalar2=None, op0=ALU.mult),
                  reads=['SINS', 'rp_ff'], writes=['SINS'])
            sc.op('dve', lambda e: e.tensor_scalar(out=r[:], in0=r[:], scalar1=float(np.pi / 2), scalar2=None, op0=ALU.add),
                  reads=['rp_r'], writes=['rp_r'])
            wrap(r, 'rp_r')
            sc.op('act', lambda e: e.activation(out=COS[:], in_=r[:], func=AF.Sin), reads=['rp_r'], writes=['COS'])
            sc.barrier()
        return COS, SINS

    def mla_sublayer(self, b, l):
        sc = self.sc
        o = l // 2
        HD = 96
        scale = float(HD ** -0.5)
        with ExitStack() as esO:
            O = self.sb(esO, "O", [128, NT, D], BF16)
            with ExitStack() as es:
                cnT = self.sb(es, "cnT", [128, 5, S], BF16)
                krT = self.sb(es, "krT", [32, S], BF16)
                COS, SINS = self.rope_tables(es, b)
                with ExitStack() as esA:
                    SH, SC1, _, _, _ = self.modulation(esA, b, l, 1, 1.0, which=('SH', 'SC1'))
                    utmp = [self.sb(esA, f"utmp{i}", [128, D], F32) for i in range(2)]
                    ub = [self.sb(esA, f"ub{i}", [128, D], BF16) for i in range(2)]
                    uT4 = self.sb(esA, "uT4", [128, 8, 512], BF16)
                    win = self.sb(esA, "win", [128, 8, 672], BF16)
                    winsw = self.sb(esA, "winsw", [128, 8, 32], BF16)
                    GQ = self.sb(esA, "GQ", [128, 640], F32)
                    cn = [self.sb(esA, f"cn{i}", [128, 640], BF16) for i in range(2)]
                    junk = self.sb(esA, "junk", [128, 384], F32)
                    rs = self.sb(esA, "rs", [128, 8], F32)
                    eps_r = self.sb(esA, "eps_r", [128, 1], F32)
                    rt = [self.sb(esA, f"rt{i}", [32, 512], F32) for i in range(2)]
                    wd_ = self.W('mla_w_in', (o,), [D, 672])
                    sc.dma('pool', win[:], wd_.rearrange("(k p) n -> p k n", p=128), writes=['win'], chan='win')
                    sc.dma('pool', winsw[:, :, 0:16], wd_[:, 656:672].rearrange("(k p) n -> p k n", p=128), writes=['winsw'], chan='win')
                    sc.dma('pool', winsw[:, :, 16:32], wd_[:, 640:656].rearrange("(k p) n -> p k n", p=128), writes=['winsw'], chan='win')
                    sc.dma('sp', GQ[:, 0:384], self.W('mla_q_norm_g', (o,), [1, 384]).partition_broadcast(128), writes=['GQ'], chan='misc')
                    sc.dma('sp', GQ[:, 384:640], self.W('mla_kv_norm_g', (o,), [1, 256]).partition_broadcast(128), writes=['GQ'], chan='misc')
                    sc.op('dve', lambda e: e.memset(eps_r[:], RMS_EPS), writes=['eps_r'])
                    for g4 in range(4):
                        for tl in range(4):
                            t = g4 * 4 + tl
                            self.mod_transpose_tile(t, SC1, SH, utmp, ub, uT4, f'uT{tl}', tl * 128, 4 + t % 2)
                            for (c0, c1, pb) in ((0, 512, 0), (512, 640, 1)):
                                for k in range(8):
                                    sc.op('pe', lambda e, k=k: e.matmul(self.ps[pb][:, 0:c1 - c0], lhsT=uT4[:, k, tl * 128:(tl + 1) * 128],
                                                                        rhs=win[:, k, c0:c1], start=(k == 0), stop=(k == 7)),
                                          reads=[f'uT{tl}', 'win'], writes=[f'ps{pb}'])
                            cps = self.psbig[:, 0:640]
                            sc.op('act', lambda e: e.activation(out=junk[:, 0:384], in_=cps[:, 0:384], func=AF.Square, accum_out=rs[:, 0:1]),
                                  reads=['ps0'], writes=['junk', 'rs0'])
                            sc.op('act', lambda e: e.activation(out=junk[:, 0:256], in_=cps[:, 384:640], func=AF.Square, accum_out=rs[:, 1:2]),
                                  reads=['ps0', 'ps1'], writes=['junk', 'rs1'])
                            sc.op('act', lambda e: e.activation(out=rs[:, 2:3], in_=rs[:, 0:1], func=AF.Sqrt, bias=eps_r[:], scale=1.0 / 384),
                                  reads=['rs0', 'eps_r'], writes=['rs2'])
                            sc.op('act', lambda e: e.activation(out=rs[:, 3:4], in_=rs[:, 1:2], func=AF.Sqrt, bias=eps_r[:], scale=1.0 / 256),
                                  reads=['rs1', 'eps_r'], writes=['rs3'])
                            sc.op('dve', lambda e: e.reciprocal(out=rs[:, 4:6], in_=rs[:, 2:4]), reads=['rs2', 'rs3'], writes=['rs4'])
                            sl = t % 2
                            sc.op('dve', lambda e: e.scalar_tensor_tensor(out=cn[sl][:, 0:384], in0=cps[:, 0:384], scalar=rs[:, 4:5], in1=GQ[:, 0:384],
                                                                          op0=ALU.mult, op1=ALU.mult),
                                  reads=['ps0', 'rs4', 'GQ'], writes=[f'cn{sl}'])
                            sc.op('dve', lambda e: e.scalar_tensor_tensor(out=cn[sl][:, 384:640], in0=cps[:, 384:640], scalar=rs[:, 5:6], in1=GQ[:, 384:640],
                                                                          op0=ALU.mult, op1=ALU.mult),
                                  reads=['ps0', 'ps1', 'rs4', 'GQ'], writes=[f'cn{sl}'])
                            pb = 2 + sl
                            pv = self.ps[pb][:].bitcast(BF16)
                            for k in range(5):
                                sc.op('pe', lambda e, k=k: e.transpose(out=pv[:, k * 128:(k + 1) * 128], in_=cn[sl][:, k * 128:(k + 1) * 128],
                                                                       identity=self.ident_b[:]),
                                      reads=[f'cn{sl}', 'ident_b'], writes=[f'ps{pb}'])
                            sc.op('act', lambda e: e.activation(out=cnT[:, :, t * 128:(t + 1) * 128],
                                                                in_=pv[:, 0:640].rearrange("p (k n) -> p k n", k=5), func=AF.Copy),
                                  reads=[f'ps{pb}'], writes=[f'cnT{t}'])
                        cols = slice(g4 * 512, (g4 + 1) * 512)
                        for k in range(8):
                            sc.op('pe', lambda e, k=k: e.matmul(self.ps[6][0:32, :], lhsT=win[:, k, 640:672], rhs=uT4[:, k, :],
                                                                start=(k == 0), stop=(k == 7)),
                                  reads=['win'] + [f'uT{i}' for i in range(4)], writes=['ps6'])
                        for k in range(8):
                            sc.op('pe', lambda e, k=k: e.matmul(self.ps[7][0:32, :], lhsT=winsw[:, k, :], rhs=uT4[:, k, :],
                                                                start=(k == 0), stop=(k == 7)),
                                  reads=['winsw'] + [f'uT{i}' for i in range(4)], writes=['ps7'])
                        sc.op('dve', lambda e: e.tensor_tensor(out=rt[0][:], in0=self.ps[6][0:32, :], in1=COS[:, cols], op=ALU.mult),
                              reads=['ps6', 'COS'], writes=['rt0'])
                        sc.op('dve', lambda e: e.tensor_tensor(out=rt[1][:], in0=self.ps[7][0:32, :], in1=SINS[:, cols], op=ALU.mult),
                              reads=['ps7', 'SINS'], writes=['rt1'])
                        sc.op('dve', lambda e: e.tensor_tensor(out=krT[:, cols], in0=rt[0][:], in1=rt[1][:], op=ALU.add),
                              reads=['rt0', 'rt1'], writes=['krT'])
                    sc.barrier()
                with ExitStack() as esB:
                    wq = self.sb(esB, "wq", [128, 3, 16, 128], BF16)
                    wkv = self.sb(esB, "wkv", [128, 2, 16, 160], BF16)
                    qT = [self.sb(esB, f"qT{i}", [96, S], BF16) for i in range(2)]
                    kT = [self.sb(esB, f"kT{i}", [96, S], BF16) for i in range(2)]
                    Vh = [self.sb(esB, f"Vh{i}", [128, NT, 64], BF16) for i in range(2)]
                    P = self.sb(esB, "P", [128, S], BF16)
                    PT = self.sb(esB, "PT", [128, 16, 128], BF16)
                    st = self.sb(esB, "at_st", [128, 4], F32)
                    rt = [self.sb(esB, f"rt{i}", [32, 512], F32) for i in range(2)]
                    maskblk = self.make_maskblk(esB)
                    wqd = self.W('mla_w_q_up', (o,), [384, 1536]).rearrange("(k p) (h d) -> p k h d", p=128, d=96)
                    wkvd = self.W('mla_w_kv_up', (o,), [256, 2048]).rearrange("(k p) (h d) -> p k h d", p=128, d=128)
                    for k in range(3):
                        sc.dma('pool', wq[:, k, :, 0:32], wqd[:, k, :, 64:96], writes=['wq'], chan='wq')
                        sc.dma('pool', wq[:, k, :, 32:96], wqd[:, k, :, 0:64], writes=['wq'], chan='wq')
                        sc.dma('pool', wq[:, k, :, 96:112], wqd[:, k, :, 80:96], writes=['wq'], chan='wq')
                        sc.dma('pool', wq[:, k, :, 112:128], wqd[:, k, :, 64:80], writes=['wq'], chan='wq')
                    sc.op('dve', lambda e: e.memset(wkv[:], 0.0), writes=['wkv'])
                    for k in range(2):
                        sc.dma('pool', wkv[:, k, :, 32:160], wkvd[:, k, :, :], writes=['wkv'], chan='wkv')
                    for h in range(16):
                        sl = h % 2
                        for g4 in range(4):
                            cols = slice(g4 * 512, (g4 + 1) * 512)
                            for k in range(3):
                                sc.op('pe', lambda e, k=k: e.matmul(self.ps[7][0:96, :], lhsT=wq[:, k, h, 0:96], rhs=cnT[:, k, cols],
                                                                    start=(k == 0), stop=(k == 2)),
                                      reads=['wq'] + [f'cnT{t}' for t in range(g4 * 4, g4 * 4 + 4)], writes=['ps7'])
                            for k in range(3):
                                sc.op('pe', lambda e, k=k: e.matmul(self.ps[6][0:32, :], lhsT=wq[:, k, h, 96:128], rhs=cnT[:, k, cols],
                                                                    start=(k == 0), stop=(k == 2)),
                                      reads=['wq'] + [f'cnT{t}' for t in range(g4 * 4, g4 * 4 + 4)], writes=['ps6'])
                            sc.op('dve', lambda e: e.tensor_tensor(out=rt[0][:], in0=self.ps[7][0:32, :], in1=COS[:, cols], op=ALU.mult),
                                  reads=['ps7', 'COS'], writes=['rt0'])
                            sc.op('dve', lambda e: e.tensor_tensor(out=rt[1][:], in0=self.ps[6][0:32, :], in1=SINS[:, cols], op=ALU.mult),
                                  reads=['ps6', 'SINS'], writes=['rt1'])
                            sc.op('dve', lambda e: e.tensor_tensor(out=qT[sl][0:32, cols], in0=rt[0][:], in1=rt[1][:], op=ALU.add),
                                  reads=['rt0', 'rt1'], writes=[f'qT{sl}'])
                            for (p0, p1) in ((32, 64), (64, 96)):
                                sc.op('act', lambda e: e.activation(out=qT[sl][p0:p1, cols], in_=self.ps[7][p0:p1, :], func=AF.Copy),
                                      reads=['ps7'], writes=[f'qT{sl}'])
                            for k in range(2):
                                sc.op('pe', lambda e, k=k: e.matmul(self.ps[7][0:96, :], lhsT=wkv[:, k, h, 0:96], rhs=cnT[:, 3 + k, cols],
                                                                    start=(k == 0), stop=(k == 1)),
                                      reads=['wkv'] + [f'cnT{t}' for t in range(g4 * 4, g4 * 4 + 4)], writes=['ps7'])
                            for (p0, p1) in ((32, 64), (64, 96)):
                                sc.op('act', lambda e: e.activation(out=kT[sl][p0:p1, cols], in_=self.ps[7][p0:p1, :], func=AF.Copy),
                                      reads=['ps7'], writes=[f'kT{sl}'])
                        sc.op('pool', lambda e: e.tensor_copy(out=kT[sl][0:32, :], in_=krT[:]), reads=['krT'], writes=[f'kT{sl}'])
                        for g8 in range(2):
                            pb = 4 + g8
                            for tl in range(8):
                                t = g8 * 8 + tl
                                for k in range(2):
                                    sc.op('pe', lambda e, k=k: e.matmul(self.ps[pb][:, tl * 64:(tl + 1) * 64], lhsT=cnT[:, 3 + k, t * 128:(t + 1) * 128],
                                                                        rhs=wkv[:, k, h, 96:160], start=(k == 0), stop=(k == 1)),
                                          reads=['wkv', f'cnT{t}'], writes=[f'ps{pb}'])
                            sc.op('act', lambda e: e.activation(out=Vh[sl][:, g8 * 8:(g8 + 1) * 8, :],
                                                                in_=self.ps[pb][:].rearrange("p (t d) -> p t d", d=64), func=AF.Copy),
                                  reads=[f'ps{pb}'], writes=[f'Vh{sl}'])
                        for qt in range(NT):
                            self.attn_tile(qT[sl][:, qt * 128:(qt + 1) * 128], kT[sl], f'kT{sl}', Vh[sl], f'Vh{sl}', (qt + 1) * 128, scale,
                                           O[:, qt, h * 64:(h + 1) * 64], f'O{qt}', (P, PT, st), maskblk, [f'qT{sl}'])
                    sc.barrier()
            self.mixer_tail(b, l, O, lambda t: f'O{t}', self.W('mla_w_out', (o,), [D, D]))


    def hyb_sublayer(self, b, l):
        sc = self.sc
        e_ = l // 2
        if not hasattr(self, 'xspill'):
            self.xspill = self.nc.dram_tensor("xspill", [S, D], F32, kind="Internal").ap()
        win_d = self.W('hyb_w_in', (e_,), [D, 4176])
        with ExitStack() as esO:
            O = self.sb(esO, "O", [128, NT, D], BF16)
            uT = self.sb(esO, "uT_all", [128, 8, S], BF16)
            with ExitStack() as esA:
                SH, SC1, _, _, _ = self.modulation(esA, b, l, 1, 1.0, which=('SH', 'SC1'))
                utmp = [self.sb(esA, f"utmp{i}", [128, D], F32) for i in range(2)]
                ub = [self.sb(esA, f"ub{i}", [128, D], BF16) for i in range(2)]
                for t in range(NT):
                    self.mod_transpose_tile(t, SC1, SH, utmp, ub, uT, f'uT{t}', t * 128, 4 + t % 2)
                    sc.dma('sp', self.xspill[t * 128:(t + 1) * 128, :], self.X[t][:], reads=[f'X{t}'], writes=[f'xsp{t}'], chan='xsp')
                sc.barrier()
            Xb = [self.X[t][:].bitcast(BF16) for t in range(NT)]
            uTk = [f'uT{t}' for t in range(NT)]
            if HYB_DSA:
                self.dsa_part(b, l, e_, win_d, O, uT, Xb, uTk)
            else:
                for t in range(NT):
                    sc.op('dve', lambda e: e.memset(O[:, t, 0:512], 0.0), writes=[f'O{t}'])
            if HYB_GDN:
                self.gdn_part(b, l, e_, win_d, O, uT, Xb, uTk)
            else:
                for t in range(NT):
                    sc.op('dve', lambda e: e.memset(O[:, t, 512:1024], 0.0), writes=[f'O{t}'])
            sc.barrier()
            for t in range(NT):
                sc.dma('sp', self.X[t][:], self.xspill[t * 128:(t + 1) * 128, :], writes=[f'X{t}'], chan='xin')
            with ExitStack() as esT:
                pass
            self.mixer_tail(b, l, O, lambda t: f'O{t}', self.W('hyb_w_out', (e_,), [D, D]))

    def proj_featmajor(self, wchunk, wkeys, loads, uT, uTk, dst_of, evac_scale=1.0):
        sc = self.sc
        for ci, (pieces, dst, dkey) in enumerate(loads):
            sl = self._wc % 2
            self._wc += 1
            for (d0, d1, srcap) in pieces:
                sc.dma('pool', wchunk[sl][:, :, d0:d1], srcap.rearrange("(k p) n -> p k n", p=128), writes=[wkeys[sl]], chan=wkeys[sl])
            for g4 in range(4):
                pb = 4 + (g4 % 2)
                for k in range(8):
                    sc.op('pe', lambda e, k=k: e.matmul(self.ps[pb][:], lhsT=wchunk[sl][:, k, :], rhs=uT[:, k, g4 * 512:(g4 + 1) * 512],
                                                        start=(k == 0), stop=(k == 7)),
                          reads=[wkeys[sl]] + uTk[g4 * 4:g4 * 4 + 4], writes=[f'ps{pb}'])
                if g4 % 2 == 0:
                    sc.op('act', lambda e: e.activation(out=dst[:, g4 * 512:(g4 + 1) * 512], in_=self.ps[pb][:], func=AF.Copy, scale=evac_scale),
                          reads=[f'ps{pb}'], writes=[dkey])
                else:
                    sc.op('dve', lambda e: e.tensor_scalar(out=dst[:, g4 * 512:(g4 + 1) * 512], in0=self.ps[pb][:], scalar1=evac_scale, scalar2=None,
                                                           op0=ALU.mult), reads=[f'ps{pb}'], writes=[dkey])

    def dsa_part(self, b, l, e_, win_d, O, uT, Xb, uTk):
        sc = self.sc
        self._wc = 0
        with ExitStack() as es:
            wchunk = [self.sb(es, f"wch{i}", [128, 8, 128], BF16) for i in range(2)]
            wkeys = ['wch0', 'wch1']
            ikT2 = self.sb(es, "ikT2", [128, S], BF16)
            iw = self.sb(es, "iw", [128, NT, 8], F32)
            posrow = self.sb(es, "posrow", [128, S], F32)
            poscol = self.sb(es, "poscol", [128, NT], F32)
            acc = self.sb(es, "acc", [128, S], F32)
            MB = self.sb(es, "MB", [128, S], F32)
            Dt = self.sb(es, "Dt", [128, S], F32)
            bh = [self.sb(es, f"bh{i}", [128, S], F32) for i in range(2)]
            rl = [self.sb(es, f"rl{i}", [128, 512], F32) for i in range(2)]
            P = self.sb(es, "P", [128, S], BF16)
            junk = P
            PT = self.sb(es, "PT", [128, 16, 128], BF16)
            st = self.sb(es, "at_st", [128, 4], F32)
            bs = self.sb(es, "bs", [128, 8], F32)
            maskf = self.sb(es, "maskf", [128, 128], F32)
            with ExitStack() as es2:
                pri = Dt[:].bitcast(I32)
                pci = self.sb(es2, "pci", [128, NT], I32)
                mi = self.sb(es2, "mi", [128, 128], I32)
                sc.dma('sp', pri, self.pos[b:b + 1, :].partition_broadcast(128), writes=['pri'], chan='misc')
                sc.dma('sp', pci[:], self.pos[b].rearrange("(t p) -> p t", p=128), writes=['pci'], chan='misc', allow_slow_non_contiguous=True)
                sc.op('dve', lambda e: e.tensor_copy(out=posrow[:], in_=pri), reads=['pri'], writes=['posrow'])
                sc.op('dve', lambda e: e.tensor_copy(out=poscol[:], in_=pci[:]), reads=['pci'], writes=['poscol'])
                sc.op('dve', lambda e: e.tensor_scalar(out=poscol[:], in0=poscol[:], scalar1=-1.0, scalar2=None, op0=ALU.mult),
                      reads=['poscol'], writes=['poscol'])
                sc.op('pool', lambda e: e.iota(mi[:], pattern=[[1, 128]], base=0, channel_multiplier=0), writes=['mi'])
                sc.op('dve', lambda e: e.tensor_scalar(out=maskf[:], in0=mi[:], scalar1=64.0, scalar2=-1e30, op0=ALU.is_ge, op1=ALU.mult),
                      reads=['mi'], writes=['maskf'])
                sc.op('dve', lambda e: e.memset(maskf[64:128, :], 0.0), reads=['maskf'], writes=['maskf'])
                sc.barrier()
            loads = []
            for c in range(4):
                loads.append(([(0, 128, win_d[:, c * 128:(c + 1) * 128])], Xb[c], f'X{c}'))
            self.proj_featmajor(wchunk, wkeys, loads, uT, uTk, None, evac_scale=0.125)
            loads = []
            for c in range(4):
                loads.append(([(0, 128, win_d[:, 512 + c * 128:512 + (c + 1) * 128])], Xb[4 + c], f'X{4 + c}'))
            for c in range(4):
                loads.append(([(0, 128, win_d[:, 1536 + c * 128:1536 + (c + 1) * 128])], Xb[8 + c], f'X{8 + c}'))
            loads.append(([(0, 64, win_d[:, 2048:2112]), (64, 128, win_d[:, 2048:2112])], ikT2, 'ikT2'))
            self.proj_featmajor(wchunk, wkeys, loads, uT, uTk, None)
            with ExitStack() as es2:
                wv = MB[:].bitcast(BF16).rearrange("p (k n) -> p k n", k=8)
                wiw = self.sb(es2, "wiw", [128, 8, 8], BF16)
                sc.dma('pool', wv, win_d[:, 1024:1536].rearrange("(k p) n -> p k n", p=128), writes=['wv'], chan='wv')
                sc.dma('pool', wiw[:], win_d[:, 2112:2120].rearrange("(k p) n -> p k n", p=128), writes=['wiw'], chan='wv')
                for t in range(NT):
                    pb = 4 + t % 2
                    for k in range(8):
                        sc.op('pe', lambda e, k=k: e.matmul(self.ps[pb][:], lhsT=uT[:, k, t * 128:(t + 1) * 128], rhs=wv[:, k, :],
                                                            start=(k == 0), stop=(k == 7)), reads=[uTk[t], 'wv'], writes=[f'ps{pb}'])
                    sc.op('act', lambda e: e.activation(out=Xb[12 + t // 4][:, (t % 4) * 512:(t % 4 + 1) * 512], in_=self.ps[pb][:], func=AF.Copy),
                          reads=[f'ps{pb}'], writes=[f'X{12 + t // 4}'])
                    pb2 = 6 + t % 2
                    for k in range(8):
                        sc.op('pe', lambda e, k=k: e.matmul(self.ps[pb2][:, 0:8], lhsT=uT[:, k, t * 128:(t + 1) * 128], rhs=wiw[:, k, :],
                                                            start=(k == 0), stop=(k == 7)), reads=[uTk[t], 'wiw'], writes=[f'ps{pb2}'])
                    sc.op('dve', lambda e: e.tensor_copy(out=iw[:, t, :], in_=self.ps[pb2][:, 0:8]), reads=[f'ps{pb2}'], writes=['iw'])
                sc.barrier()
            NIT = 14
            for qt in range(NT):
                KV = (qt + 1) * 128
                qs = slice(qt * 128, (qt + 1) * 128)
                nkc = (KV + 511) // 512
                bank = 0
                for kc in range(nkc):
                    c0 = kc * 512
                    c1 = min(KV, c0 + 512)
                    for h in range(8):
                        pb = bank % 4
                        bank += 1
                        hp = slice((h % 2) * 64, (h % 2) * 64 + 64)
                        sc.op('pe', lambda e: e.matmul(self.ps[pb][:, 0:c1 - c0], lhsT=Xb[8 + h // 2][hp, qs], rhs=ikT2[hp, c0:c1],
                                                       start=True, stop=True),
                              reads=[f'X{8 + h // 2}', 'ikT2'], writes=[f'ps{pb}', 'psS'])
                        rs_ = h % 2
                        sc.op('act', lambda e: e.activation(out=rl[rs_][:, 0:c1 - c0], in_=self.ps[pb][:, 0:c1 - c0], func=AF.Relu),
                              reads=[f'ps{pb}'], writes=[f'rl{rs_}'])
                        if h == 0:
                            sc.op('dve', lambda e: e.tensor_scalar(out=acc[:, c0:c1], in0=rl[rs_][:, 0:c1 - c0], scalar1=iw[:, qt, 0:1], scalar2=None,
                                                                   op0=ALU.mult), reads=[f'rl{rs_}', 'iw'], writes=['acc'])
                        else:
                            sc.op('dve', lambda e: e.scalar_tensor_tensor(out=acc[:, c0:c1], in0=rl[rs_][:, 0:c1 - c0], scalar=iw[:, qt, h:h + 1],
                                                                          in1=acc[:, c0:c1], op0=ALU.mult, op1=ALU.add),
                                  reads=[f'rl{rs_}', 'iw', 'acc'], writes=['acc'])
                if KV > 256:
                    sc.op('dve', lambda e: e.tensor_reduce(out=bs[:, 0:1], in_=acc[:, 0:KV], axis=AX.X, op=ALU.max), reads=['acc'], writes=['bs_hi'])
                    sc.op('dve', lambda e: e.tensor_reduce(out=bs[:, 1:2], in_=acc[:, 0:KV], axis=AX.X, op=ALU.min), reads=['acc'], writes=['bs_lo'])
                    sc.op('dve', lambda e: e.tensor_tensor(out=bs[:, 2:3], in0=bs[:, 0:1], in1=bs[:, 1:2], op=ALU.subtract),
                          reads=['bs_hi', 'bs_lo'], writes=['bs_h'])
                sc.op('dve', lambda e: e.tensor_tensor(out=acc[:, KV - 128:KV], in0=acc[:, KV - 128:KV], in1=maskf[:], op=ALU.add),
                      reads=['acc', 'maskf'], writes=['acc'])
                if KV > 256:
                    for it in range(NIT):
                        sc.op('dve', lambda e: e.tensor_scalar(out=bs[:, 2:3], in0=bs[:, 2:3], scalar1=0.5, scalar2=None, op0=ALU.mult),
                              reads=['bs_h'], writes=['bs_h'])
                        sc.op('dve', lambda e: e.tensor_tensor(out=bs[:, 3:4], in0=bs[:, 1:2], in1=bs[:, 2:3], op=ALU.add),
                              reads=['bs_lo', 'bs_h'], writes=['bs_mid'])
                        sc.op('dve', lambda e: e.tensor_scalar(out=junk[:, 0:KV], in0=acc[:, 0:KV], scalar1=bs[:, 3:4], scalar2=0.0,
                                                               op0=ALU.is_ge, op1=ALU.add, accum_out=bs[:, 4:5]),
                              reads=['acc', 'bs_mid'], writes=['at_P', 'bs_cnt'])
                        sc.op('dve', lambda e: e.tensor_scalar(out=bs[:, 5:6], in0=bs[:, 4:5], scalar1=255.5, scalar2=bs[:, 2:3],
                                                               op0=ALU.is_ge, op1=ALU.mult), reads=['bs_cnt', 'bs_h'], writes=['bs_g'])
                        sc.op('dve', lambda e: e.tensor_tensor(out=bs[:, 1:2], in0=bs[:, 1:2], in1=bs[:, 5:6], op=ALU.add),
                              reads=['bs_lo', 'bs_g'], writes=['bs_lo'])
                    thr = bs[:, 1:2]
                else:
                    sc.op('dve', lambda e: e.memset(bs[:, 1:2], -1e29), writes=['bs_lo'])
                    thr = bs[:, 1:2]
                sc.op('dve', lambda e: e.tensor_scalar(out=MB[:, 0:KV], in0=acc[:, 0:KV], scalar1=thr, scalar2=-1e30, op0=ALU.is_lt, op1=ALU.mult),
                      reads=['acc', 'bs_lo'], writes=['MB'])
                sc.op('act', lambda e: e.activation(out=Dt[:, 0:KV], in_=posrow[:, 0:KV], func=AF.Abs, bias=poscol[:, qt:qt + 1], scale=1.0),
                      reads=['posrow', 'poscol'], writes=['Dt'])
                for h in range(8):
                    sl = h % 2
                    slope = float(2.0 ** (-(h + 1)))
                    sc.op('dve', lambda e: e.scalar_tensor_tensor(out=bh[sl][:, 0:KV], in0=Dt[:, 0:KV], scalar=-slope, in1=MB[:, 0:KV],
                                                                  op0=ALU.mult, op1=ALU.add), reads=['Dt', 'MB'], writes=[f'bh{sl}'])
                    hp = slice((h % 2) * 64, (h % 2) * 64 + 64)
                    self.attn_tile(Xb[h // 2][hp, qs], Xb[4 + h // 2][hp, :], f'X{4 + h // 2}', None, None, KV, 1.0,
                                   O[:, qt, h * 64:(h + 1) * 64], f'O{qt}', (P, PT, st), None, [f'X{h // 2}'],
                                   bias=bh[sl], bias_key=f'bh{sl}',
                                   v_of=lambda blk, h=h: (Xb[12 + blk // 4][:, (blk % 4) * 512 + h * 64:(blk % 4) * 512 + (h + 1) * 64], f'X{12 + blk // 4}'),
                                   dv=64)
            sc.barrier()


    def gdn_part(self, b, l, e_, win_d, O, uT, Xb, uTk):
        sc = self.sc
        self._wc = 0
        T_ = slice
        with ExitStack() as es:
            U = self.sb(es, "gU", [128, 128], F32)
            Li = self.sb(es, "gLi", [128, 128], F32)
            Ls = self.sb(es, "gLs", [128, 128], F32)
            ones = self.sb(es, "gones", [128, 128], F32)
            NG = self.sb(es, "gNG", [128, 128], F32)
            cw = self.sb(es, "gcw", [128, 12, 4], F32)
            prm = self.sb(es, "gprm", [128, 16], F32)
            epsb = self.sb(es, "gepsb", [128, 4], F32)
            ab = self.sb(es, "gab", [128, NT, 8], F32)
            gg = self.sb(es, "ggg", [128, NT, 4], F32)
            bt = self.sb(es, "gbt", [128, NT, 4], F32)
            sc.op('dve', lambda e: e.tensor_scalar(out=U[:], in0=self.iot[:], scalar1=0.0, scalar2=None, op0=ALU.is_ge), reads=['iot'], writes=['gU'])
            sc.op('dve', lambda e: e.tensor_scalar(out=Li[:], in0=self.iot[:], scalar1=0.0, scalar2=None, op0=ALU.is_le), reads=['iot'], writes=['gLi'])
            sc.op('dve', lambda e: e.tensor_scalar(out=Ls[:], in0=self.iot[:], scalar1=0.0, scalar2=None, op0=ALU.is_lt), reads=['iot'], writes=['gLs'])
            sc.op('dve', lambda e: e.memset(ones[:], 1.0), writes=['gones'])
            sc.op('dve', lambda e: e.memset(epsb[:, 0:1], RMS_EPS), writes=['gepsb'])
            sc.op('dve', lambda e: e.memset(epsb[:, 1:2], 128.0 * RMS_EPS), writes=['gepsb'])
            sc.dma('sp', NG[:], self.W('gdn_norm_g', (e_,), [1, 128]).partition_broadcast(128), writes=['gNG'], chan='misc')
            for j in range(4):
                sc.dma('sp', cw[:, :, j], self.W('gdn_conv_w', (e_,), [4, 1536])[j].rearrange("(c p) -> p c", p=128), writes=['gcw'], chan='misc',
                       allow_slow_non_contiguous=True)
            sc.dma('sp', prm[:, 0:4], self.W('gdn_dt_bias', (e_,), [1, 4]).partition_broadcast(128), writes=['gprm'], chan='misc')
            sc.dma('sp', prm[:, 4:8], self.W('gdn_a_log', (e_,), [1, 4]).partition_broadcast(128), writes=['gprm'], chan='misc')
            sc.op('act', lambda e: e.activation(out=prm[:, 8:12], in_=prm[:, 4:8], func=AF.Exp), reads=['gprm'], writes=['gprm'])
            sc.op('dve', lambda e: e.tensor_scalar(out=prm[:, 8:12], in0=prm[:, 8:12], scalar1=-1.0, scalar2=None, op0=ALU.mult),
                  reads=['gprm'], writes=['gprm'])
            with ExitStack() as es1:
                wchunk = [self.sb(es1, f"wch{i}", [128, 8, 128], BF16) for i in range(2)]
                raw = self.sb(es1, "graw", [128, S + 3], F32)
                y = self.sb(es1, "gy", [128, S], F32)
                sq = [self.sb(es1, f"gsq{i}", [128, 512], F32) for i in range(2)]
                rin = [self.sb(es1, f"grin{i}", [128, 512], F32) for i in range(2)]
                sc.op('dve', lambda e: e.memset(raw[:, 0:3], 0.0), writes=['graw'])
                for ci in range(12):
                    sl = ci % 2
                    c0 = 2120 + ci * 128
                    sc.dma('pool', wchunk[sl][:], win_d[:, c0:c0 + 128].rearrange("(k p) n -> p k n", p=128), writes=[f'wch{sl}'], chan=f'wch{sl}')
                    for g4 in range(4):
                        pb = 4 + (g4 % 2)
                        for k in range(8):
                            sc.op('pe', lambda e, k=k: e.matmul(self.ps[pb][:], lhsT=wchunk[sl][:, k, :], rhs=uT[:, k, g4 * 512:(g4 + 1) * 512],
                                                                start=(k == 0), stop=(k == 7)),
                                  reads=[f'wch{sl}'] + uTk[g4 * 4:g4 * 4 + 4], writes=[f'ps{pb}'])
                        sc.op('act', lambda e: e.activation(out=raw[:, 3 + g4 * 512:3 + (g4 + 1) * 512], in_=self.ps[pb][:], func=AF.Copy),
                              reads=[f'ps{pb}'], writes=['graw'])
                    sc.op('dve', lambda e: e.tensor_scalar(out=y[:], in0=raw[:, 0:S], scalar1=cw[:, ci, 0:1], scalar2=None, op0=ALU.mult),
                          reads=['graw', 'gcw'], writes=['gy'])
                    for j in range(1, 4):
                        sc.op('dve', lambda e, j=j: e.scalar_tensor_tensor(out=y[:], in0=raw[:, j:S + j], scalar=cw[:, ci, j:j + 1], in1=y[:],
                                                                           op0=ALU.mult, op1=ALU.add), reads=['graw', 'gcw', 'gy'], writes=['gy'])
                    dst = Xb[ci]
                    if ci >= 8:
                        sc.op('act', lambda e: e.activation(out=dst[:, :], in_=y[:], func=AF.Silu), reads=['gy'], writes=[f'X{ci}'])
                    else:
                        sc.op('act', lambda e: e.activation(out=y[:], in_=y[:], func=AF.Silu), reads=['gy'], writes=['gy'])
                        for g4 in range(4):
                            cs_ = slice(g4 * 512, (g4 + 1) * 512)
                            s2 = g4 % 2
                            sc.op('pool', lambda e: e.tensor_tensor(out=sq[s2][:], in0=y[:, cs_], in1=y[:, cs_], op=ALU.mult),
                                  reads=['gy'], writes=[f'gsq{s2}'])
                            pb = 6 + s2
                            sc.op('pe', lambda e: e.matmul(self.ps[pb][:], lhsT=ones[:], rhs=sq[s2][:], start=True, stop=True),
                                  reads=['gones', f'gsq{s2}'], writes=[f'ps{pb}'])
                            if ci < 4:
                                sc.op('act', lambda e: e.activation(out=rin[s2][:], in_=self.ps[pb][:], func=AF.Sqrt, bias=epsb[:, 1:2], scale=128.0),
                                      reads=[f'ps{pb}', 'gepsb'], writes=[f'grin{s2}'])
                            else:
                                sc.op('act', lambda e: e.activation(out=rin[s2][:], in_=self.ps[pb][:], func=AF.Sqrt, bias=epsb[:, 0:1], scale=1.0),
                                      reads=[f'ps{pb}', 'gepsb'], writes=[f'grin{s2}'])
                            sc.op('dve', lambda e: e.reciprocal(out=rin[s2][:], in_=rin[s2][:]), reads=[f'grin{s2}'], writes=[f'grin{s2}'])
                            sc.op('dve', lambda e: e.tensor_tensor(out=dst[:, cs_], in0=y[:, cs_], in1=rin[s2][:], op=ALU.mult),
                                  reads=['gy', f'grin{s2}'], writes=[f'X{ci}'])
                sc.barrier()
            if GDN_STOP <= 1:
                for t in range(NT):
                    sc.op('dve', lambda e: e.memset(O[:, t, 512:1024], 0.0), writes=[f'O{t}'])
                sc.barrier()
                return
            with ExitStack() as es2:
                wz = self.sb(es2, "gwz", [128, 8, 512], BF16)
                wab = self.sb(es2, "gwab", [128, 8, 8], BF16)
                tmp = self.sb(es2, "gtmp", [128, NT, 4], F32)
                sc.dma('pool', wz[:], win_d[:, 3664:4176].rearrange("(k p) n -> p k n", p=128), writes=['gwz'], chan='wv')
                sc.dma('pool', wab[:], win_d[:, 3656:3664].rearrange("(k p) n -> p k n", p=128), writes=['gwab'], chan='wv')
                for t in range(NT):
                    pb = 4 + t % 2
                    for k in range(8):
                        sc.op('pe', lambda e, k=k: e.matmul(self.ps[pb][:], lhsT=uT[:, k, t * 128:(t + 1) * 128], rhs=wz[:, k, :],
                                                            start=(k == 0), stop=(k == 7)), reads=[uTk[t], 'gwz'], writes=[f'ps{pb}'])
                    sc.op('act', lambda e: e.activation(out=Xb[12 + t // 4][:, (t % 4) * 512:(t % 4 + 1) * 512], in_=self.ps[pb][:], func=AF.Silu),
                          reads=[f'ps{pb}'], writes=[f'X{12 + t // 4}'])
                    pb2 = 6 + t % 2
                    for k in range(8):
                        sc.op('pe', lambda e, k=k: e.matmul(self.ps[pb2][:, 0:8], lhsT=uT[:, k, t * 128:(t + 1) * 128], rhs=wab[:, k, :],
                                                            start=(k == 0), stop=(k == 7)), reads=[uTk[t], 'gwab'], writes=[f'ps{pb2}'])
                    sc.op('dve', lambda e: e.tensor_copy(out=ab[:, t, :], in_=self.ps[pb2][:, 0:8]), reads=[f'ps{pb2}'], writes=['gab'])
                bc4 = lambda ap: ap.unsqueeze(1).to_broadcast([128, NT, 4])
                sc.op('dve', lambda e: e.tensor_tensor(out=tmp[:], in0=ab[:, :, 0:4], in1=bc4(prm[:, 0:4]), op=ALU.add),
                      reads=['gab', 'gprm'], writes=['gtmp'])
                sc.op('act', lambda e: e.activation(out=tmp[:], in_=tmp[:], func=AF.Exp), reads=['gtmp'], writes=['gtmp'])
                sc.op('act', lambda e: e.activation(out=tmp[:], in_=tmp[:], func=AF.Ln, bias=1.0, scale=1.0), reads=['gtmp'], writes=['gtmp'])
                sc.op('dve', lambda e: e.tensor_tensor(out=gg[:], in0=tmp[:], in1=bc4(prm[:, 8:12]), op=ALU.mult),
                      reads=['gtmp', 'gprm'], writes=['ggg'])
                sc.op('act', lambda e: e.activation(out=bt[:], in_=ab[:, :, 4:8], func=AF.Sigmoid), reads=['gab'], writes=['gbt'])
                sc.barrier()
            if GDN_STOP <= 2:
                for t in range(NT):
                    sc.op('dve', lambda e: e.memset(O[:, t, 512:1024], 0.0), writes=[f'O{t}'])
                sc.barrier()
                return
            with ExitStack() as es3:
                f32t = lambda n: self.sb(es3, n, [128, 128], F32)
                b16t = lambda n: self.sb(es3, n, [128, 128], BF16)
                NS = 4
                B = []
                for i in range(NS):
                    B.append(dict(gb=f32t(f"g_gb{i}"), dd=f32t(f"g_dd{i}"), E=f32t(f"g_E{i}"), ET=f32t(f"g_ET{i}"),
                                  Pa=f32t(f"g_Pa{i}"), Pb=f32t(f"g_Pb{i}"), Qa=f32t(f"g_Qa{i}"), Qb=f32t(f"g_Qb{i}"),
                                  R=f32t(f"g_R{i}"), Rb=b16t(f"g_Rb{i}"), AT=b16t(f"g_AT{i}"), vb=b16t(f"g_vb{i}"),
                                  kbg=b16t(f"g_kbg{i}"), kdec=b16t(f"g_kdec{i}"), u=f32t(f"g_u{i}"), wT=b16t(f"g_wT{i}"),
                                  vnb=b16t(f"g_vnb{i}"), o=f32t(f"g_o{i}"), o2=f32t(f"g_o2{i}"), sm=self.sb(es3, f"g_sm{i}", [128, 16], F32)))
                Sst = [f32t(f"g_S{h}") for h in range(4)]
                Sbf = [b16t(f"g_Sb{h}") for h in range(4)]
                for h in range(4):
                    sc.op('dve', lambda e: e.memset(Sst[h][:], 0.0), writes=[f'gS{h}'])
                    sc.op('dve', lambda e: e.memset(Sbf[h][:], 0.0), writes=[f'gSb{h}'])
                q = lambda pb, j: self.ps[pb][:, j * 128:(j + 1) * 128]
                for t in range(0 if G3_LEVEL < 5 else GDN_NT, NT):
                    sc.op('dve', lambda e: e.memset(O[:, t, 512:1024], 0.0), writes=[f'O{t}'])
                def step(t, h):
                    ts_ = slice(t * 128, (t + 1) * 128)
                    PA = lambda j: self.ps[2 * h][:, j * 128:(j + 1) * 128]
                    PB = lambda j: self.ps[2 * h + 1][:, j * 128:(j + 1) * 128]
                    kA, kB = f'ps{2 * h}', f'ps{2 * h + 1}'
                    i = h % NS
                    bb = B[i]
                    K = lambda n: f'g_{n}{i}'
                    sm = bb['sm']
                    qT, kT, vT = Xb[h][:, ts_], Xb[4 + h][:, ts_], Xb[8 + h][:, ts_]
                    kq, kk_, kv = f'X{h}', f'X{4 + h}', f'X{8 + h}'
                    gcol = gg[:, t, h:h + 1]
                    bcol = bt[:, t, h:h + 1]
                    sc.op('dve', lambda e: e.tensor_copy(out=bb['gb'][:], in_=gcol.to_broadcast([128, 128])), reads=['ggg'], writes=[K('gb')])
                    yield
                    sc.op('pe', lambda e: e.matmul(PB(0), lhsT=U[:], rhs=bb['gb'][:], start=True, stop=True),
                          reads=['gU', K('gb')], writes=[kB])
                    yield
                    sc.op('pe', lambda e: e.matmul(PA(0), lhsT=bb['gb'][:], rhs=U[:], start=True, stop=True),
                          reads=['gU', K('gb')], writes=[kA])
                    yield
                    sc.op('act', lambda e: e.activation(out=sm[:, 0:1], in_=PB(0)[:, 0:1], func=AF.Copy), reads=[kB], writes=[K('sm0')])
                    yield
                    sc.op('dve', lambda e: e.tensor_scalar(out=bb['dd'][:], in0=PA(0), scalar1=sm[:, 0:1], scalar2=0.0,
                                                           op0=ALU.subtract, op1=ALU.max), reads=[kA, K('sm0')], writes=[K('dd')])
                    yield
                    sc.op('act', lambda e: e.activation(out=bb['E'][:], in_=bb['dd'][:], func=AF.Exp, scale=-1.0), reads=[K('dd')], writes=[K('E')])
                    yield
                    sc.op('dve', lambda e: e.tensor_scalar(out=bb['dd'][:], in0=PA(0), scalar1=sm[:, 0:1], scalar2=0.0,
                                                           op0=ALU.subtract, op1=ALU.min), reads=[kA, K('sm0'), K('dd')], writes=[K('dd')])
                    yield
                    sc.op('act', lambda e: e.activation(out=bb['ET'][:], in_=bb['dd'][:], func=AF.Exp), reads=[K('dd')], writes=[K('ET')])
                    yield
                    sc.op('pool', lambda e: e.tensor_tensor(out=bb['ET'][:], in0=bb['ET'][:], in1=U[:], op=ALU.mult), reads=[K('ET'), 'gU'], writes=[K('ET')])
                    yield
                    sc.op('pool', lambda e: e.tensor_tensor(out=bb['E'][:], in0=bb['E'][:], in1=Ls[:], op=ALU.mult), reads=[K('E'), 'gLs'], writes=[K('E')])
                    yield
                    sc.op('act', lambda e: e.activation(out=sm[:, 1:2], in_=sm[:, 0:1], func=AF.Exp), reads=[K('sm0')], writes=[K('sm1')])
                    yield
                    sc.op('dve', lambda e: e.tensor_tensor(out=sm[:, 2:3], in0=sm[:, 1:2], in1=bcol, op=ALU.mult), reads=[K('sm1'), 'gbt'], writes=[K('sm2')])
                    yield
                    sc.op('dve', lambda e: e.tensor_tensor(out=sm[:, 3:4], in0=PA(0)[:, 127:128], in1=sm[:, 0:1], op=ALU.subtract),
                          reads=[kA, K('sm0')], writes=[K('sm3')])
                    yield
                    sc.op('act', lambda e: e.activation(out=sm[:, 3:4], in_=sm[:, 3:4], func=AF.Exp), reads=[K('sm3')], writes=[K('sm3')])
                    yield
                    sc.op('dve', lambda e: e.tensor_copy(out=sm[:, 8:9], in_=PA(0)[:, 127:128]), reads=[kA], writes=[K('sm8')])
                    yield
                    sc.op('act', lambda e: e.activation(out=sm[:, 4:5], in_=sm[:, 8:9], func=AF.Exp), reads=[K('sm8')], writes=[K('sm4')])
                    yield
                    if G3_LEVEL < 2:
                        return
                    sc.op('pe', lambda e: e.matmul(PA(1), lhsT=kT, rhs=kT, start=True, stop=True), reads=[kk_], writes=[kA])
                    yield
                    sc.op('pe', lambda e: e.matmul(PA(2), lhsT=kT, rhs=qT, start=True, stop=True), reads=[kk_, kq], writes=[kA])
                    yield
                    if G3_LEVEL < 2.07:
                        return
                    sc.op('dve', lambda e: e.scalar_tensor_tensor(out=bb['Qa'][:], in0=PA(1), scalar=bcol, in1=bb['E'][:], op0=ALU.mult, op1=ALU.mult),
                          reads=[kA, 'gbt', K('E')], writes=[K('Qa')])
                    yield
                    sc.op('dve', lambda e: e.tensor_tensor(out=bb['AT'][:], in0=PA(2), in1=bb['ET'][:], op=ALU.mult),
                          reads=[kA, K('ET')], writes=[K('AT')])
                    yield
                    if G3_LEVEL < 2.15:
                        return
                    sc.op('pe', lambda e: e.matmul(PB(1), lhsT=bb['Qa'][:], rhs=self.ident_f[:], start=True, stop=True), reads=[K('Qa'), 'ident_f'], writes=[kB])
                    yield
                    sc.op('act', lambda e: e.activation(out=bb['Pa'][:], in_=PB(1), func=AF.Copy), reads=[kB], writes=[K('Pa')])
                    yield
                    sc.op('dve', lambda e: e.tensor_tensor(out=bb['R'][:], in0=self.ident_f[:], in1=bb['Pa'][:], op=ALU.subtract),
                          reads=[K('Pa'), 'ident_f'], writes=[K('R')])
                    yield
                    if G3_LEVEL < 2.25:
                        return
                    pk = PB(2).bitcast(BF16)
                    sc.op('pe', lambda e: e.transpose(out=pk[:, 0:128], in_=kT, identity=self.ident_b[:]), reads=[kk_, 'ident_b'], writes=[kB])
                    yield
                    sc.op('pe', lambda e: e.transpose(out=pk[:, 128:256], in_=vT, identity=self.ident_b[:]), reads=[kv, 'ident_b'], writes=[kB])
                    yield
                    sc.op('act', lambda e: e.activation(out=bb['kbg'][:], in_=pk[:, 0:128], func=AF.Copy, scale=sm[:, 2:3]), reads=[kB, K('sm2')], writes=[K('kbg')])
                    yield
                    sc.op('act', lambda e: e.activation(out=bb['kdec'][:], in_=pk[:, 0:128], func=AF.Copy, scale=sm[:, 3:4]), reads=[kB, K('sm3')], writes=[K('kdec')])
                    yield
                    sc.op('act', lambda e: e.activation(out=bb['vb'][:], in_=pk[:, 128:256], func=AF.Copy, scale=bcol),
                          reads=[kB, 'gbt'], writes=[K('vb')])
                    yield
                    if G3_LEVEL < 3:
                        return
                    P_, Q_ = bb['Pa'], bb['Qa']
                    Pn, Qn = bb['Pb'], bb['Qb']
                    kP, kQ, kPn, kQn = K('Pa'), K('Qa'), K('Pb'), K('Qb')
                    for lev in range(6):
                        lastl = (lev == 5)
                        sc.op('pe', lambda e: e.matmul(PB(3), lhsT=P_[:], rhs=Q_[:], start=True, stop=True), reads=[kP, kQ], writes=[kB])
                        yield
                        sc.op('act', lambda e: e.activation(out=Qn[:], in_=PB(3), func=AF.Copy), reads=[kB], writes=[kQn])
                        yield
                        if not lastl:
                            sc.op('pe', lambda e: e.matmul(PB(1), lhsT=Q_[:], rhs=P_[:], start=True, stop=True), reads=[kP, kQ], writes=[kB])
                            yield
                            sc.op('act', lambda e: e.activation(out=Pn[:], in_=PB(1), func=AF.Copy), reads=[kB], writes=[kPn])
                            yield
                        sc.op('pe', lambda e: e.matmul(PA(3), lhsT=Qn[:], rhs=bb['R'][:], start=True, stop=True), reads=[kQn, K('R')], writes=[kA])
                        yield
                        sc.op('dve', lambda e: e.tensor_tensor(out=bb['R'][:], in0=bb['R'][:], in1=PA(3), op=ALU.add), reads=[kA, K('R')], writes=[K('R')])
                        yield
                        P_, Pn = Pn, P_
                        Q_, Qn = Qn, Q_
                        kP, kPn = kPn, kP
                        kQ, kQn = kQn, kQ
                    if G3_LEVEL < 4:
                        return
                    sc.op('act', lambda e: e.activation(out=bb['Rb'][:], in_=bb['R'][:], func=AF.Copy), reads=[K('R')], writes=[K('Rb')])
                    yield
                    sc.op('pe', lambda e: e.matmul(PB(0), lhsT=bb['Rb'][:], rhs=bb['vb'][:], start=True, stop=True), reads=[K('Rb'), K('vb')], writes=[kB])
                    yield
                    sc.op('pe', lambda e: e.matmul(PB(1), lhsT=bb['kbg'][:], rhs=bb['Rb'][:], start=True, stop=True), reads=[K('Rb'), K('kbg')], writes=[kB])
                    yield
                    sc.op('act', lambda e: e.activation(out=bb['u'][:], in_=PB(0), func=AF.Copy), reads=[kB], writes=[K('u')])
                    yield
                    sc.op('act', lambda e: e.activation(out=bb['wT'][:], in_=PB(1), func=AF.Copy), reads=[kB], writes=[K('wT')])
                    yield
                    sc.op('pe', lambda e: e.matmul(PA(0), lhsT=bb['wT'][:], rhs=Sbf[h][:], start=True, stop=True), reads=[K('wT'), f'gSb{h}'], writes=[kA])
                    yield
                    sc.op('pe', lambda e: e.matmul(PA(1), lhsT=qT, rhs=Sbf[h][:], start=True, stop=True), reads=[kq, f'gSb{h}'], writes=[kA])
                    yield
                    sc.op('dve', lambda e: e.tensor_tensor(out=bb['vnb'][:], in0=bb['u'][:], in1=PA(0), op=ALU.subtract), reads=[kA, K('u')], writes=[K('vnb')])
                    yield
                    sc.op('pe', lambda e: e.matmul(PA(2), lhsT=bb['AT'][:], rhs=bb['vnb'][:], start=True, stop=True), reads=[K('AT'), K('vnb')], writes=[kA])
                    yield
                    sc.op('pe', lambda e: e.matmul(PA(3), lhsT=bb['kdec'][:], rhs=bb['vnb'][:], start=True, stop=True), reads=[K('kdec'), K('vnb')], writes=[kA])
                    yield
                    sc.op('dve', lambda e: e.tensor_scalar(out=bb['o'][:], in0=PA(1), scalar1=sm[:, 1:2], scalar2=None, op0=ALU.mult),
                          reads=[kA, K('sm1')], writes=[K('o')])
                    yield
                    sc.op('dve', lambda e: e.tensor_tensor(out=bb['o'][:], in0=bb['o'][:], in1=PA(2), op=ALU.add), reads=[kA, K('o')], writes=[K('o')])
                    yield
                    sc.op('dve', lambda e: e.scalar_tensor_tensor(out=Sst[h][:], in0=Sst[h][:], scalar=sm[:, 4:5], in1=PA(3), op0=ALU.mult, op1=ALU.add),
                          reads=[kA, K('sm4'), f'gS{h}'], writes=[f'gS{h}'])
                    yield
                    sc.op('act', lambda e: e.activation(out=Sbf[h][:], in_=Sst[h][:], func=AF.Copy), reads=[f'gS{h}'], writes=[f'gSb{h}'])
                    yield
                    if G3_LEVEL < 5:
                        return
                    sc.op('act', lambda e: e.activation(out=bb['o2'][:], in_=bb['o'][:], func=AF.Square, accum_out=sm[:, 5:6]),
                          reads=[K('o')], writes=[K('o2'), K('sm5')])
                    yield
                    sc.op('act', lambda e: e.activation(out=sm[:, 6:7], in_=sm[:, 5:6], func=AF.Sqrt, bias=epsb[:, 0:1], scale=1.0 / 128),
                          reads=[K('sm5'), 'gepsb'], writes=[K('sm6')])
                    yield
                    sc.op('dve', lambda e: e.reciprocal(out=sm[:, 7:8], in_=sm[:, 6:7]), reads=[K('sm6')], writes=[K('sm7')])
                    yield
                    sc.op('dve', lambda e: e.scalar_tensor_tensor(out=bb['o2'][:], in0=bb['o'][:], scalar=sm[:, 7:8], in1=NG[:], op0=ALU.mult, op1=ALU.mult),
                          reads=[K('o'), K('sm7'), 'gNG', K('o2')], writes=[K('o2')])
                    yield
                    zs = Xb[12 + t // 4][:, (t % 4) * 512 + h * 128:(t % 4) * 512 + (h + 1) * 128]
                    sc.op('pool', lambda e: e.tensor_tensor(out=O[:, t, 512 + h * 128:512 + (h + 1) * 128], in0=bb['o2'][:], in1=zs, op=ALU.mult),
                          reads=[K('o2'), f'X{12 + t // 4}'], writes=[f'O{t}'])
                    yield
                for t in range(GDN_NT):
                    gens = [step(t, h) for h in range(4)]
                    while gens:
                        for g_ in list(gens):
                            try:
                                next(g_)
                            except StopIteration:
                                gens.remove(g_)
                sc.barrier()


_PLAN = FULL_PLAN
POOL_ELEM = 'dve'
DEBUG = False
HYB_DSA = True
HYB_GDN = True
GDN_STOP = 99
GDN_NT = NT
G3_LEVEL = 5
_LAST = None
_NINS = 0


def kernel(**inputs):
    n = 8
    bld = Builder(_PLAN)
    nc = bld.build()
    global _NINS
    _NINS = bld.sc.n_ins
    wviews = {}
    for name, (key, idx, cols) in bld.wsrc.items():
        a = np.asarray(inputs[key])[idx]
        if cols is not None:
            a = a[..., cols[0]:cols[1]]
        wviews[name] = np.ascontiguousarray(a).reshape(bld.win[name].shape)
    in_maps = []
    for i in range(n):
        m = {}
        for k in ("x", "c", "positions"):
            m[k] = np.ascontiguousarray(np.asarray(inputs[k])[i * NB:(i + 1) * NB])
        for name, arr in wviews.items():
            m[name] = arr
        in_maps.append(m)
    res = run_bass_kernel_spmd(nc, in_maps, core_ids=list(range(n)))
    global _LAST
    _LAST = res
    return np.concatenate([r["out"] for r in res.results], axis=0)
```

```python
import numpy as np
from contextlib import ExitStack
import concourse.bass as bass
import concourse.mybir as mybir
from concourse.bass_utils import run_bass_kernel_spmd

F32 = mybir.dt.float32
BF16 = mybir.dt.bfloat16
I32 = mybir.dt.int32
AF = mybir.ActivationFunctionType
ALU = mybir.AluOpType
AX = mybir.AxisListType

D = 1024
S = 2048
DFF = 2816
NT = S // 128
DEPTH = 4
ALPHA = float((2.0 * DEPTH) ** 0.25)
LN_EPS = 1e-5
RMS_EPS = 1e-6
NB = 2

FULL_PLAN = [(l, s) for l in range(DEPTH) for s in range(3)]


class Sched:
    NDMA = 32

    def __init__(self, nc, es):
        self.nc = nc
        self.es = es
        self.eng = {'pe': nc.tensor, 'act': nc.scalar, 'dve': nc.vector, 'pool': nc.gpsimd, 'sp': nc.sync}
        self.sem = {}
        self.val = {}
        self.waited = {e: {} for e in self.eng}
        for e in self.eng:
            self._mksem(e)
        self.dsem = [f'd{i}' for i in range(self.NDMA)]
        for d in self.dsem:
            self._mksem(d)
        self.qpool = {'sp': self.dsem[:12], 'pool': self.dsem[12:]}
        self.rr = {'sp': 0, 'pool': 0}
        self.lastw = {}
        self.readers = {}
        self.n_ins = 0

    def _mksem(self, name):
        self.sem[name] = self.es.enter_context(self.nc.semaphore('s_' + name))
        self.val[name] = 0

    def _deps(self, reads, writes):
        deps = {}

        def add(evs):
            for s, v in evs.items():
                if deps.get(s, 0) < v:
                    deps[s] = v
        for k in reads:
            add(self.lastw.get(k, {}))
        for k in writes:
            add(self.lastw.get(k, {}))
            add(self.readers.get(k, {}))
        return deps

    def _wait(self, e, deps):
        for s, v in deps.items():
            if s == e and e == 'pe':
                continue
            if self.waited[e].get(s, 0) >= v:
                continue
            self.eng[e].wait_ge(self.sem[s], v)
            self.waited[e][s] = v
            self.n_ins += 1

    def op(self, e, fn, reads=(), writes=()):
        self._wait(e, self._deps(reads, writes))
        ins = fn(self.eng[e])
        self.val[e] += 1
        v = self.val[e]
        ins.then_inc(self.sem[e], 1)
        self.n_ins += 1
        for k in reads:
            r = self.readers.setdefault(k, {})
            if r.get(e, 0) < v:
                r[e] = v
        for k in writes:
            self.lastw[k] = {e: v}
            self.readers[k] = {}

    def dma(self, q, out, in_, reads=(), writes=(), chan=None, **kw):
        pool_ = self.qpool[q]
        s = pool_[self.rr[q] % len(pool_)]
        self.rr[q] += 1
        deps = self._deps(reads, writes)
        if self.val[s] > 0:
            deps[s] = self.val[s]
        self._wait(q, deps)
        ins = self.eng[q].dma_start(out=out, in_=in_, **kw)
        self.val[s] += 16
        v = self.val[s]
        ins.then_inc(self.sem[s], 16)
        self.n_ins += 1
        for k in reads:
            self.readers.setdefault(k, {})[s] = v
        for k in writes:
            prev = self.lastw.get(k, {})
            keep = {a: b for a, b in prev.items() if a in self.val and a.startswith('d') and a[1:].isdigit()}
            keep[s] = v
            self.lastw[k] = keep
            self.readers[k] = {}

    def barrier(self):
        for e in self.eng:
            for s, v in self.val.items():
                if (s == e and e == 'pe') or v == 0:
                    continue
                if self.waited[e].get(s, 0) >= v:
                    continue
                self.eng[e].wait_ge(self.sem[s], v)
                self.waited[e][s] = v
        self.lastw.clear()
        self.readers.clear()

    def wait_all_dma(self, e):
        for s in self.dsem:
            v = self.val[s]
            if v > 0 and self.waited[e].get(s, 0) < v:
                self.eng[e].wait_ge(self.sem[s], v)
                self.waited[e][s] = v


class Builder:
    def __init__(self, plan):
        self.plan = plan
        self.nc = bass.Bass("TRN2", target_bir_lowering=False)
        nc = self.nc
        di = lambda n, sh, dt=F32: nc.dram_tensor(n, sh, dt, kind="ExternalInput").ap()
        self.x = di("x", [NB, S, D])
        self.c = di("c", [NB, D])
        self.pos = di("positions", [NB, S], I32)
        self.win = {}
        self.wsrc = {}
        self.out = nc.dram_tensor("out", [NB, S, D], F32, kind="ExternalOutput").ap()
        self.dbg = nc.dram_tensor("dbg", [128, 4096], F32, kind="ExternalOutput").ap() if DEBUG else None
        self.dumped = set()

    def W(self, key, idx, shape, cols=None):
        name = key + "_" + "_".join(str(i) for i in idx) + ("" if cols is None else f"_c{cols[0]}")
        if name not in self.win:
            self.win[name] = self.nc.dram_tensor(name, list(shape), F32, kind="ExternalInput").ap()
            self.wsrc[name] = (key, tuple(idx), cols)
        return self.win[name]

    def sb(self, es, name, shape, dt):
        self._uid = getattr(self, '_uid', 0) + 1
        return es.enter_context(self.nc.sbuf_tensor(f"{name}_{self._uid}", shape, dt))

    def dump(self, tag, ap, key, col0, shape=None):
        if not DEBUG or tag in self.dumped:
            return
        self.dumped.add(tag)
        n = int(np.prod(ap.shape[1:]))
        dst = self.dbg[:, col0:col0 + n]
        if len(ap.shape) == 3:
            dst = dst.rearrange("p (a b) -> p a b", a=ap.shape[1])
        self.sc.dma('pool', dst, ap, reads=[key] if isinstance(key, str) else key, chan='dbg')

    def build(self):
        nc = self.nc
        with ExitStack() as es:
            self.sc = Sched(nc, es)
            sc = self.sc
            self.psbig = es.enter_context(nc.psum_tensor("psbig", [128, 2048], F32))
            self.ps = [self.psbig[:, i * 512:(i + 1) * 512] for i in range(4)] + \
                      [es.enter_context(nc.psum_tensor(f"ps{i}", [128, 512], F32)) for i in range(4, 8)]
            self.ident_f = self.sb(es, "ident_f", [128, 128], F32)
            self.ident_b = self.sb(es, "ident_b", [128, 128], BF16)
            self.iot = self.sb(es, "iot", [128, 128], I32)
            self.eps_ln = self.sb(es, "eps_ln", [128, 1], F32)
            sc.op('pool', lambda e: e.iota(self.iot[:], pattern=[[1, 128]], base=0, channel_multiplier=-1),
                  writes=['iot'])
            sc.op('dve', lambda e: e.tensor_scalar(out=self.ident_f[:], in0=self.iot[:], scalar1=0.0, scalar2=None,
                                                   op0=ALU.is_equal), reads=['iot'], writes=['ident_f'])
            sc.op('dve', lambda e: e.tensor_copy(out=self.ident_b[:], in_=self.ident_f[:]),
                  reads=['ident_f'], writes=['ident_b'])
            sc.op('dve', lambda e: e.memset(self.eps_ln[:], LN_EPS), writes=['eps_ln'])
            self.X = [self.sb(es, f"X{t}", [128, D], F32) for t in range(NT)]
            self.csrep = self.sb(es, "csrep", [128, 8, 128], BF16)
            self.cs_f = self.sb(es, "cs_f", [128, 8], F32)
            self.cs_b = self.sb(es, "cs_b", [128, 8], BF16)
            for b in range(NB):
                self.run_sequence(b)
            sc.wait_all_dma('sp')
        return nc

    def run_sequence(self, b):
        sc = self.sc
        nc = self.nc
        for t in range(NT):
            sc.dma('sp', self.X[t][:], self.x[b, t * 128:(t + 1) * 128, :], writes=[f'X{t}'], chan='xin')
        sc.dma('sp', self.cs_f[:], self.c[b].rearrange("(k p) -> p k", p=128), writes=['cs_f'], chan='misc',
               allow_slow_non_contiguous=True)
        sc.op('act', lambda e: e.activation(out=self.cs_b[:], in_=self.cs_f[:], func=AF.Silu),
              reads=['cs_f'], writes=['cs_b'])
        sc.op('dve', lambda e: e.tensor_copy(out=self.csrep[:], in_=self.cs_b[:].unsqueeze(2).to_broadcast([128, 8, 128])),
              reads=['cs_b'], writes=['csrep'])
        for (l, s) in self.plan:
            if s in (0, 2):
                self.ffn_sublayer(b, l, s)
            elif l % 2 == 1:
                self.mla_sublayer(b, l)
            else:
                self.hyb_sublayer(b, l)
        for t in range(NT):
            sc.dma('sp', self.out[b, t * 128:(t + 1) * 128, :], self.X[t][:], reads=[f'X{t}'], chan='out')

    def modulation(self, es, b, l, s, gate_mul, which=('SH', 'SC1', 'G1', 'LG', 'LB')):
        sc = self.sc
        mk = lambda n: self.sb(es, n, [128, D], F32) if n in which else None
        SH, SC1, G1, LG, LB = mk('SH'), mk('SC1'), mk('G1'), mk('LG'), mk('LB')
        with ExitStack() as es2:
            self._modulation(es2, b, l, s, gate_mul, which, SH, SC1, G1, LG, LB)
            sc.barrier()
        return SH, SC1, G1, LG, LB

    def _modulation(self, es, b, l, s, gate_mul, which, SH, SC1, G1, LG, LB):
        sc = self.sc
        mw = [self.sb(es, f"mw{i}", [128, 8, 512], BF16) for i in range(2)]
        mb = [self.sb(es, f"mb{i}", [128, 512], F32) for i in range(2)]
        if LG is not None:
            sc.dma('sp', LG[:], self.W('ln_g', (l, s), [1, D]).partition_broadcast(128), writes=['LG'], chan='misc')
        if LB is not None:
            sc.dma('sp', LB[:], self.W('ln_b', (l, s), [1, D]).partition_broadcast(128), writes=['LB'], chan='misc')
        dst = [SH, SC1, G1]
        names = ['SH', 'SC1', 'G1']
        i = 0
        for j in range(3):
            if dst[j] is None:
                continue
            for hf in range(2):
                c0 = j * D + hf * 512
                mwd = self.W('mod_w', (l,), [D, 3 * D], cols=(s * 3 * D, (s + 1) * 3 * D))
                mbd = self.W('mod_b', (l,), [1, 3 * D], cols=(s * 3 * D, (s + 1) * 3 * D))
                sl = i % 2
                sc.dma('pool', mw[sl][:], mwd[:, c0:c0 + 512].rearrange("(k p) n -> p k n", p=128),
                       writes=[f'mw{sl}'], chan=f'mw{sl}')
                sc.dma('sp', mb[sl][:], mbd[:, c0:c0 + 512].partition_broadcast(128),
                       writes=[f'mb{sl}'], chan=f'mb{sl}')
                pb = 6 + sl
                for k in range(8):
                    sc.op('pe', lambda e, k=k: e.matmul(self.ps[pb][:], lhsT=self.csrep[:, k, :], rhs=mw[sl][:, k, :],
                                                        start=(k == 0), stop=(k == 7)),
                          reads=['csrep', f'mw{sl}'], writes=[f'ps{pb}'])
                addc = 0.0 if j == 0 else 1.0
                sc.op('dve', lambda e: e.scalar_tensor_tensor(out=dst[j][:, hf * 512:(hf + 1) * 512], in0=self.ps[pb][:],
                                                              scalar=addc, in1=mb[sl][:], op0=ALU.add, op1=ALU.add),
                      reads=[f'ps{pb}', f'mb{sl}'], writes=[names[j]])
                i += 1
        if gate_mul != 1.0 and G1 is not None:
            sc.op('dve', lambda e: e.tensor_scalar(out=G1[:], in0=G1[:], scalar1=gate_mul, scalar2=None, op0=ALU.mult),
                  reads=['G1'], writes=['G1'])

    def layer_norm_tile(self, t, LG, LB, lnbuf):
        sc = self.sc
        X = self.X[t]
        st, mv, sd, rstd, nb = lnbuf
        k = f'X{t}'
        for hf in range(2):
            sc.op('dve', lambda e, hf=hf: e.bn_stats(out=st[:, hf, :], in_=X[:, hf * 512:(hf + 1) * 512]),
                  reads=[k], writes=['ln_st'])
        sc.op('dve', lambda e: e.bn_aggr(out=mv[:], in_=st[:].rearrange("p a b -> p (a b)")), reads=['ln_st'], writes=['ln_mv'])
        sc.op('act', lambda e: e.activation(out=sd[:], in_=mv[:, 1:2], func=AF.Sqrt, bias=self.eps_ln[:], scale=1.0),
              reads=['ln_mv', 'eps_ln'], writes=['ln_sd'])
        sc.op('dve', lambda e: e.reciprocal(out=rstd[:], in_=sd[:]), reads=['ln_sd'], writes=['ln_rstd'])
        sc.op('dve', lambda e: e.tensor_scalar(out=nb[:], in0=mv[:, 0:1], scalar1=rstd[:], scalar2=-1.0,
                                               op0=ALU.mult, op1=ALU.mult), reads=['ln_mv', 'ln_rstd'], writes=['ln_nb'])
        sc.op('act', lambda e: e.activation(out=X[:], in_=X[:], func=AF.Identity, bias=nb[:], scale=rstd[:]),
              reads=[k, 'ln_nb', 'ln_rstd'], writes=[k])
        sc.op('dve', lambda e: e.tensor_tensor(out=X[:], in0=X[:], in1=LG[:], op=ALU.mult), reads=[k, 'LG'], writes=[k])
        sc.op(POOL_ELEM, lambda e: e.tensor_tensor(out=X[:], in0=X[:], in1=LB[:], op=ALU.add), reads=[k, 'LB'], writes=[k])

    def ln_bufs(self, es):
        return (self.sb(es, "ln_st", [128, 2, 6], F32), self.sb(es, "ln_mv", [128, 2], F32),
                self.sb(es, "ln_sd", [128, 1], F32), self.sb(es, "ln_rstd", [128, 1], F32),
                self.sb(es, "ln_nb", [128, 1], F32))

    def mod_transpose_tile(self, t, SC1, SH, utmp, ub, dstT, dst_key, col0, pbank):
        sc = self.sc
        sl = t % 2
        sc.op('dve', lambda e: e.tensor_tensor(out=utmp[sl][:], in0=self.X[t][:], in1=SC1[:], op=ALU.mult),
              reads=[f'X{t}', 'SC1'], writes=[f'utmp{sl}'])
        sc.op(POOL_ELEM, lambda e: e.tensor_tensor(out=ub[sl][:], in0=utmp[sl][:], in1=SH[:], op=ALU.add),
              reads=[f'utmp{sl}', 'SH'], writes=[f'ub{sl}'])
        pv = self.ps[pbank][:].bitcast(BF16)
        for k in range(8):
            sc.op('pe', lambda e, k=k: e.transpose(out=pv[:, k * 128:(k + 1) * 128], in_=ub[sl][:, k * 128:(k + 1) * 128],
                                                   identity=self.ident_b[:]),
                  reads=[f'ub{sl}', 'ident_b'], writes=[f'ps{pbank}'])
        sc.op('act', lambda e: e.activation(out=dstT[:, :, col0:col0 + 128], in_=pv.rearrange("p (k n) -> p k n", k=8),
                                            func=AF.Copy),
              reads=[f'ps{pbank}'], writes=[dst_key])

    def ffn_sublayer(self, b, l, s):
        sc = self.sc
        fi = 0 if s == 0 else 1
        with ExitStack() as es:
            SH, SC1, G1, LG, LB = self.modulation(es, b, l, s, 0.5)
            lnbuf = self.ln_bufs(es)
            utmp = [self.sb(es, f"utmp{i}", [128, D], F32) for i in range(2)]
            ub = [self.sb(es, f"ub{i}", [128, D], BF16) for i in range(2)]
            uT = self.sb(es, "uT", [128, 8, 1024], BF16)
            hT = self.sb(es, "hT", [128, 12, 1024], BF16)
            wgb = [self.sb(es, f"wgb{i}", [128, 8, 256], BF16) for i in range(3)]
            wub = [self.sb(es, f"wub{i}", [128, 8, 256], BF16) for i in range(3)]
            wdb = [self.sb(es, f"wdb{i}", [128, 2, D], BF16) for i in range(6)]
            sg = [self.sb(es, f"sg{i}", [128, 512], F32) for i in range(2)]
            t1 = [self.sb(es, f"t1_{i}", [128, 512], F32) for i in range(2)]
            cnt = 0
            self.dump('SC1', SC1[:], 'SC1', 0)
            self.dump('G1', G1[:], 'G1', 1024)
            for grp in range(2):
                for tl in range(8):
                    t = grp * 8 + tl
                    self.mod_transpose_tile(t, SC1, SH, utmp, ub, uT, f'uT{tl}', tl * 128, tl % 2)
                self.dump('uT', uT[:, :, 0:128], 'uT0', 2048)
                for ph in range(2):
                    blocks = list(range(0, 6)) if ph == 0 else list(range(6, 11))
                    for bi, blk in enumerate(blocks):
                        sl = cnt % 3
                        cnt += 1
                        c0 = blk * 256
                        sc.dma('pool', wgb[sl][:], self.W('ffn_w_gate', (l, fi), [D, DFF])[:, c0:c0 + 256].rearrange("(k p) n -> p k n", p=128),
                               writes=[f'wgb{sl}'], chan=f'wgb{sl}')
                        sc.dma('pool', wub[sl][:], self.W('ffn_w_up', (l, fi), [D, DFF])[:, c0:c0 + 256].rearrange("(k p) n -> p k n", p=128),
                               writes=[f'wub{sl}'], chan=f'wub{sl}')
                        sc.dma('pool', wdb[bi][:], self.W('ffn_w_down', (l, fi), [DFF, D])[c0:c0 + 256, :].rearrange("(k p) n -> p k n", p=128),
                               writes=[f'wdb{bi}'], chan=f'wdb{bi}')
                        for cc in range(2):
                            fl = bi * 2 + cc
                            for hf in range(2):
                                pg = 2 + hf
                                pu = 4 + hf
                                for k in range(8):
                                    sc.op('pe', lambda e, k=k: e.matmul(self.ps[pg][:], lhsT=wgb[sl][:, k, cc * 128:(cc + 1) * 128],
                                                                        rhs=uT[:, k, hf * 512:(hf + 1) * 512],
                                                                        start=(k == 0), stop=(k == 7)),
                                          reads=[f'wgb{sl}'] + [f'uT{i}' for i in range(hf * 4, hf * 4 + 4)], writes=[f'ps{pg}'])
                                for k in range(8):
                                    sc.op('pe', lambda e, k=k: e.matmul(self.ps[pu][:], lhsT=wub[sl][:, k, cc * 128:(cc + 1) * 128],
                                                                        rhs=uT[:, k, hf * 512:(hf + 1) * 512],
                                                                        start=(k == 0), stop=(k == 7)),
                                          reads=[f'wub{sl}'] + [f'uT{i}' for i in range(hf * 4, hf * 4 + 4)], writes=[f'ps{pu}'])
                                sc.op('act', lambda e: e.activation(out=sg[hf][:], in_=self.ps[pg][:], func=AF.Silu),
                                      reads=[f'ps{pg}'], writes=[f'sg{hf}'])
                                sc.op('dve', lambda e: e.tensor_tensor(out=hT[:, fl, hf * 512:(hf + 1) * 512], in0=sg[hf][:],
                                                                       in1=self.ps[pu][:], op=ALU.mult),
                                      reads=[f'sg{hf}', f'ps{pu}'], writes=[f'hT{fl}_{hf}'])
                    nfl = len(blocks) * 2
                    self.dump('hT', hT[:, 0, 0:512], 'hT0_0', 3072)
                    for tl in range(8):
                        t = grp * 8 + tl
                        for hf in range(2):
                            py = 6 + hf
                            for fl in range(nfl):
                                sc.op('pe', lambda e, fl=fl: e.matmul(self.ps[py][:], lhsT=hT[:, fl, tl * 128:(tl + 1) * 128],
                                                                      rhs=wdb[fl // 2][:, fl % 2, hf * 512:(hf + 1) * 512],
                                                                      start=(fl == 0), stop=(fl == nfl - 1)),
                                      reads=[f'hT{fl}_{tl // 4}', f'wdb{fl // 2}'], writes=[f'ps{py}'])
                            Xs = self.X[t][:, hf * 512:(hf + 1) * 512]
                            sc.op('dve', lambda e: e.tensor_tensor(out=t1[hf][:], in0=self.ps[py][:], in1=G1[:, hf * 512:(hf + 1) * 512],
                                                                   op=ALU.mult),
                                  reads=[f'ps{py}', 'G1'], writes=[f't1_{hf}'])
                            if hf == 0:
                                self.dump('t1', t1[0][:], 't1_0', 3584)
                            if ph == 0:
                                sc.op('dve', lambda e: e.scalar_tensor_tensor(out=Xs, in0=Xs, scalar=ALPHA, in1=t1[hf][:],
                                                                              op0=ALU.mult, op1=ALU.add),
                                      reads=[f'X{t}', f't1_{hf}'], writes=[f'X{t}'])
                            else:
                                sc.op(POOL_ELEM, lambda e: e.tensor_tensor(out=Xs, in0=Xs, in1=t1[hf][:], op=ALU.add),
                                      reads=[f'X{t}', f't1_{hf}'], writes=[f'X{t}'])
                        if ph == 1:
                            self.layer_norm_tile(t, LG, LB, lnbuf)
            sc.barrier()


    def mixer_tail(self, b, l, O, okey, wout_d):
        sc = self.sc
        with ExitStack() as es:
            _, _, G1, LG, LB = self.modulation(es, b, l, 1, 1.0, which=('G1', 'LG', 'LB'))
            lnbuf = self.ln_bufs(es)
            wo = self.sb(es, "wo", [128, 8, D], BF16)
            OT = [self.sb(es, f"OT{i}", [128, 8, 128], BF16) for i in range(2)]
            t1 = [self.sb(es, f"t1_{i}", [128, 512], F32) for i in range(2)]
            for hf in range(2):
                sc.dma('pool', wo[:, :, hf * 512:(hf + 1) * 512],
                       wout_d[:, hf * 512:(hf + 1) * 512].rearrange("(k p) n -> p k n", p=128), writes=[f'wo{hf}'], chan=f'wo{hf}')
            for t in range(NT):
                sl = t % 2
                pb = 4 + sl
                pv = self.ps[pb][:].bitcast(BF16)
                for k in range(8):
                    sc.op('pe', lambda e, k=k: e.transpose(out=pv[:, k * 128:(k + 1) * 128], in_=O[:, t, k * 128:(k + 1) * 128],
                                                           identity=self.ident_b[:]),
                          reads=[okey(t), 'ident_b'], writes=[f'ps{pb}'])
                sc.op('act', lambda e: e.activation(out=OT[sl][:], in_=pv.rearrange("p (k n) -> p k n", k=8), func=AF.Copy),
                      reads=[f'ps{pb}'], writes=[f'OT{sl}'])
                for hf in range(2):
                    py = 6 + hf
                    for k in range(8):
                        sc.op('pe', lambda e, k=k: e.matmul(self.ps[py][:], lhsT=OT[sl][:, k, :], rhs=wo[:, k, hf * 512:(hf + 1) * 512],
                                                            start=(k == 0), stop=(k == 7)),
                              reads=[f'OT{sl}', f'wo{hf}'], writes=[f'ps{py}'])
                    Xs = self.X[t][:, hf * 512:(hf + 1) * 512]
                    sc.op('dve', lambda e: e.tensor_tensor(out=t1[hf][:], in0=self.ps[py][:], in1=G1[:, hf * 512:(hf + 1) * 512], op=ALU.mult),
                          reads=[f'ps{py}', 'G1'], writes=[f't1_{hf}'])
                    sc.op('dve', lambda e: e.scalar_tensor_tensor(out=Xs, in0=Xs, scalar=ALPHA, in1=t1[hf][:], op0=ALU.mult, op1=ALU.add),
                          reads=[f'X{t}', f't1_{hf}'], writes=[f'X{t}'])
                self.layer_norm_tile(t, LG, LB, lnbuf)
            sc.barrier()

    def attn_tile(self, qT_ap, kT, kT_key, V, V_key, KV, scale, out_ap, out_key, bufs, maskblk, q_keys, bias=None, bias_key=None,
                  v_of=None, dv=None):
        sc = self.sc
        P, PT, st = bufs
        if v_of is None:
            dv = V.shape[2]
            v_of = lambda blk: (V[:, blk, :], V_key)
        Sp = self.psbig
        PSK = ['psS', 'ps0', 'ps1', 'ps2', 'ps3']
        nkc = (KV + 511) // 512
        for kc in range(nkc):
            c0 = kc * 512
            c1 = min(KV, c0 + 512)
            last = (kc == nkc - 1)
            sc.op('pe', lambda e: e.matmul(Sp[:, c0:c1], lhsT=qT_ap, rhs=kT[:, c0:c1], start=True,
                                           stop=(not last) or (maskblk is None)),
                  reads=list(q_keys) + [kT_key], writes=PSK)
        if maskblk is not None:
            sc.op('pe', lambda e: e.matmul(Sp[:, KV - 128:KV], lhsT=self.ident_b[:], rhs=maskblk[:], start=False, stop=True),
                  reads=['ident_b', 'maskblk'], writes=PSK)
        src = Sp[:, 0:KV]
        skeys = PSK
        if bias is not None:
            sc.op('dve', lambda e: e.tensor_tensor(out=bias[:, 0:KV], in0=Sp[:, 0:KV], in1=bias[:, 0:KV], op=ALU.add),
                  reads=PSK + [bias_key], writes=[bias_key])
            src = bias[:, 0:KV]
            skeys = [bias_key]
        sc.op('dve', lambda e: e.tensor_reduce(out=st[:, 0:1], in_=src, axis=AX.X, op=ALU.max), reads=skeys, writes=['at_m'])
        sc.op('dve', lambda e: e.tensor_scalar(out=st[:, 1:2], in0=st[:, 0:1], scalar1=-scale, scalar2=None, op0=ALU.mult),
              reads=['at_m'], writes=['at_nm'])
        sc.op('act', lambda e: e.activation(out=P[:, 0:KV], in_=src, func=AF.Exp, bias=st[:, 1:2], scale=scale, accum_out=st[:, 2:3]),
              reads=skeys + ['at_nm'], writes=['at_P', 'at_sum'])
        sc.op('dve', lambda e: e.reciprocal(out=st[:, 3:4], in_=st[:, 2:3]), reads=['at_sum'], writes=['at_rinv'])
        nblk = KV // 128
        for g in range((nblk + 7) // 8):
            pb = 4 + (g % 2)
            pv = self.ps[pb][:].bitcast(BF16)
            nb_ = min(8, nblk - g * 8)
            for j in range(nb_):
                blk = g * 8 + j
                sc.op('pe', lambda e, j=j, blk=blk: e.transpose(out=pv[:, j * 128:(j + 1) * 128], in_=P[:, blk * 128:(blk + 1) * 128],
                                                                identity=self.ident_b[:]),
                      reads=['at_P', 'ident_b'], writes=[f'ps{pb}'])
            eng = 'act' if g % 2 == 0 else 'dve'
            if eng == 'act':
                sc.op('act', lambda e: e.activation(out=PT[:, g * 8:g * 8 + nb_, :], in_=pv[:, 0:nb_ * 128].rearrange("p (k n) -> p k n", n=128),
                                                    func=AF.Copy), reads=[f'ps{pb}'], writes=[f'at_PT{g}'])
            else:
                sc.op('dve', lambda e: e.tensor_copy(out=PT[:, g * 8:g * 8 + nb_, :], in_=pv[:, 0:nb_ * 128].rearrange("p (k n) -> p k n", n=128)),
                      reads=[f'ps{pb}'], writes=[f'at_PT{g}'])
        po = self.ps[6]
        for blk in range(nblk):
            vap, vkey = v_of(blk)
            sc.op('pe', lambda e, blk=blk: e.matmul(po[:, 0:dv], lhsT=PT[:, blk, :], rhs=vap, start=(blk == 0), stop=(blk == nblk - 1)),
                  reads=[f'at_PT{blk // 8}', vkey], writes=['ps6'])
        sc.op('act', lambda e: e.activation(out=out_ap, in_=po[:, 0:dv], func=AF.Copy, scale=st[:, 3:4]),
              reads=['ps6', 'at_rinv'], writes=[out_key])

    def attn_gen(self, si, qT_ap, kT, kT_key, V, V_key, KV, scale, out_ap, out_key, bufs, maskblk, q_keys, bias=None, bias_key=None,
                  v_of=None, dv=None):
        sc = self.sc
        sfx = f'_s{si}'
        P, PT, st = bufs
        if v_of is None:
            dv = V.shape[2]
            v_of = lambda blk: (V[:, blk, :], V_key)
        Sp = self.psbig
        PSK = ['psS', 'ps0', 'ps1', 'ps2', 'ps3']
        nkc = (KV + 511) // 512
        for kc in range(nkc):
            c0 = kc * 512
            c1 = min(KV, c0 + 512)
            last = (kc == nkc - 1)
            sc.op('pe', lambda e: e.matmul(Sp[:, c0:c1], lhsT=qT_ap, rhs=kT[:, c0:c1], start=True,
                                           stop=(not last) or (maskblk is None)),
                  reads=list(q_keys) + [kT_key], writes=PSK)
        if maskblk is not None:
            sc.op('pe', lambda e: e.matmul(Sp[:, KV - 128:KV], lhsT=self.ident_b[:], rhs=maskblk[:], start=False, stop=True),
                  reads=['ident_b', 'maskblk'], writes=PSK)
        src = Sp[:, 0:KV]
        skeys = PSK
        if bias is not None:
            sc.op('dve', lambda e: e.tensor_tensor(out=bias[:, 0:KV], in0=Sp[:, 0:KV], in1=bias[:, 0:KV], op=ALU.add),
                  reads=PSK + [bias_key], writes=[bias_key])
            src = bias[:, 0:KV]
            skeys = [bias_key]
        sc.op('dve', lambda e: e.tensor_reduce(out=st[:, 0:1], in_=src, axis=AX.X, op=ALU.max), reads=skeys, writes=[('at_m' + sfx)])
        sc.op('dve', lambda e: e.tensor_scalar(out=st[:, 1:2], in0=st[:, 0:1], scalar1=-scale, scalar2=None, op0=ALU.mult),
              reads=[('at_m' + sfx)], writes=[('at_nm' + sfx)])
        sc.op('act', lambda e: e.activation(out=P[:, 0:KV], in_=src, func=AF.Exp, bias=st[:, 1:2], scale=scale, accum_out=st[:, 2:3]),
              reads=skeys + [('at_nm' + sfx)], writes=[('at_P' + sfx), ('at_sum' + sfx)])
        sc.op('dve', lambda e: e.reciprocal(out=st[:, 3:4], in_=st[:, 2:3]), reads=[('at_sum' + sfx)], writes=[('at_rinv' + sfx)])
        yield
        nblk = KV // 128
        for g in range((nblk + 7) // 8):
            pb = 4 + si
            pv = self.ps[pb][:].bitcast(BF16)
            nb_ = min(8, nblk - g * 8)
            for j in range(nb_):
                blk = g * 8 + j
                sc.op('pe', lambda e, j=j, blk=blk: e.transpose(out=pv[:, j * 128:(j + 1) * 128], in_=P[:, blk * 128:(blk + 1) * 128],
                                                                identity=self.ident_b[:]),
                      reads=[('at_P' + sfx), 'ident_b'], writes=[f'ps{pb}'])
            eng = 'act' if g % 2 == 0 else 'dve'
            if eng == 'act':
                sc.op('act', lambda e: e.activation(out=PT[:, g * 8:g * 8 + nb_, :], in_=pv[:, 0:nb_ * 128].rearrange("p (k n) -> p k n", n=128),
                                                    func=AF.Copy), reads=[f'ps{pb}'], writes=[f'at_PT{g}' + sfx])
            else:
                sc.op('dve', lambda e: e.tensor_copy(out=PT[:, g * 8:g * 8 + nb_, :], in_=pv[:, 0:nb_ * 128].rearrange("p (k n) -> p k n", n=128)),
                      reads=[f'ps{pb}'], writes=[f'at_PT{g}' + sfx])
        po = self.ps[6 + si]
        pok = f'ps{6 + si}'
        for blk in range(nblk):
            vap, vkey = v_of(blk)
            sc.op('pe', lambda e, blk=blk: e.matmul(po[:, 0:dv], lhsT=PT[:, blk, :], rhs=vap, start=(blk == 0), stop=(blk == nblk - 1)),
                  reads=[f'at_PT{blk // 8}' + sfx, vkey], writes=[pok])
        sc.op('act', lambda e: e.activation(out=out_ap, in_=po[:, 0:dv], func=AF.Copy, scale=st[:, 3:4]),
              reads=[pok, ('at_rinv' + sfx)], writes=[out_key])
        yield

    def make_maskblk(self, es):
        sc = self.sc
        mi = self.sb(es, "mask_i", [128, 128], I32)
        mf = self.sb(es, "mask_f", [128, 128], F32)
        mb = self.sb(es, "maskblk", [128, 128], BF16)
        sc.op('pool', lambda e: e.iota(mi[:], pattern=[[1, 128]], base=0, channel_multiplier=0), writes=['mask_i'])
        sc.op('dve', lambda e: e.tensor_scalar(out=mf[:], in0=mi[:], scalar1=64.0, scalar2=None, op0=ALU.is_ge),
              reads=['mask_i'], writes=['mask_f'])
        sc.op('dve', lambda e: e.memset(mf[64:128, :], 0.0), reads=['mask_f'], writes=['mask_f'])
        sc.op('dve', lambda e: e.tensor_scalar(out=mb[:], in0=mf[:], scalar1=-1e30, scalar2=None, op0=ALU.mult),
              reads=['mask_f'], writes=['maskblk'])
        return mb

    def rope_tables(self, es, b):
        sc = self.sc
        COS = self.sb(es, "COS", [32, S], F32)
        SINS = self.sb(es, "SINS", [32, S], F32)
        with ExitStack() as es2:
            pi_ = self.sb(es2, "rp_pi", [32, S], I32)
            ang = self.sb(es2, "rp_ang", [32, S], F32)
            kf = self.sb(es2, "rp_kf", [32, S], F32)
            ki = self.sb(es2, "rp_ki", [32, S], I32)
            r = self.sb(es2, "rp_r", [32, S], F32)
            m = self.sb(es2, "rp_m", [32, S], F32)
            fi = self.sb(es2, "rp_fi", [32, 1], I32)
            ff = self.sb(es2, "rp_ff", [32, 4], F32)
            sc.dma('sp', pi_[:], self.pos[b:b + 1, :].partition_broadcast(32), writes=['rp_pi'], chan='misc')
            sc.op('pool', lambda e: e.iota(fi[:], pattern=[[0, 1]], base=0, channel_multiplier=1), writes=['rp_fi'])
            sc.op('dve', lambda e: e.tensor_copy(out=ff[:, 0:1], in_=fi[:]), reads=['rp_fi'], writes=['rp_ff'])
            sc.op('dve', lambda e: e.tensor_scalar(out=ff[:, 1:2], in0=ff[:, 0:1], scalar1=16.0, scalar2=-16.0, op0=ALU.is_ge, op1=ALU.mult),
                  reads=['rp_ff'], writes=['rp_ff'])
            sc.op('dve', lambda e: e.tensor_tensor(out=ff[:, 2:3], in0=ff[:, 0:1], in1=ff[:, 1:2], op=ALU.add), reads=['rp_ff'], writes=['rp_ff'])
            sc.op('act', lambda e: e.activation(out=ff[:, 3:4], in_=ff[:, 2:3], func=AF.Exp, scale=-float(np.log(10000.0)) / 16.0),
                  reads=['rp_ff'], writes=['rp_ff'])
            sc.op('dve', lambda e: e.tensor_copy(out=ang[:], in_=pi_[:]), reads=['rp_pi'], writes=['rp_ang'])
            sc.op('dve', lambda e: e.tensor_scalar(out=ang[:], in0=ang[:], scalar1=ff[:, 3:4], scalar2=None, op0=ALU.mult),
                  reads=['rp_ang', 'rp_ff'], writes=['rp_ang'])
            TWO_PI = float(2 * np.pi)
            C1 = 6.28125
            C2 = float(2 * np.pi - 6.28125)
            sc.op('dve', lambda e: e.tensor_scalar(out=kf[:], in0=ang[:], scalar1=1.0 / TWO_PI, scalar2=None, op0=ALU.mult),
                  reads=['rp_ang'], writes=['rp_kf'])
            sc.op('dve', lambda e: e.tensor_copy(out=ki[:], in_=kf[:]), reads=['rp_kf'], writes=['rp_ki'])
            sc.op('dve', lambda e: e.tensor_copy(out=kf[:], in_=ki[:]), reads=['rp_ki'], writes=['rp_kf'])
            sc.op('dve', lambda e: e.scalar_tensor_tensor(out=r[:], in0=kf[:], scalar=-C1, in1=ang[:], op0=ALU.mult, op1=ALU.add),
                  reads=['rp_kf', 'rp_ang'], writes=['rp_r'])
            sc.op('dve', lambda e: e.scalar_tensor_tensor(out=r[:], in0=kf[:], scalar=-C2, in1=r[:], op0=ALU.mult, op1=ALU.add),
                  reads=['rp_kf', 'rp_r'], writes=['rp_r'])

            def wrap(buf, key):
                sc.op('dve', lambda e: e.tensor_scalar(out=m[:], in0=buf[:], scalar1=float(np.pi), scalar2=-TWO_PI, op0=ALU.is_gt, op1=ALU.mult),
                      reads=[key], writes=['rp_m'])
                sc.op('dve', lambda e: e.tensor_tensor(out=buf[:], in0=buf[:], in1=m[:], op=ALU.add), reads=[key, 'rp_m'], writes=[key])
                sc.op('dve', lambda e: e.tensor_scalar(out=m[:], in0=buf[:], scalar1=-float(np.pi), scalar2=TWO_PI, op0=ALU.is_lt, op1=ALU.mult),
                      reads=[key], writes=['rp_m'])
                sc.op('dve', lambda e: e.tensor_tensor(out=buf[:], in0=buf[:], in1=m[:], op=ALU.add), reads=[key, 'rp_m'], writes=[key])
            wrap(r, 'rp_r')
            sc.op('act', lambda e: e.activation(out=SINS[:], in_=r[:], func=AF.Sin), reads=['rp_r'], writes=['SINS'])
            sc.op('dve', lambda e: e.tensor_scalar(out=ff[:, 1:2], in0=ff[:, 0:1], scalar1=16.0, scalar2=2.0, op0=ALU.is_ge, op1=ALU.mult),
                  reads=['rp_ff'], writes=['rp_ff'])
            sc.op('dve', lambda e: e.tensor_scalar(out=ff[:, 1:2], in0=ff[:, 1:2], scalar1=-1.0, scalar2=None, op0=ALU.add),
                  reads=['rp_ff'], writes=['rp_ff'])
            sc.op('dve', lambda e: e.tensor_scalar(out=SINS[:], in0=SINS[:], scalar1=ff[:, 1:2], scalar2=None, op0=ALU.mult),
                  reads=['SINS', 'rp_ff'], writes=['SINS'])
            sc.op('dve', lambda e: e.tensor_scalar(out=r[:], in0=r[:], scalar1=float(np.pi / 2), scalar2=None, op0=ALU.add),
                  reads=['rp_r'], writes=['rp_r'])
            wrap(r, 'rp_r')
            sc.op('act', lambda e: e.activation(out=COS[:], in_=r[:], func=AF.Sin), reads=['rp_r'], writes=['COS'])
            sc.barrier()
        return COS, SINS

    def mla_sublayer(self, b, l):
        sc = self.sc
        o = l // 2
        HD = 96
        scale = float(HD ** -0.5)
        with ExitStack() as esO:
            O = self.sb(esO, "O", [128, NT, D], BF16)
            with ExitStack() as es:
                cnT = self.sb(es, "cnT", [128, 5, S], BF16)
                krT = self.sb(es, "krT", [32, S], BF16)
                COS, SINS = self.rope_tables(es, b)
                with ExitStack() as esA:
                    SH, SC1, _, _, _ = self.modulation(esA, b, l, 1, 1.0, which=('SH', 'SC1'))
                    utmp = [self.sb(esA, f"utmp{i}", [128, D], F32) for i in range(2)]
                    ub = [self.sb(esA, f"ub{i}", [128, D], BF16) for i in range(2)]
                    uT4 = self.sb(esA, "uT4", [128, 8, 512], BF16)
                    win = self.sb(esA, "win", [128, 8, 672], BF16)
                    winsw = self.sb(esA, "winsw", [128, 8, 32], BF16)
                    GQ = self.sb(esA, "GQ", [128, 640], F32)
                    cn = [self.sb(esA, f"cn{i}", [128, 640], BF16) for i in range(2)]
                    junk = self.sb(esA, "junk", [128, 384], F32)
                    rs = self.sb(esA, "rs", [128, 8], F32)
                    eps_r = self.sb(esA, "eps_r", [128, 1], F32)
                    rt = [self.sb(esA, f"rt{i}", [32, 512], F32) for i in range(2)]
                    wd_ = self.W('mla_w_in', (o,), [D, 672])
                    sc.dma('pool', win[:], wd_.rearrange("(k p) n -> p k n", p=128), writes=['win'], chan='win')
                    sc.dma('pool', winsw[:, :, 0:16], wd_[:, 656:672].rearrange("(k p) n -> p k n", p=128), writes=['winsw'], chan='win')
                    sc.dma('pool', winsw[:, :, 16:32], wd_[:, 640:656].rearrange("(k p) n -> p k n", p=128), writes=['winsw'], chan='win')
                    sc.dma('sp', GQ[:, 0:384], self.W('mla_q_norm_g', (o,), [1, 384]).partition_broadcast(128), writes=['GQ'], chan='misc')
                    sc.dma('sp', GQ[:, 384:640], self.W('mla_kv_norm_g', (o,), [1, 256]).partition_broadcast(128), writes=['GQ'], chan='misc')
                    sc.op('dve', lambda e: e.memset(eps_r[:], RMS_EPS), writes=['eps_r'])
                    for g4 in range(4):
                        for tl in range(4):
                            t = g4 * 4 + tl
                            self.mod_transpose_tile(t, SC1, SH, utmp, ub, uT4, f'uT{tl}', tl * 128, 4 + t % 2)
                            for (c0, c1, pb) in ((0, 512, 0), (512, 640, 1)):
                                for k in range(8):
                                    sc.op('pe', lambda e, k=k: e.matmul(self.ps[pb][:, 0:c1 - c0], lhsT=uT4[:, k, tl * 128:(tl + 1) * 128],
                                                                        rhs=win[:, k, c0:c1], start=(k == 0), stop=(k == 7)),
                                          reads=[f'uT{tl}', 'win'], writes=[f'ps{pb}'])
                            cps = self.psbig[:, 0:640]
                            sc.op('act', lambda e: e.activation(out=junk[:, 0:384], in_=cps[:, 0:384], func=AF.Square, accum_out=rs[:, 0:1]),
                                  reads=['ps0'], writes=['junk', 'rs0'])
                            sc.op('act', lambda e: e.activation(out=junk[:, 0:256], in_=cps[:, 384:640], func=AF.Square, accum_out=rs[:, 1:2]),
                                  reads=['ps0', 'ps1'], writes=['junk', 'rs1'])
                            sc.op('act', lambda e: e.activation(out=rs[:, 2:3], in_=rs[:, 0:1], func=AF.Sqrt, bias=eps_r[:], scale=1.0 / 384),
                                  reads=['rs0', 'eps_r'], writes=['rs2'])
                            sc.op('act', lambda e: e.activation(out=rs[:, 3:4], in_=rs[:, 1:2], func=AF.Sqrt, bias=eps_r[:], scale=1.0 / 256),
                                  reads=['rs1', 'eps_r'], writes=['rs3'])
                            sc.op('dve', lambda e: e.reciprocal(out=rs[:, 4:6], in_=rs[:, 2:4]), reads=['rs2', 'rs3'], writes=['rs4'])
                            sl = t % 2
                            sc.op('dve', lambda e: e.scalar_tensor_tensor(out=cn[sl][:, 0:384], in0=cps[:, 0:384], scalar=rs[:, 4:5], in1=GQ[:, 0:384],
                                                                          op0=ALU.mult, op1=ALU.mult),
                                  reads=['ps0', 'rs4', 'GQ'], writes=[f'cn{sl}'])
                            sc.op('dve', lambda e: e.scalar_tensor_tensor(out=cn[sl][:, 384:640], in0=cps[:, 384:640], scalar=rs[:, 5:6], in1=GQ[:, 384:640],
                                                                          op0=ALU.mult, op1=ALU.mult),
                                  reads=['ps0', 'ps1', 'rs4', 'GQ'], writes=[f'cn{sl}'])
                            pb = 2 + sl
                            pv = self.ps[pb][:].bitcast(BF16)
                            for k in range(5):
                                sc.op('pe', lambda e, k=k: e.transpose(out=pv[:, k * 128:(k + 1) * 128], in_=cn[sl][:, k * 128:(k + 1) * 128],
                                                                       identity=self.ident_b[:]),
                                      reads=[f'cn{sl}', 'ident_b'], writes=[f'ps{pb}'])
                            sc.op('act', lambda e: e.activation(out=cnT[:, :, t * 128:(t + 1) * 128],
                                                                in_=pv[:, 0:640].rearrange("p (k n) -> p k n", k=5), func=AF.Copy),
                                  reads=[f'ps{pb}'], writes=[f'cnT{t}'])
                        cols = slice(g4 * 512, (g4 + 1) * 512)
                        for k in range(8):
                            sc.op('pe', lambda e, k=k: e.matmul(self.ps[6][0:32, :], lhsT=win[:, k, 640:672], rhs=uT4[:, k, :],
                                                                start=(k == 0), stop=(k == 7)),
                                  reads=['win'] + [f'uT{i}' for i in range(4)], writes=['ps6'])
                        for k in range(8):
                            sc.op('pe', lambda e, k=k: e.matmul(self.ps[7][0:32, :], lhsT=winsw[:, k, :], rhs=uT4[:, k, :],
                                                                start=(k == 0), stop=(k == 7)),
                                  reads=['winsw'] + [f'uT{i}' for i in range(4)], writes=['ps7'])
                        sc.op('dve', lambda e: e.tensor_tensor(out=rt[0][:], in0=self.ps[6][0:32, :], in1=COS[:, cols], op=ALU.mult),
                              reads=['ps6', 'COS'], writes=['rt0'])
                        sc.op('dve', lambda e: e.tensor_tensor(out=rt[1][:], in0=self.ps[7][0:32, :], in1=SINS[:, cols], op=ALU.mult),
                              reads=['ps7', 'SINS'], writes=['rt1'])
                        sc.op('dve', lambda e: e.tensor_tensor(out=krT[:, cols], in0=rt[0][:], in1=rt[1][:], op=ALU.add),
                              reads=['rt0', 'rt1'], writes=['krT'])
                    sc.barrier()
                with ExitStack() as esB:
                    wq = self.sb(esB, "wq", [128, 3, 16, 128], BF16)
                    wkv = self.sb(esB, "wkv", [128, 2, 16, 160], BF16)
                    qT = [self.sb(esB, f"qT{i}", [96, S], BF16) for i in range(2)]
                    kT = [self.sb(esB, f"kT{i}", [96, S], BF16) for i in range(2)]
                    Vh = [self.sb(esB, f"Vh{i}", [128, NT, 64], BF16) for i in range(2)]
                    P2 = [self.sb(esB, f"P{i}", [128, S], BF16) for i in range(2)]
                    PT2 = [self.sb(esB, f"PT{i}", [128, 16, 128], BF16) for i in range(2)]
                    st2 = [self.sb(esB, f"at_st{i}", [128, 4], F32) for i in range(2)]
                    rt = [self.sb(esB, f"rt{i}", [32, 512], F32) for i in range(2)]
                    maskblk = self.make_maskblk(esB)
                    wqd = self.W('mla_w_q_up', (o,), [384, 1536]).rearrange("(k p) (h d) -> p k h d", p=128, d=96)
                    wkvd = self.W('mla_w_kv_up', (o,), [256, 2048]).rearrange("(k p) (h d) -> p k h d", p=128, d=128)
                    for k in range(3):
                        sc.dma('pool', wq[:, k, :, 0:32], wqd[:, k, :, 64:96], writes=['wq'], chan='wq')
                        sc.dma('pool', wq[:, k, :, 32:96], wqd[:, k, :, 0:64], writes=['wq'], chan='wq')
                        sc.dma('pool', wq[:, k, :, 96:112], wqd[:, k, :, 80:96], writes=['wq'], chan='wq')
                        sc.dma('pool', wq[:, k, :, 112:128], wqd[:, k, :, 64:80], writes=['wq'], chan='wq')
                    sc.op('dve', lambda e: e.memset(wkv[:], 0.0), writes=['wkv'])
                    for k in range(2):
                        sc.dma('pool', wkv[:, k, :, 32:160], wkvd[:, k, :, :], writes=['wkv'], chan='wkv')
                    def mla_head(h):
                        sl = h % 2
                        for g4 in range(4):
                            cols = slice(g4 * 512, (g4 + 1) * 512)
                            for k in range(3):
                                sc.op('pe', lambda e, k=k: e.matmul(self.ps[7][0:96, :], lhsT=wq[:, k, h, 0:96], rhs=cnT[:, k, cols],
                                                                    start=(k == 0), stop=(k == 2)),
                                      reads=['wq'] + [f'cnT{t}' for t in range(g4 * 4, g4 * 4 + 4)], writes=['ps7'])
                            for k in range(3):
                                sc.op('pe', lambda e, k=k: e.matmul(self.ps[6][0:32, :], lhsT=wq[:, k, h, 96:128], rhs=cnT[:, k, cols],
                                                                    start=(k == 0), stop=(k == 2)),
                                      reads=['wq'] + [f'cnT{t}' for t in range(g4 * 4, g4 * 4 + 4)], writes=['ps6'])
                            sc.op('dve', lambda e: e.tensor_tensor(out=rt[0][:], in0=self.ps[7][0:32, :], in1=COS[:, cols], op=ALU.mult),
                                  reads=['ps7', 'COS'], writes=['rt0'])
                            sc.op('dve', lambda e: e.tensor_tensor(out=rt[1][:], in0=self.ps[6][0:32, :], in1=SINS[:, cols], op=ALU.mult),
                                  reads=['ps6', 'SINS'], writes=['rt1'])
                            sc.op('dve', lambda e: e.tensor_tensor(out=qT[sl][0:32, cols], in0=rt[0][:], in1=rt[1][:], op=ALU.add),
                                  reads=['rt0', 'rt1'], writes=[f'qT{sl}'])
                            for (p0, p1) in ((32, 64), (64, 96)):
                                sc.op('act', lambda e: e.activation(out=qT[sl][p0:p1, cols], in_=self.ps[7][p0:p1, :], func=AF.Copy),
                                      reads=['ps7'], writes=[f'qT{sl}'])
                            for k in range(2):
                                sc.op('pe', lambda e, k=k: e.matmul(self.ps[7][0:96, :], lhsT=wkv[:, k, h, 0:96], rhs=cnT[:, 3 + k, cols],
                                                                    start=(k == 0), stop=(k == 1)),
                                      reads=['wkv'] + [f'cnT{t}' for t in range(g4 * 4, g4 * 4 + 4)], writes=['ps7'])
                            for (p0, p1) in ((32, 64), (64, 96)):
                                sc.op('act', lambda e: e.activation(out=kT[sl][p0:p1, cols], in_=self.ps[7][p0:p1, :], func=AF.Copy),
                                      reads=['ps7'], writes=[f'kT{sl}'])
                        sc.op('pool', lambda e: e.tensor_copy(out=kT[sl][0:32, :], in_=krT[:]), reads=['krT'], writes=[f'kT{sl}'])
                        for g8 in range(2):
                            pb = 4 + g8
                            for tl in range(8):
                                t = g8 * 8 + tl
                                for k in range(2):
                                    sc.op('pe', lambda e, k=k: e.matmul(self.ps[pb][:, tl * 64:(tl + 1) * 64], lhsT=cnT[:, 3 + k, t * 128:(t + 1) * 128],
                                                                        rhs=wkv[:, k, h, 96:160], start=(k == 0), stop=(k == 1)),
                                          reads=['wkv', f'cnT{t}'], writes=[f'ps{pb}'])
                            sc.op('act', lambda e: e.activation(out=Vh[sl][:, g8 * 8:(g8 + 1) * 8, :],
                                                                in_=self.ps[pb][:].rearrange("p (t d) -> p t d", d=64), func=AF.Copy),
                                  reads=[f'ps{pb}'], writes=[f'Vh{sl}'])
                        for qt in range(NT):
                            yield from self.attn_gen(sl, qT[sl][:, qt * 128:(qt + 1) * 128], kT[sl], f'kT{sl}', Vh[sl], f'Vh{sl}', (qt + 1) * 128, scale,
                                           O[:, qt, h * 64:(h + 1) * 64], f'O{qt}', (P2[sl], PT2[sl], st2[sl]), maskblk, [f'qT{sl}'])


                    for hp in range(8):
                        gens = [mla_head(2 * hp), mla_head(2 * hp + 1)]
                        while gens:
                            for g_ in list(gens):
                                try:
                                    next(g_)
                                except StopIteration:
                                    gens.remove(g_)
                    sc.barrier()
            self.mixer_tail(b, l, O, lambda t: f'O{t}', self.W('mla_w_out', (o,), [D, D]))


    def hyb_sublayer(self, b, l):
        sc = self.sc
        e_ = l // 2
        if not hasattr(self, 'xspill'):
            self.xspill = self.nc.dram_tensor("xspill", [S, D], F32, kind="Internal").ap()
        win_d = self.W('hyb_w_in', (e_,), [D, 4176])
        with ExitStack() as esO:
            O = self.sb(esO, "O", [128, NT, D], BF16)
            uT = self.sb(esO, "uT_all", [128, 8, S], BF16)
            with ExitStack() as esA:
                SH, SC1, _, _, _ = self.modulation(esA, b, l, 1, 1.0, which=('SH', 'SC1'))
                utmp = [self.sb(esA, f"utmp{i}", [128, D], F32) for i in range(2)]
                ub = [self.sb(esA, f"ub{i}", [128, D], BF16) for i in range(2)]
                for t in range(NT):
                    self.mod_transpose_tile(t, SC1, SH, utmp, ub, uT, f'uT{t}', t * 128, 4 + t % 2)
                    sc.dma('sp', self.xspill[t * 128:(t + 1) * 128, :], self.X[t][:], reads=[f'X{t}'], writes=[f'xsp{t}'], chan='xsp')
                sc.barrier()
            Xb = [self.X[t][:].bitcast(BF16) for t in range(NT)]
            uTk = [f'uT{t}' for t in range(NT)]
            if HYB_DSA:
                self.dsa_part(b, l, e_, win_d, O, uT, Xb, uTk)
            else:
                for t in range(NT):
                    sc.op('dve', lambda e: e.memset(O[:, t, 0:512], 0.0), writes=[f'O{t}'])
            if HYB_GDN:
                self.gdn_part(b, l, e_, win_d, O, uT, Xb, uTk)
            else:
                for t in range(NT):
                    sc.op('dve', lambda e: e.memset(O[:, t, 512:1024], 0.0), writes=[f'O{t}'])
            sc.barrier()
            for t in range(NT):
                sc.dma('sp', self.X[t][:], self.xspill[t * 128:(t + 1) * 128, :], writes=[f'X{t}'], chan='xin')
            with ExitStack() as esT:
                pass
            self.mixer_tail(b, l, O, lambda t: f'O{t}', self.W('hyb_w_out', (e_,), [D, D]))

    def proj_featmajor(self, wchunk, wkeys, loads, uT, uTk, dst_of, evac_scale=1.0):
        sc = self.sc
        for ci, (pieces, dst, dkey) in enumerate(loads):
            sl = self._wc % 2
            self._wc += 1
            for (d0, d1, srcap) in pieces:
                sc.dma('pool', wchunk[sl][:, :, d0:d1], srcap.rearrange("(k p) n -> p k n", p=128), writes=[wkeys[sl]], chan=wkeys[sl])
            for g4 in range(4):
                pb = 4 + (g4 % 2)
                for k in range(8):
                    sc.op('pe', lambda e, k=k: e.matmul(self.ps[pb][:], lhsT=wchunk[sl][:, k, :], rhs=uT[:, k, g4 * 512:(g4 + 1) * 512],
                                                        start=(k == 0), stop=(k == 7)),
                          reads=[wkeys[sl]] + uTk[g4 * 4:g4 * 4 + 4], writes=[f'ps{pb}'])
                if g4 % 2 == 0:
                    sc.op('act', lambda e: e.activation(out=dst[:, g4 * 512:(g4 + 1) * 512], in_=self.ps[pb][:], func=AF.Copy, scale=evac_scale),
                          reads=[f'ps{pb}'], writes=[dkey])
                else:
                    sc.op('dve', lambda e: e.tensor_scalar(out=dst[:, g4 * 512:(g4 + 1) * 512], in0=self.ps[pb][:], scalar1=evac_scale, scalar2=None,
                                                           op0=ALU.mult), reads=[f'ps{pb}'], writes=[dkey])

    def dsa_part(self, b, l, e_, win_d, O, uT, Xb, uTk):
        sc = self.sc
        self._wc = 0
        with ExitStack() as es:
            wchunk = [self.sb(es, f"wch{i}", [128, 8, 128], BF16) for i in range(2)]
            wkeys = ['wch0', 'wch1']
            ikT2 = self.sb(es, "ikT2", [128, S], BF16)
            iw = self.sb(es, "iw", [128, NT, 8], F32)
            posrow = self.sb(es, "posrow", [128, S], F32)
            poscol = self.sb(es, "poscol", [128, NT], F32)
            acc = self.sb(es, "acc", [128, S], F32)
            MB = self.sb(es, "MB", [128, S], F32)
            Dt = self.sb(es, "Dt", [128, S], F32)
            bh = [self.sb(es, f"bh{i}", [128, S], F32) for i in range(2)]
            rl = [self.sb(es, f"rl{i}", [128, 512], F32) for i in range(2)]
            P = self.sb(es, "P", [128, S], BF16)
            junk = P
            PT = self.sb(es, "PT", [128, 16, 128], BF16)
            st = self.sb(es, "at_st", [128, 4], F32)
            bs = self.sb(es, "bs", [128, 8], F32)
            maskf = self.sb(es, "maskf", [128, 128], F32)
            with ExitStack() as es2:
                pri = Dt[:].bitcast(I32)
                pci = self.sb(es2, "pci", [128, NT], I32)
                mi = self.sb(es2, "mi", [128, 128], I32)
                sc.dma('sp', pri, self.pos[b:b + 1, :].partition_broadcast(128), writes=['pri'], chan='misc')
                sc.dma('sp', pci[:], self.pos[b].rearrange("(t p) -> p t", p=128), writes=['pci'], chan='misc', allow_slow_non_contiguous=True)
                sc.op('dve', lambda e: e.tensor_copy(out=posrow[:], in_=pri), reads=['pri'], writes=['posrow'])
                sc.op('dve', lambda e: e.tensor_copy(out=poscol[:], in_=pci[:]), reads=['pci'], writes=['poscol'])
                sc.op('dve', lambda e: e.tensor_scalar(out=poscol[:], in0=poscol[:], scalar1=-1.0, scalar2=None, op0=ALU.mult),
                      reads=['poscol'], writes=['poscol'])
                sc.op('pool', lambda e: e.iota(mi[:], pattern=[[1, 128]], base=0, channel_multiplier=0), writes=['mi'])
                sc.op('dve', lambda e: e.tensor_scalar(out=maskf[:], in0=mi[:], scalar1=64.0, scalar2=-1e30, op0=ALU.is_ge, op1=ALU.mult),
                      reads=['mi'], writes=['maskf'])
                sc.op('dve', lambda e: e.memset(maskf[64:128, :], 0.0), reads=['maskf'], writes=['maskf'])
                sc.barrier()
            loads = []
            for c in range(4):
                loads.append(([(0, 128, win_d[:, c * 128:(c + 1) * 128])], Xb[c], f'X{c}'))
            self.proj_featmajor(wchunk, wkeys, loads, uT, uTk, None, evac_scale=0.125)
            loads = []
            for c in range(4):
                loads.append(([(0, 128, win_d[:, 512 + c * 128:512 + (c + 1) * 128])], Xb[4 + c], f'X{4 + c}'))
            for c in range(4):
                loads.append(([(0, 128, win_d[:, 1536 + c * 128:1536 + (c + 1) * 128])], Xb[8 + c], f'X{8 + c}'))
            loads.append(([(0, 64, win_d[:, 2048:2112]), (64, 128, win_d[:, 2048:2112])], ikT2, 'ikT2'))
            self.proj_featmajor(wchunk, wkeys, loads, uT, uTk, None)
            with ExitStack() as es2:
                wv = MB[:].bitcast(BF16).rearrange("p (k n) -> p k n", k=8)
                wiw = self.sb(es2, "wiw", [128, 8, 8], BF16)
                sc.dma('pool', wv, win_d[:, 1024:1536].rearrange("(k p) n -> p k n", p=128), writes=['wv'], chan='wv')
                sc.dma('pool', wiw[:], win_d[:, 2112:2120].rearrange("(k p) n -> p k n", p=128), writes=['wiw'], chan='wv')
                for t in range(NT):
                    pb = 4 + t % 2
                    for k in range(8):
                        sc.op('pe', lambda e, k=k: e.matmul(self.ps[pb][:], lhsT=uT[:, k, t * 128:(t + 1) * 128], rhs=wv[:, k, :],
                                                            start=(k == 0), stop=(k == 7)), reads=[uTk[t], 'wv'], writes=[f'ps{pb}'])
                    sc.op('act', lambda e: e.activation(out=Xb[12 + t // 4][:, (t % 4) * 512:(t % 4 + 1) * 512], in_=self.ps[pb][:], func=AF.Copy),
                          reads=[f'ps{pb}'], writes=[f'X{12 + t // 4}'])
                    pb2 = 6 + t % 2
                    for k in range(8):
                        sc.op('pe', lambda e, k=k: e.matmul(self.ps[pb2][:, 0:8], lhsT=uT[:, k, t * 128:(t + 1) * 128], rhs=wiw[:, k, :],
                                                            start=(k == 0), stop=(k == 7)), reads=[uTk[t], 'wiw'], writes=[f'ps{pb2}'])
                    sc.op('dve', lambda e: e.tensor_copy(out=iw[:, t, :], in_=self.ps[pb2][:, 0:8]), reads=[f'ps{pb2}'], writes=['iw'])
                sc.barrier()
            NIT = 14
            for qt in range(NT):
                KV = (qt + 1) * 128
                qs = slice(qt * 128, (qt + 1) * 128)
                nkc = (KV + 511) // 512
                bank = 0
                for kc in range(nkc):
                    c0 = kc * 512
                    c1 = min(KV, c0 + 512)
                    for h in range(8):
                        pb = bank % 4
                        bank += 1
                        hp = slice((h % 2) * 64, (h % 2) * 64 + 64)
                        sc.op('pe', lambda e: e.matmul(self.ps[pb][:, 0:c1 - c0], lhsT=Xb[8 + h // 2][hp, qs], rhs=ikT2[hp, c0:c1],
                                                       start=True, stop=True),
                              reads=[f'X{8 + h // 2}', 'ikT2'], writes=[f'ps{pb}', 'psS'])
                        rs_ = h % 2
                        sc.op('act', lambda e: e.activation(out=rl[rs_][:, 0:c1 - c0], in_=self.ps[pb][:, 0:c1 - c0], func=AF.Relu),
                              reads=[f'ps{pb}'], writes=[f'rl{rs_}'])
                        if h == 0:
                            sc.op('dve', lambda e: e.tensor_scalar(out=acc[:, c0:c1], in0=rl[rs_][:, 0:c1 - c0], scalar1=iw[:, qt, 0:1], scalar2=None,
                                                                   op0=ALU.mult), reads=[f'rl{rs_}', 'iw'], writes=['acc'])
                        else:
                            sc.op('dve', lambda e: e.scalar_tensor_tensor(out=acc[:, c0:c1], in0=rl[rs_][:, 0:c1 - c0], scalar=iw[:, qt, h:h + 1],
                                                                          in1=acc[:, c0:c1], op0=ALU.mult, op1=ALU.add),
                                  reads=[f'rl{rs_}', 'iw', 'acc'], writes=['acc'])
                if KV > 256:
                    sc.op('dve', lambda e: e.tensor_reduce(out=bs[:, 0:1], in_=acc[:, 0:KV], axis=AX.X, op=ALU.max), reads=['acc'], writes=['bs_hi'])
                    sc.op('dve', lambda e: e.tensor_reduce(out=bs[:, 1:2], in_=acc[:, 0:KV], axis=AX.X, op=ALU.min), reads=['acc'], writes=['bs_lo'])
                    sc.op('dve', lambda e: e.tensor_tensor(out=bs[:, 2:3], in0=bs[:, 0:1], in1=bs[:, 1:2], op=ALU.subtract),
                          reads=['bs_hi', 'bs_lo'], writes=['bs_h'])
                sc.op('dve', lambda e: e.tensor_tensor(out=acc[:, KV - 128:KV], in0=acc[:, KV - 128:KV], in1=maskf[:], op=ALU.add),
                      reads=['acc', 'maskf'], writes=['acc'])
                if KV > 256:
                    for it in range(NIT):
                        sc.op('dve', lambda e: e.tensor_scalar(out=bs[:, 2:3], in0=bs[:, 2:3], scalar1=0.5, scalar2=None, op0=ALU.mult),
                              reads=['bs_h'], writes=['bs_h'])
                        sc.op('dve', lambda e: e.tensor_tensor(out=bs[:, 3:4], in0=bs[:, 1:2], in1=bs[:, 2:3], op=ALU.add),
                              reads=['bs_lo', 'bs_h'], writes=['bs_mid'])
                        sc.op('dve', lambda e: e.tensor_scalar(out=junk[:, 0:KV], in0=acc[:, 0:KV], scalar1=bs[:, 3:4], scalar2=0.0,
                                                               op0=ALU.is_ge, op1=ALU.add, accum_out=bs[:, 4:5]),
                              reads=['acc', 'bs_mid'], writes=['at_P', 'bs_cnt'])
                        sc.op('dve', lambda e: e.tensor_scalar(out=bs[:, 5:6], in0=bs[:, 4:5], scalar1=255.5, scalar2=bs[:, 2:3],
                                                               op0=ALU.is_ge, op1=ALU.mult), reads=['bs_cnt', 'bs_h'], writes=['bs_g'])
                        sc.op('dve', lambda e: e.tensor_tensor(out=bs[:, 1:2], in0=bs[:, 1:2], in1=bs[:, 5:6], op=ALU.add),
                              reads=['bs_lo', 'bs_g'], writes=['bs_lo'])
                    thr = bs[:, 1:2]
                else:
                    sc.op('dve', lambda e: e.memset(bs[:, 1:2], -1e29), writes=['bs_lo'])
                    thr = bs[:, 1:2]
                sc.op('dve', lambda e: e.tensor_scalar(out=MB[:, 0:KV], in0=acc[:, 0:KV], scalar1=thr, scalar2=-1e30, op0=ALU.is_lt, op1=ALU.mult),
                      reads=['acc', 'bs_lo'], writes=['MB'])
                sc.op('act', lambda e: e.activation(out=Dt[:, 0:KV], in_=posrow[:, 0:KV], func=AF.Abs, bias=poscol[:, qt:qt + 1], scale=1.0),
                      reads=['posrow', 'poscol'], writes=['Dt'])
                for h in range(8):
                    sl = h % 2
                    slope = float(2.0 ** (-(h + 1)))
                    sc.op('dve', lambda e: e.scalar_tensor_tensor(out=bh[sl][:, 0:KV], in0=Dt[:, 0:KV], scalar=-slope, in1=MB[:, 0:KV],
                                                                  op0=ALU.mult, op1=ALU.add), reads=['Dt', 'MB'], writes=[f'bh{sl}'])
                    hp = slice((h % 2) * 64, (h % 2) * 64 + 64)
                    self.attn_tile(Xb[h // 2][hp, qs], Xb[4 + h // 2][hp, :], f'X{4 + h // 2}', None, None, KV, 1.0,
                                   O[:, qt, h * 64:(h + 1) * 64], f'O{qt}', (P, PT, st), None, [f'X{h // 2}'],
                                   bias=bh[sl], bias_key=f'bh{sl}',
                                   v_of=lambda blk, h=h: (Xb[12 + blk // 4][:, (blk % 4) * 512 + h * 64:(blk % 4) * 512 + (h + 1) * 64], f'X{12 + blk // 4}'),
                                   dv=64)
            sc.barrier()


    def gdn_part(self, b, l, e_, win_d, O, uT, Xb, uTk):
        sc = self.sc
        self._wc = 0
        T_ = slice
        with ExitStack() as es:
            U = self.sb(es, "gU", [128, 128], F32)
            Li = self.sb(es, "gLi", [128, 128], F32)
            Ls = self.sb(es, "gLs", [128, 128], F32)
            ones = self.sb(es, "gones", [128, 128], F32)
            NG = self.sb(es, "gNG", [128, 128], F32)
            cw = self.sb(es, "gcw", [128, 12, 4], F32)
            prm = self.sb(es, "gprm", [128, 16], F32)
            epsb = self.sb(es, "gepsb", [128, 4], F32)
            ab = self.sb(es, "gab", [128, NT, 8], F32)
            gg = self.sb(es, "ggg", [128, NT, 4], F32)
            bt = self.sb(es, "gbt", [128, NT, 4], F32)
            sc.op('dve', lambda e: e.tensor_scalar(out=U[:], in0=self.iot[:], scalar1=0.0, scalar2=None, op0=ALU.is_ge), reads=['iot'], writes=['gU'])
            sc.op('dve', lambda e: e.tensor_scalar(out=Li[:], in0=self.iot[:], scalar1=0.0, scalar2=None, op0=ALU.is_le), reads=['iot'], writes=['gLi'])
            sc.op('dve', lambda e: e.tensor_scalar(out=Ls[:], in0=self.iot[:], scalar1=0.0, scalar2=None, op0=ALU.is_lt), reads=['iot'], writes=['gLs'])
            sc.op('dve', lambda e: e.memset(ones[:], 1.0), writes=['gones'])
            sc.op('dve', lambda e: e.memset(epsb[:, 0:1], RMS_EPS), writes=['gepsb'])
            sc.op('dve', lambda e: e.memset(epsb[:, 1:2], 128.0 * RMS_EPS), writes=['gepsb'])
            sc.dma('sp', NG[:], self.W('gdn_norm_g', (e_,), [1, 128]).partition_broadcast(128), writes=['gNG'], chan='misc')
            for j in range(4):
                sc.dma('sp', cw[:, :, j], self.W('gdn_conv_w', (e_,), [4, 1536])[j].rearrange("(c p) -> p c", p=128), writes=['gcw'], chan='misc',
                       allow_slow_non_contiguous=True)
            sc.dma('sp', prm[:, 0:4], self.W('gdn_dt_bias', (e_,), [1, 4]).partition_broadcast(128), writes=['gprm'], chan='misc')
            sc.dma('sp', prm[:, 4:8], self.W('gdn_a_log', (e_,), [1, 4]).partition_broadcast(128), writes=['gprm'], chan='misc')
            sc.op('act', lambda e: e.activation(out=prm[:, 8:12], in_=prm[:, 4:8], func=AF.Exp), reads=['gprm'], writes=['gprm'])
            sc.op('dve', lambda e: e.tensor_scalar(out=prm[:, 8:12], in0=prm[:, 8:12], scalar1=-1.0, scalar2=None, op0=ALU.mult),
                  reads=['gprm'], writes=['gprm'])
            with ExitStack() as es1:
                wchunk = [self.sb(es1, f"wch{i}", [128, 8, 128], BF16) for i in range(2)]
                raw = self.sb(es1, "graw", [128, S + 3], F32)
                y = self.sb(es1, "gy", [128, S], F32)
                sq = [self.sb(es1, f"gsq{i}", [128, 512], F32) for i in range(2)]
                rin = [self.sb(es1, f"grin{i}", [128, 512], F32) for i in range(2)]
                sc.op('dve', lambda e: e.memset(raw[:, 0:3], 0.0), writes=['graw'])
                for ci in range(12):
                    sl = ci % 2
                    c0 = 2120 + ci * 128
                    sc.dma('pool', wchunk[sl][:], win_d[:, c0:c0 + 128].rearrange("(k p) n -> p k n", p=128), writes=[f'wch{sl}'], chan=f'wch{sl}')
                    for g4 in range(4):
                        pb = 4 + (g4 % 2)
                        for k in range(8):
                            sc.op('pe', lambda e, k=k: e.matmul(self.ps[pb][:], lhsT=wchunk[sl][:, k, :], rhs=uT[:, k, g4 * 512:(g4 + 1) * 512],
                                                                start=(k == 0), stop=(k == 7)),
                                  reads=[f'wch{sl}'] + uTk[g4 * 4:g4 * 4 + 4], writes=[f'ps{pb}'])
                        sc.op('act', lambda e: e.activation(out=raw[:, 3 + g4 * 512:3 + (g4 + 1) * 512], in_=self.ps[pb][:], func=AF.Copy),
                              reads=[f'ps{pb}'], writes=['graw'])
                    sc.op('dve', lambda e: e.tensor_scalar(out=y[:], in0=raw[:, 0:S], scalar1=cw[:, ci, 0:1], scalar2=None, op0=ALU.mult),
                          reads=['graw', 'gcw'], writes=['gy'])
                    for j in range(1, 4):
                        sc.op('dve', lambda e, j=j: e.scalar_tensor_tensor(out=y[:], in0=raw[:, j:S + j], scalar=cw[:, ci, j:j + 1], in1=y[:],
                                                                           op0=ALU.mult, op1=ALU.add), reads=['graw', 'gcw', 'gy'], writes=['gy'])
                    dst = Xb[ci]
                    if ci >= 8:
                        sc.op('act', lambda e: e.activation(out=dst[:, :], in_=y[:], func=AF.Silu), reads=['gy'], writes=[f'X{ci}'])
                    else:
                        sc.op('act', lambda e: e.activation(out=y[:], in_=y[:], func=AF.Silu), reads=['gy'], writes=['gy'])
                        for g4 in range(4):
                            cs_ = slice(g4 * 512, (g4 + 1) * 512)
                            s2 = g4 % 2
                            sc.op('pool', lambda e: e.tensor_tensor(out=sq[s2][:], in0=y[:, cs_], in1=y[:, cs_], op=ALU.mult),
                                  reads=['gy'], writes=[f'gsq{s2}'])
                            pb = 6 + s2
                            sc.op('pe', lambda e: e.matmul(self.ps[pb][:], lhsT=ones[:], rhs=sq[s2][:], start=True, stop=True),
                                  reads=['gones', f'gsq{s2}'], writes=[f'ps{pb}'])
                            if ci < 4:
                                sc.op('act', lambda e: e.activation(out=rin[s2][:], in_=self.ps[pb][:], func=AF.Sqrt, bias=epsb[:, 1:2], scale=128.0),
                                      reads=[f'ps{pb}', 'gepsb'], writes=[f'grin{s2}'])
                            else:
                                sc.op('act', lambda e: e.activation(out=rin[s2][:], in_=self.ps[pb][:], func=AF.Sqrt, bias=epsb[:, 0:1], scale=1.0),
                                      reads=[f'ps{pb}', 'gepsb'], writes=[f'grin{s2}'])
                            sc.op('dve', lambda e: e.reciprocal(out=rin[s2][:], in_=rin[s2][:]), reads=[f'grin{s2}'], writes=[f'grin{s2}'])
                            sc.op('dve', lambda e: e.tensor_tensor(out=dst[:, cs_], in0=y[:, cs_], in1=rin[s2][:], op=ALU.mult),
                                  reads=['gy', f'grin{s2}'], writes=[f'X{ci}'])
                sc.barrier()
            if GDN_STOP <= 1:
                for t in range(NT):
                    sc.op('dve', lambda e: e.memset(O[:, t, 512:1024], 0.0), writes=[f'O{t}'])
                sc.barrier()
                return
            with ExitStack() as es2:
                wz = self.sb(es2, "gwz", [128, 8, 512], BF16)
                wab = self.sb(es2, "gwab", [128, 8, 8], BF16)
                tmp = self.sb(es2, "gtmp", [128, NT, 4], F32)
                sc.dma('pool', wz[:], win_d[:, 3664:4176].rearrange("(k p) n -> p k n", p=128), writes=['gwz'], chan='wv')
                sc.dma('pool', wab[:], win_d[:, 3656:3664].rearrange("(k p) n -> p k n", p=128), writes=['gwab'], chan='wv')
                for t in range(NT):
                    pb = 4 + t % 2
                    for k in range(8):
                        sc.op('pe', lambda e, k=k: e.matmul(self.ps[pb][:], lhsT=uT[:, k, t * 128:(t + 1) * 128], rhs=wz[:, k, :],
                                                            start=(k == 0), stop=(k == 7)), reads=[uTk[t], 'gwz'], writes=[f'ps{pb}'])
                    sc.op('act', lambda e: e.activation(out=Xb[12 + t // 4][:, (t % 4) * 512:(t % 4 + 1) * 512], in_=self.ps[pb][:], func=AF.Silu),
                          reads=[f'ps{pb}'], writes=[f'X{12 + t // 4}'])
                    pb2 = 6 + t % 2
                    for k in range(8):
                        sc.op('pe', lambda e, k=k: e.matmul(self.ps[pb2][:, 0:8], lhsT=uT[:, k, t * 128:(t + 1) * 128], rhs=wab[:, k, :],
                                                            start=(k == 0), stop=(k == 7)), reads=[uTk[t], 'gwab'], writes=[f'ps{pb2}'])
                    sc.op('dve', lambda e: e.tensor_copy(out=ab[:, t, :], in_=self.ps[pb2][:, 0:8]), reads=[f'ps{pb2}'], writes=['gab'])
                bc4 = lambda ap: ap.unsqueeze(1).to_broadcast([128, NT, 4])
                sc.op('dve', lambda e: e.tensor_tensor(out=tmp[:], in0=ab[:, :, 0:4], in1=bc4(prm[:, 0:4]), op=ALU.add),
                      reads=['gab', 'gprm'], writes=['gtmp'])
                sc.op('act', lambda e: e.activation(out=tmp[:], in_=tmp[:], func=AF.Exp), reads=['gtmp'], writes=['gtmp'])
                sc.op('act', lambda e: e.activation(out=tmp[:], in_=tmp[:], func=AF.Ln, bias=1.0, scale=1.0), reads=['gtmp'], writes=['gtmp'])
                sc.op('dve', lambda e: e.tensor_tensor(out=gg[:], in0=tmp[:], in1=bc4(prm[:, 8:12]), op=ALU.mult),
                      reads=['gtmp', 'gprm'], writes=['ggg'])
                sc.op('act', lambda e: e.activation(out=bt[:], in_=ab[:, :, 4:8], func=AF.Sigmoid), reads=['gab'], writes=['gbt'])
                sc.barrier()
            if GDN_STOP <= 2:
                for t in range(NT):
                    sc.op('dve', lambda e: e.memset(O[:, t, 512:1024], 0.0), writes=[f'O{t}'])
                sc.barrier()
                return
            with ExitStack() as es3:
                f32t = lambda n: self.sb(es3, n, [128, 128], F32)
                b16t = lambda n: self.sb(es3, n, [128, 128], BF16)
                NS = 4
                B = []
                for i in range(NS):
                    B.append(dict(gb=f32t(f"g_gb{i}"), dd=f32t(f"g_dd{i}"), E=f32t(f"g_E{i}"), ET=f32t(f"g_ET{i}"),
                                  Pa=f32t(f"g_Pa{i}"), Pb=f32t(f"g_Pb{i}"), Qa=f32t(f"g_Qa{i}"), Qb=f32t(f"g_Qb{i}"),
                                  R=f32t(f"g_R{i}"), Rb=b16t(f"g_Rb{i}"), AT=b16t(f"g_AT{i}"), vb=b16t(f"g_vb{i}"),
                                  kbg=b16t(f"g_kbg{i}"), kdec=b16t(f"g_kdec{i}"), u=f32t(f"g_u{i}"), wT=b16t(f"g_wT{i}"),
                                  vnb=b16t(f"g_vnb{i}"), o=f32t(f"g_o{i}"), o2=f32t(f"g_o2{i}"), sm=self.sb(es3, f"g_sm{i}", [128, 16], F32)))
                Sst = [f32t(f"g_S{h}") for h in range(4)]
                Sbf = [b16t(f"g_Sb{h}") for h in range(4)]
                for h in range(4):
                    sc.op('dve', lambda e: e.memset(Sst[h][:], 0.0), writes=[f'gS{h}'])
                    sc.op('dve', lambda e: e.memset(Sbf[h][:], 0.0), writes=[f'gSb{h}'])
                q = lambda pb, j: self.ps[pb][:, j * 128:(j + 1) * 128]
                for t in range(0 if G3_LEVEL < 5 else GDN_NT, NT):
                    sc.op('dve', lambda e: e.memset(O[:, t, 512:1024], 0.0), writes=[f'O{t}'])
                def step(t, h):
                    ts_ = slice(t * 128, (t + 1) * 128)
                    PA = lambda j: self.ps[2 * h][:, j * 128:(j + 1) * 128]
                    PB = lambda j: self.ps[2 * h + 1][:, j * 128:(j + 1) * 128]
                    kA, kB = f'ps{2 * h}', f'ps{2 * h + 1}'
                    i = h % NS
                    bb = B[i]
                    K = lambda n: f'g_{n}{i}'
                    sm = bb['sm']
                    qT, kT, vT = Xb[h][:, ts_], Xb[4 + h][:, ts_], Xb[8 + h][:, ts_]
                    kq, kk_, kv = f'X{h}', f'X{4 + h}', f'X{8 + h}'
                    gcol = gg[:, t, h:h + 1]
                    bcol = bt[:, t, h:h + 1]
                    sc.op('dve', lambda e: e.tensor_copy(out=bb['gb'][:], in_=gcol.to_broadcast([128, 128])), reads=['ggg'], writes=[K('gb')])
                    yield
                    sc.op('pe', lambda e: e.matmul(PB(0), lhsT=U[:], rhs=bb['gb'][:], start=True, stop=True),
                          reads=['gU', K('gb')], writes=[kB])
                    yield
                    sc.op('pe', lambda e: e.matmul(PA(0), lhsT=bb['gb'][:], rhs=U[:], start=True, stop=True),
                          reads=['gU', K('gb')], writes=[kA])
                    yield
                    sc.op('act', lambda e: e.activation(out=sm[:, 0:1], in_=PB(0)[:, 0:1], func=AF.Copy), reads=[kB], writes=[K('sm0')])
                    yield
                    sc.op('dve', lambda e: e.tensor_scalar(out=bb['dd'][:], in0=PA(0), scalar1=sm[:, 0:1], scalar2=0.0,
                                                           op0=ALU.subtract, op1=ALU.max), reads=[kA, K('sm0')], writes=[K('dd')])
                    yield
                    sc.op('act', lambda e: e.activation(out=bb['E'][:], in_=bb['dd'][:], func=AF.Exp, scale=-1.0), reads=[K('dd')], writes=[K('E')])
                    yield
                    sc.op('dve', lambda e: e.tensor_scalar(out=bb['dd'][:], in0=PA(0), scalar1=sm[:, 0:1], scalar2=0.0,
                                                           op0=ALU.subtract, op1=ALU.min), reads=[kA, K('sm0'), K('dd')], writes=[K('dd')])
                    yield
                    sc.op('act', lambda e: e.activation(out=bb['ET'][:], in_=bb['dd'][:], func=AF.Exp), reads=[K('dd')], writes=[K('ET')])
                    yield
                    sc.op('pool', lambda e: e.tensor_tensor(out=bb['ET'][:], in0=bb['ET'][:], in1=U[:], op=ALU.mult), reads=[K('ET'), 'gU'], writes=[K('ET')])
                    yield
                    sc.op('pool', lambda e: e.tensor_tensor(out=bb['E'][:], in0=bb['E'][:], in1=Ls[:], op=ALU.mult), reads=[K('E'), 'gLs'], writes=[K('E')])
                    yield
                    sc.op('act', lambda e: e.activation(out=sm[:, 1:2], in_=sm[:, 0:1], func=AF.Exp), reads=[K('sm0')], writes=[K('sm1')])
                    yield
                    sc.op('dve', lambda e: e.tensor_tensor(out=sm[:, 2:3], in0=sm[:, 1:2], in1=bcol, op=ALU.mult), reads=[K('sm1'), 'gbt'], writes=[K('sm2')])
                    yield
                    sc.op('dve', lambda e: e.tensor_tensor(out=sm[:, 3:4], in0=PA(0)[:, 127:128], in1=sm[:, 0:1], op=ALU.subtract),
                          reads=[kA, K('sm0')], writes=[K('sm3')])
                    yield
                    sc.op('act', lambda e: e.activation(out=sm[:, 3:4], in_=sm[:, 3:4], func=AF.Exp), reads=[K('sm3')], writes=[K('sm3')])
                    yield
                    sc.op('dve', lambda e: e.tensor_copy(out=sm[:, 8:9], in_=PA(0)[:, 127:128]), reads=[kA], writes=[K('sm8')])
                    yield
                    sc.op('act', lambda e: e.activation(out=sm[:, 4:5], in_=sm[:, 8:9], func=AF.Exp), reads=[K('sm8')], writes=[K('sm4')])
                    yield
                    if G3_LEVEL < 2:
                        return
                    sc.op('pe', lambda e: e.matmul(PA(1), lhsT=kT, rhs=kT, start=True, stop=True), reads=[kk_], writes=[kA])
                    yield
                    sc.op('pe', lambda e: e.matmul(PA(2), lhsT=kT, rhs=qT, start=True, stop=True), reads=[kk_, kq], writes=[kA])
                    yield
                    if G3_LEVEL < 2.07:
                        return
                    sc.op('dve', lambda e: e.scalar_tensor_tensor(out=bb['Qa'][:], in0=PA(1), scalar=bcol, in1=bb['E'][:], op0=ALU.mult, op1=ALU.mult),
                          reads=[kA, 'gbt', K('E')], writes=[K('Qa')])
                    yield
                    sc.op('dve', lambda e: e.tensor_tensor(out=bb['AT'][:], in0=PA(2), in1=bb['ET'][:], op=ALU.mult),
                          reads=[kA, K('ET')], writes=[K('AT')])
                    yield
                    if G3_LEVEL < 2.15:
                        return
                    sc.op('pe', lambda e: e.matmul(PB(1), lhsT=bb['Qa'][:], rhs=self.ident_f[:], start=True, stop=True), reads=[K('Qa'), 'ident_f'], writes=[kB])
                    yield
                    sc.op('act', lambda e: e.activation(out=bb['Pa'][:], in_=PB(1), func=AF.Copy), reads=[kB], writes=[K('Pa')])
                    yield
                    sc.op('dve', lambda e: e.tensor_tensor(out=bb['R'][:], in0=self.ident_f[:], in1=bb['Pa'][:], op=ALU.subtract),
                          reads=[K('Pa'), 'ident_f'], writes=[K('R')])
                    yield
                    if G3_LEVEL < 2.25:
                        return
                    pk = PB(2).bitcast(BF16)
                    sc.op('pe', lambda e: e.transpose(out=pk[:, 0:128], in_=kT, identity=self.ident_b[:]), reads=[kk_, 'ident_b'], writes=[kB])
                    yield
                    sc.op('pe', lambda e: e.transpose(out=pk[:, 128:256], in_=vT, identity=self.ident_b[:]), reads=[kv, 'ident_b'], writes=[kB])
                    yield
                    sc.op('act', lambda e: e.activation(out=bb['kbg'][:], in_=pk[:, 0:128], func=AF.Copy, scale=sm[:, 2:3]), reads=[kB, K('sm2')], writes=[K('kbg')])
                    yield
                    sc.op('act', lambda e: e.activation(out=bb['kdec'][:], in_=pk[:, 0:128], func=AF.Copy, scale=sm[:, 3:4]), reads=[kB, K('sm3')], writes=[K('kdec')])
                    yield
                    sc.op('act', lambda e: e.activation(out=bb['vb'][:], in_=pk[:, 128:256], func=AF.Copy, scale=bcol),
                          reads=[kB, 'gbt'], writes=[K('vb')])
                    yield
                    if G3_LEVEL < 3:
                        return
                    P_, Q_ = bb['Pa'], bb['Qa']
                    Pn, Qn = bb['Pb'], bb['Qb']
                    kP, kQ, kPn, kQn = K('Pa'), K('Qa'), K('Pb'), K('Qb')
                    for lev in range(6):
                        lastl = (lev == 5)
                        sc.op('pe', lambda e: e.matmul(PB(3), lhsT=P_[:], rhs=Q_[:], start=True, stop=True), reads=[kP, kQ], writes=[kB])
                        yield
                        sc.op('act', lambda e: e.activation(out=Qn[:], in_=PB(3), func=AF.Copy), reads=[kB], writes=[kQn])
                        yield
                        if not lastl:
                            sc.op('pe', lambda e: e.matmul(PB(1), lhsT=Q_[:], rhs=P_[:], start=True, stop=True), reads=[kP, kQ], writes=[kB])
                            yield
                            sc.op('act', lambda e: e.activation(out=Pn[:], in_=PB(1), func=AF.Copy), reads=[kB], writes=[kPn])
                            yield
                        sc.op('pe', lambda e: e.matmul(PA(3), lhsT=Qn[:], rhs=bb['R'][:], start=True, stop=True), reads=[kQn, K('R')], writes=[kA])
                        yield
                        sc.op('dve', lambda e: e.tensor_tensor(out=bb['R'][:], in0=bb['R'][:], in1=PA(3), op=ALU.add), reads=[kA, K('R')], writes=[K('R')])
                        yield
                        P_, Pn = Pn, P_
                        Q_, Qn = Qn, Q_
                        kP, kPn = kPn, kP
                        kQ, kQn = kQn, kQ
                    if G3_LEVEL < 4:
                        return
                    sc.op('act', lambda e: e.activation(out=bb['Rb'][:], in_=bb['R'][:], func=AF.Copy), reads=[K('R')], writes=[K('Rb')])
                    yield
                    sc.op('pe', lambda e: e.matmul(PB(0), lhsT=bb['Rb'][:], rhs=bb['vb'][:], start=True, stop=True), reads=[K('Rb'), K('vb')], writes=[kB])
                    yield
                    sc.op('pe', lambda e: e.matmul(PB(1), lhsT=bb['kbg'][:], rhs=bb['Rb'][:], start=True, stop=True), reads=[K('Rb'), K('kbg')], writes=[kB])
                    yield
                    sc.op('act', lambda e: e.activation(out=bb['u'][:], in_=PB(0), func=AF.Copy), reads=[kB], writes=[K('u')])
                    yield
                    sc.op('act', lambda e: e.activation(out=bb['wT'][:], in_=PB(1), func=AF.Copy), reads=[kB], writes=[K('wT')])
                    yield
                    sc.op('pe', lambda e: e.matmul(PA(0), lhsT=bb['wT'][:], rhs=Sbf[h][:], start=True, stop=True), reads=[K('wT'), f'gSb{h}'], writes=[kA])
                    yield
                    sc.op('pe', lambda e: e.matmul(PA(1), lhsT=qT, rhs=Sbf[h][:], start=True, stop=True), reads=[kq, f'gSb{h}'], writes=[kA])
                    yield
                    sc.op('dve', lambda e: e.tensor_tensor(out=bb['vnb'][:], in0=bb['u'][:], in1=PA(0), op=ALU.subtract), reads=[kA, K('u')], writes=[K('vnb')])
                    yield
                    sc.op('pe', lambda e: e.matmul(PA(2), lhsT=bb['AT'][:], rhs=bb['vnb'][:], start=True, stop=True), reads=[K('AT'), K('vnb')], writes=[kA])
                    yield
                    sc.op('pe', lambda e: e.matmul(PA(3), lhsT=bb['kdec'][:], rhs=bb['vnb'][:], start=True, stop=True), reads=[K('kdec'), K('vnb')], writes=[kA])
                    yield
                    sc.op('dve', lambda e: e.tensor_scalar(out=bb['o'][:], in0=PA(1), scalar1=sm[:, 1:2], scalar2=None, op0=ALU.mult),
                          reads=[kA, K('sm1')], writes=[K('o')])
                    yield
                    sc.op('dve', lambda e: e.tensor_tensor(out=bb['o'][:], in0=bb['o'][:], in1=PA(2), op=ALU.add), reads=[kA, K('o')], writes=[K('o')])
                    yield
                    sc.op('dve', lambda e: e.scalar_tensor_tensor(out=Sst[h][:], in0=Sst[h][:], scalar=sm[:, 4:5], in1=PA(3), op0=ALU.mult, op1=ALU.add),
                          reads=[kA, K('sm4'), f'gS{h}'], writes=[f'gS{h}'])
                    yield
                    sc.op('act', lambda e: e.activation(out=Sbf[h][:], in_=Sst[h][:], func=AF.Copy), reads=[f'gS{h}'], writes=[f'gSb{h}'])
                    yield
                    if G3_LEVEL < 5:
                        return
                    sc.op('act', lambda e: e.activation(out=bb['o2'][:], in_=bb['o'][:], func=AF.Square, accum_out=sm[:, 5:6]),
                          reads=[K('o')], writes=[K('o2'), K('sm5')])
                    yield
                    sc.op('act', lambda e: e.activation(out=sm[:, 6:7], in_=sm[:, 5:6], func=AF.Sqrt, bias=epsb[:, 0:1], scale=1.0 / 128),
                          reads=[K('sm5'), 'gepsb'], writes=[K('sm6')])
                    yield
                    sc.op('dve', lambda e: e.reciprocal(out=sm[:, 7:8], in_=sm[:, 6:7]), reads=[K('sm6')], writes=[K('sm7')])
                    yield
                    sc.op('dve', lambda e: e.scalar_tensor_tensor(out=bb['o2'][:], in0=bb['o'][:], scalar=sm[:, 7:8], in1=NG[:], op0=ALU.mult, op1=ALU.mult),
                          reads=[K('o'), K('sm7'), 'gNG', K('o2')], writes=[K('o2')])
                    yield
                    zs = Xb[12 + t // 4][:, (t % 4) * 512 + h * 128:(t % 4) * 512 + (h + 1) * 128]
                    sc.op('pool', lambda e: e.tensor_tensor(out=O[:, t, 512 + h * 128:512 + (h + 1) * 128], in0=bb['o2'][:], in1=zs, op=ALU.mult),
                          reads=[K('o2'), f'X{12 + t // 4}'], writes=[f'O{t}'])
                    yield
                for t in range(GDN_NT):
                    gens = [step(t, h) for h in range(4)]
                    while gens:
                        for g_ in list(gens):
                            try:
                                next(g_)
                            except StopIteration:
                                gens.remove(g_)
                sc.barrier()


_PLAN = FULL_PLAN
POOL_ELEM = 'dve'
DEBUG = False
HYB_DSA = True
HYB_GDN = True
GDN_STOP = 99
GDN_NT = NT
G3_LEVEL = 5
_LAST = None
_NINS = 0


def kernel(**inputs):
    n = 8
    bld = Builder(_PLAN)
    nc = bld.build()
    global _NINS
    _NINS = bld.sc.n_ins
    wviews = {}
    for name, (key, idx, cols) in bld.wsrc.items():
        a = np.asarray(inputs[key])[idx]
        if cols is not None:
            a = a[..., cols[0]:cols[1]]
        wviews[name] = np.ascontiguousarray(a).reshape(bld.win[name].shape)
    in_maps = []
    for i in range(n):
        m = {}
        for k in ("x", "c", "positions"):
            m[k] = np.ascontiguousarray(np.asarray(inputs[k])[i * NB:(i + 1) * NB])
        for name, arr in wviews.items():
            m[name] = arr
        in_maps.append(m)
    res = run_bass_kernel_spmd(nc, in_maps, core_ids=list(range(n)))
    global _LAST
    _LAST = res
    return np.concatenate([r["out"] for r in res.results], axis=0)
```
